# Optimizing a Trainium2 kernel written in Bass

```python
import math
import jax
import jax.numpy as jnp
from jax import lax
import numpy as np

D_MODEL = 1024
BATCH = 8
SEQ = 2048
DEPTH = 2

GRID_W = 64
CTX_LEN = 256
NORM_EPS = 1e-6

RW_HEAD_DIM = 64
RW_DIM = D_MODEL // 2
RW_HEADS = RW_DIM // RW_HEAD_DIM
RW_LORA_W = 32
RW_LORA_A = 32
RW_LORA_G = 96
RW_GN_EPS = 64e-5
RW_COLS = 3 * RW_DIM + 2 * RW_LORA_W + 2 * RW_LORA_A + RW_LORA_G
RW_SPLITS = (RW_DIM, 2 * RW_DIM, 3 * RW_DIM, 3 * RW_DIM + 2 * RW_LORA_W, 3 * RW_DIM + 2 * RW_LORA_W + 2 * RW_LORA_A)

MB_HEAD_DIM = 64
MB_DIM = D_MODEL // 2
MB_HEADS = MB_DIM // MB_HEAD_DIM
MB_GROUPS = 2
MB_STATE = 128
MB_CONV = 5
MB_CHUNK = 128
MB_XBC = MB_DIM + 2 * MB_GROUPS * MB_STATE
MB_COLS = MB_DIM + MB_XBC + 2 * MB_HEADS
MB_SPLITS = (MB_DIM, MB_DIM + MB_XBC)

MIX_IN = RW_COLS + MB_COLS
MIX_OUT = RW_DIM + MB_DIM

NA_HEAD_DIM = 64
NA_HEADS = D_MODEL // NA_HEAD_DIM
NA_KH = 8
NA_KW = 16
NA_QB = 16
NA_KB = 32

D_FF = 2816
N_EXPERTS = 8
TOP_K = 2

kernel_name = 'hybrid_rwkv7_mamba2_natten_moe_dit'


def _rms_norm(h, g):
    hf = h.astype(jnp.float32)
    hf = hf * lax.rsqrt(jnp.mean(hf * hf, axis=-1, keepdims=True) + NORM_EPS)
    return (hf * g.astype(jnp.float32)).astype(h.dtype)


def _modulate(h, shift, scale):
    return h * (1 + scale) + shift


def _heads(t, n_heads):
    return t.reshape(t.shape[:-1] + (n_heads, t.shape[-1] // n_heads))


def _swiglu(h, w1, w3, w2):
    return (jax.nn.silu(h @ w1) * (h @ w3)) @ w2


def _centred_shift(z, mu_prev, mu_next):
    z_prev = jnp.pad(z, ((0, 0), (1, 0), (0, 0)))[:, :-1]
    z_next = jnp.pad(z, ((0, 0), (0, 1), (0, 0)))[:, 1:]
    return z + mu_prev * (z_prev - z) + mu_next * (z_next - z)


def _centred_dwconv(x, w, b):
    k_w, ch = w.shape
    y = lax.conv_general_dilated(x, w[:, None, :].astype(x.dtype), window_strides=(1,),
                                 padding=[(k_w // 2, k_w // 2)],
                                 dimension_numbers=('NWC', 'WIO', 'NWC'), feature_group_count=ch)
    return y + b


def _rwkv7_scan(s0, r, w, k, v, a, b):
    def step(S, inp):
        r_t, w_t, k_t, v_t, a_t, b_t = inp
        sa = jnp.einsum('bhvk,bhk->bhv', S, a_t)
        S = S * w_t[:, :, None, :] + sa[..., None] * b_t[:, :, None, :] + v_t[..., None] * k_t[:, :, None, :]
        return S, jnp.einsum('bhvk,bhk->bhv', S, r_t)
    xs = tuple(jnp.moveaxis(t, 1, 0) for t in (r, w, k, v, a, b))
    s_fin, ys = lax.scan(step, s0, xs)
    return s_fin, jnp.moveaxis(ys, 0, 1)


def _segsum(x):
    n = x.shape[-1]
    cs = jnp.cumsum(x, axis=-1)
    return jnp.where(np.tril(np.ones((n, n), dtype=bool)), cs[..., :, None] - cs[..., None, :], -jnp.inf)


def _ssd_chunked(s0, X, A, Bm, Cm):
    b, t, nh, p = X.shape
    n = Bm.shape[-1]
    nc, cl = t // MB_CHUNK, MB_CHUNK
    X = X.reshape(b, nc, cl, nh, p)
    Bm = Bm.reshape(b, nc, cl, nh, n)
    Cm = Cm.reshape(b, nc, cl, nh, n)
    A = A.reshape(b, nc, cl, nh).transpose(0, 3, 1, 2)
    A_cs = jnp.cumsum(A, axis=-1)
    L = jnp.exp(_segsum(A))
    y_diag = jnp.einsum('bclhn,bcshn,bhcls,bcshp->bclhp', Cm, Bm, L, X)
    decay_states = jnp.exp(A_cs[..., -1:] - A_cs)
    states = jnp.einsum('bclhn,bhcl,bclhp->bchpn', Bm, decay_states, X)
    states = jnp.concatenate([s0[:, None], states], axis=1)
    chunk_decay = jnp.exp(_segsum(jnp.pad(A_cs[..., -1], ((0, 0), (0, 0), (1, 0)))))
    states = jnp.einsum('bhzc,bchpn->bzhpn', chunk_decay, states)
    y_off = jnp.einsum('bclhn,bchpn,bhcl->bclhp', Cm, states[:, :-1], jnp.exp(A_cs))
    return states[:, -1], (y_diag + y_off).reshape(b, t, nh, p)


def _prefix_scan(scan_fn, ctx_args, lat_args, s0, reverse):
    if reverse:
        ctx_args = tuple(jnp.flip(t, axis=1) for t in ctx_args)
        lat_args = tuple(jnp.flip(t, axis=1) for t in lat_args)
    s_ctx, y_ctx = scan_fn(s0, *ctx_args)
    _, y_lat = scan_fn(s_ctx, *lat_args)
    if reverse:
        y_ctx, y_lat = jnp.flip(y_ctx, axis=1), jnp.flip(y_lat, axis=1)
    return y_ctx, y_lat


def _rwkv_mamba_mixer(a_ctx, a_lat, w_in, w_out, rw_mu, rw_w0, rw_w_up, rw_a0, rw_a_up, rw_g_up,
                      rw_k_k, rw_k_a, rw_r_k, rw_gn_w, rw_gn_b, mb_conv_w, mb_conv_b,
                      mb_dt_bias, mb_a_log, mb_d, mb_norm_w):
    f32 = jnp.float32

    def prepare(h):
        b, t, _ = h.shape
        z = h @ w_in
        z_rw = _centred_shift(z[..., :RW_COLS], rw_mu[0], rw_mu[1])
        r, k, v, lw, la, lg = jnp.split(z_rw, RW_SPLITS, axis=-1)
        lw = lw.reshape(b, t, 2, RW_LORA_W)
        la = la.reshape(b, t, 2, RW_LORA_A)
        logw = (rw_w0 + jnp.einsum('btdl,dlc->btdc', jnp.tanh(lw), rw_w_up)).astype(f32)
        decay = jnp.exp(-jnp.exp(-jax.nn.softplus(-logw) - 0.5))
        iclr = jax.nn.sigmoid((rw_a0 + jnp.einsum('btdl,dlc->btdc', la, rw_a_up)).astype(f32))
        gate = jax.nn.sigmoid(lg) @ rw_g_up
        kk = _heads(k * rw_k_k, RW_HEADS).astype(f32)
        kk = kk / jnp.maximum(jnp.sqrt(jnp.sum(kk * kk, axis=-1, keepdims=True)), 1e-12)
        k_dir = _heads((k[:, :, None, :] * (1 + (iclr - 1) * rw_k_a)).astype(f32), RW_HEADS)
        rh = _heads(r.astype(f32), RW_HEADS)
        vh = _heads(v.astype(f32), RW_HEADS)
        dh = _heads(decay, RW_HEADS)
        ih = _heads(iclr, RW_HEADS)
        rw_args = [(rh, dh[:, :, d], k_dir[:, :, d], vh, -kk, kk * ih[:, :, d]) for d in range(2)]
        bonus_f = jnp.sum(rh * k_dir[:, :, 0] * rw_r_k[0].astype(f32), axis=-1, keepdims=True)
        bonus_b = jnp.sum(rh * k_dir[:, :, 1] * rw_r_k[1].astype(f32), axis=-1, keepdims=True)
        bonus = (bonus_f + bonus_b) * vh
        zg, xbc, dt_raw = jnp.split(z[..., RW_COLS:], MB_SPLITS, axis=-1)
        xbc = jax.nn.silu(_centred_dwconv(xbc, mb_conv_w, mb_conv_b))
        xm, bm, cm = jnp.split(xbc, (MB_DIM, MB_DIM + MB_GROUPS * MB_STATE), axis=-1)
        xm = _heads(xm.astype(f32), MB_HEADS)
        rep = MB_HEADS // MB_GROUPS
        bm = jnp.repeat(_heads(bm.astype(f32), MB_GROUPS), rep, axis=2)
        cm = jnp.repeat(_heads(cm.astype(f32), MB_GROUPS), rep, axis=2)
        dt = jax.nn.softplus(dt_raw.reshape(b, t, 2, MB_HEADS).astype(f32) + mb_dt_bias.astype(f32))
        a_neg = -jnp.exp(mb_a_log.astype(f32))
        mb_args = [(xm * dt[:, :, d, :, None], dt[:, :, d] * a_neg[d], bm, cm) for d in range(2)]
        return rw_args, mb_args, bonus, gate, xm, zg

    pc, pl = prepare(a_ctx), prepare(a_lat)
    bsz = a_lat.shape[0]
    s0_rw = jnp.zeros((bsz, RW_HEADS, RW_HEAD_DIM, RW_HEAD_DIM), f32)
    s0_mb = jnp.zeros((bsz, MB_HEADS, MB_HEAD_DIM, MB_STATE), f32)
    yrf_c, yrf_l = _prefix_scan(_rwkv7_scan, pc[0][0], pl[0][0], s0_rw, False)
    yrb_c, yrb_l = _prefix_scan(_rwkv7_scan, pc[0][1], pl[0][1], s0_rw, True)
    ymf_c, ymf_l = _prefix_scan(_ssd_chunked, pc[1][0], pl[1][0], s0_mb, False)
    ymb_c, ymb_l = _prefix_scan(_ssd_chunked, pc[1][1], pl[1][1], s0_mb, True)

    def finish(h, p, y_rw, y_mb):
        b, t, _ = h.shape
        _, _, bonus, gate, xm, zg = p
        mu = jnp.mean(y_rw, axis=-1, keepdims=True)
        var = jnp.mean(jnp.square(y_rw - mu), axis=-1, keepdims=True)
        y_rw = ((y_rw - mu) * lax.rsqrt(var + RW_GN_EPS)).reshape(b, t, RW_DIM) * rw_gn_w + rw_gn_b
        o_rw = (y_rw + bonus.reshape(b, t, RW_DIM)) * gate
        y_mb = (y_mb + mb_d.astype(f32)[:, None] * xm).reshape(b, t, MB_DIM) * jax.nn.silu(zg.astype(f32))
        y_mb = y_mb.reshape(b, t, MB_GROUPS, MB_DIM // MB_GROUPS)
        y_mb = (y_mb * lax.rsqrt(jnp.mean(y_mb * y_mb, axis=-1, keepdims=True) + NORM_EPS)).reshape(b, t, MB_DIM) * mb_norm_w
        return jnp.concatenate([o_rw, y_mb], axis=-1).astype(h.dtype) @ w_out

    return (finish(a_ctx, pc, yrf_c + yrb_c, ymf_c + ymb_c),
            finish(a_lat, pl, yrf_l + yrb_l, ymf_l + ymb_l))


def _neighbourhood_attention(a_ctx, a_lat, w_qkv, w_out, rpb, with_ctx_queries):
    b, t, d_model = a_lat.shape
    rows = t // GRID_W
    kh = min(NA_KH, rows)
    scale = NA_HEAD_DIM ** -0.5
    q, k, v = jnp.split(a_lat @ w_qkv, 3, axis=-1)
    qc, kc, vc = jnp.split(a_ctx @ w_qkv, 3, axis=-1)
    q_grid = (_heads(q, NA_HEADS) * scale).reshape(b, rows, GRID_W, NA_HEADS, NA_HEAD_DIM)
    k_grid = _heads(k, NA_HEADS).reshape(b, rows, GRID_W, NA_HEADS, NA_HEAD_DIM)
    v_grid = _heads(v, NA_HEADS).reshape(b, rows, GRID_W, NA_HEADS, NA_HEAD_DIM)
    qc, kc, vc = _heads(qc, NA_HEADS) * scale, _heads(kc, NA_HEADS), _heads(vc, NA_HEADS)

    n_cb = GRID_W // NA_QB
    q_cols = np.arange(GRID_W).reshape(n_cb, NA_QB)
    kb_start = np.clip(np.arange(n_cb) * NA_QB - NA_KW // 2, 0, GRID_W - NA_KB)
    key_cols = kb_start[:, None] + np.arange(NA_KB)
    win_start = np.clip(q_cols - NA_KW // 2, 0, GRID_W - NA_KW)
    kcol = key_cols[:, None, :]
    col_in = (kcol >= win_start[..., None]) & (kcol < win_start[..., None] + NA_KW)
    col_bias_idx = np.clip(kcol - q_cols[:, :, None] + NA_KW - 1, 0, 2 * NA_KW - 2)
    mask = np.broadcast_to(col_in[:, :, None, :], (n_cb, NA_QB, kh, NA_KB)).reshape(n_cb, NA_QB, kh * NA_KB)
    n_lat = kh * NA_KB

    def row_block(r):
        rs = jnp.clip(r - kh // 2, 0, rows - kh)
        k_rows = lax.dynamic_slice_in_dim(k_grid, rs, kh, axis=1)
        v_rows = lax.dynamic_slice_in_dim(v_grid, rs, kh, axis=1)
        k_blk = k_rows[:, :, key_cols].transpose(0, 2, 1, 3, 4, 5).reshape(b, n_cb, n_lat, NA_HEADS, NA_HEAD_DIM)
        v_blk = v_rows[:, :, key_cols].transpose(0, 2, 1, 3, 4, 5).reshape(b, n_cb, n_lat, NA_HEADS, NA_HEAD_DIM)
        q_blk = lax.dynamic_index_in_dim(q_grid, r, axis=1, keepdims=False).reshape(b, n_cb, NA_QB, NA_HEADS, NA_HEAD_DIM)
        row_idx = rs + jnp.arange(kh) - r + (NA_KH - 1)
        bias = rpb[:, row_idx][:, :, col_bias_idx]
        bias = bias.transpose(0, 2, 3, 1, 4).reshape(NA_HEADS, n_cb, NA_QB, n_lat).astype(jnp.float32)
        s_lat = jnp.einsum('bjqhd,bjkhd->bhjqk', q_blk, k_blk).astype(jnp.float32) + bias
        s_lat = jnp.where(mask, s_lat, -jnp.inf)
        s_ctx = jnp.einsum('bjqhd,bkhd->bhjqk', q_blk, kc).astype(jnp.float32)
        p = jax.nn.softmax(jnp.concatenate([s_lat, s_ctx], axis=-1), axis=-1).astype(v.dtype)
        o = (jnp.einsum('bhjqk,bjkhd->bjqhd', p[..., :n_lat], v_blk)
             + jnp.einsum('bhjqk,bkhd->bjqhd', p[..., n_lat:], vc))
        return o.reshape(b, GRID_W, d_model)

    o_rows = lax.map(row_block, jnp.arange(rows))
    o_lat = o_rows.transpose(1, 0, 2, 3).reshape(b, t, d_model) @ w_out
    o_ctx = None
    if with_ctx_queries:
        s = jnp.einsum('bqhd,bkhd->bhqk', qc, kc).astype(jnp.float32)
        p = jax.nn.softmax(s, axis=-1).astype(vc.dtype)
        o_ctx = jnp.einsum('bhqk,bkhd->bqhd', p, vc).reshape(a_ctx.shape) @ w_out
    return o_ctx, o_lat


def _moe_swiglu(h, router_w, router_b, w1, w3, w2):
    logits = (h @ router_w).astype(jnp.float32) + router_b.astype(jnp.float32)
    top_vals, top_idx = lax.top_k(logits, TOP_K)
    top_w = jax.nn.softmax(top_vals, axis=-1)
    gates = jnp.sum(jax.nn.one_hot(top_idx, N_EXPERTS, dtype=jnp.float32) * top_w[..., None], axis=-2).astype(h.dtype)
    out = jnp.zeros_like(h)
    for e in range(N_EXPERTS):
        out = out + gates[..., e:e + 1] * _swiglu(h, w1[e], w3[e], w2[e])
    return out


def setup_inputs(seed: int = 0) -> dict:
    key = jax.random.key(seed)
    keys = iter(jax.random.split(key, 64))
    d = D_MODEL
    ne, no = (DEPTH + 1) // 2, DEPTH // 2

    def nrm(shape, scale):
        return scale * jax.random.normal(next(keys), shape, jnp.float32)

    def uni(shape, lo, hi):
        return jax.random.uniform(next(keys), shape, jnp.float32, lo, hi)

    dt0 = jnp.exp(uni((ne, 2, MB_HEADS), math.log(1e-3), math.log(1e-1)))
    return {
        'x': nrm((BATCH, SEQ, d), 1.0),
        'c': nrm((BATCH, d), 1.0),
        'ctx': nrm((BATCH, CTX_LEN, d), 1.0),
        'c_ctx': nrm((d,), 1.0),
        'w_ada': nrm((DEPTH, d, 6 * d), 0.5 * d ** -0.5),
        'b_ada': nrm((DEPTH, 6 * d), 0.02),
        'norm_mix': 1.0 + nrm((DEPTH, d), 0.05),
        'norm_ffn': 1.0 + nrm((DEPTH, d), 0.05),
        'norm_final': 1.0 + nrm((d,), 0.05),
        'mix_w_in': nrm((ne, d, MIX_IN), d ** -0.5),
        'mix_w_out': nrm((ne, MIX_OUT, d), MIX_OUT ** -0.5),
        'rw_mu': uni((ne, 2, RW_COLS), 0.0, 0.5),
        'rw_w0': uni((ne, 2, RW_DIM), -6.0, 1.0),
        'rw_w_up': nrm((ne, 2, RW_LORA_W, RW_DIM), 0.5 * RW_LORA_W ** -0.5),
        'rw_a0': nrm((ne, 2, RW_DIM), 0.5),
        'rw_a_up': nrm((ne, 2, RW_LORA_A, RW_DIM), 0.5 * RW_LORA_A ** -0.5),
        'rw_g_up': nrm((ne, RW_LORA_G, RW_DIM), RW_LORA_G ** -0.5),
        'rw_k_k': 0.85 + nrm((ne, RW_DIM), 0.05),
        'rw_k_a': 1.0 + nrm((ne, RW_DIM), 0.05),
        'rw_r_k': nrm((ne, 2, RW_HEADS, RW_HEAD_DIM), 0.1),
        'rw_gn_w': 1.0 + nrm((ne, RW_DIM), 0.05),
        'rw_gn_b': nrm((ne, RW_DIM), 0.02),
        'mb_conv_w': nrm((ne, MB_CONV, MB_XBC), MB_CONV ** -0.5),
        'mb_conv_b': nrm((ne, MB_XBC), 0.02),
        'mb_dt_bias': dt0 + jnp.log(-jnp.expm1(-dt0)),
        'mb_a_log': jnp.log(uni((ne, 2, MB_HEADS), 1.0, 16.0)),
        'mb_d': 1.0 + nrm((ne, MB_HEADS), 0.1),
        'mb_norm_w': 1.0 + nrm((ne, MB_DIM), 0.05),
        'ffn_w1': nrm((ne, d, D_FF), d ** -0.5),
        'ffn_w3': nrm((ne, d, D_FF), d ** -0.5),
        'ffn_w2': nrm((ne, D_FF, d), D_FF ** -0.5),
        'na_w_qkv': nrm((no, d, 3 * d), d ** -0.5),
        'na_w_out': nrm((no, d, d), d ** -0.5),
        'na_rpb': nrm((no, NA_HEADS, 2 * NA_KH - 1, 2 * NA_KW - 1), 0.1),
        'moe_router_w': nrm((no, d, N_EXPERTS), d ** -0.5),
        'moe_router_b': nrm((no, N_EXPERTS), 0.01),
        'moe_w1': nrm((no, N_EXPERTS, d, D_FF), d ** -0.5),
        'moe_w3': nrm((no, N_EXPERTS, d, D_FF), d ** -0.5),
        'moe_w2': nrm((no, N_EXPERTS, D_FF, d), D_FF ** -0.5),
    }


def reference(x, c, ctx, c_ctx, w_ada, b_ada, norm_mix, norm_ffn, norm_final, mix_w_in, mix_w_out,
              rw_mu, rw_w0, rw_w_up, rw_a0, rw_a_up, rw_g_up, rw_k_k, rw_k_a, rw_r_k, rw_gn_w, rw_gn_b,
              mb_conv_w, mb_conv_b, mb_dt_bias, mb_a_log, mb_d, mb_norm_w, ffn_w1, ffn_w3, ffn_w2,
              na_w_qkv, na_w_out, na_rpb, moe_router_w, moe_router_b, moe_w1, moe_w3, moe_w2):
    h_ctx, h_lat = ctx, x
    c_lat_act, c_ctx_act = jax.nn.silu(c), jax.nn.silu(c_ctx)
    for i in range(DEPTH):
        j = i // 2
        update_ctx = i < DEPTH - 1
        m_lat = jnp.split((c_lat_act @ w_ada[i] + b_ada[i])[:, None, :], 6, axis=-1)
        m_ctx = jnp.split(c_ctx_act @ w_ada[i] + b_ada[i], 6, axis=-1)
        a_lat = _modulate(_rms_norm(h_lat, norm_mix[i]), m_lat[0], m_lat[1])
        a_ctx = _modulate(_rms_norm(h_ctx, norm_mix[i]), m_ctx[0], m_ctx[1])
        if i % 2 == 0:
            o_ctx, o_lat = _rwkv_mamba_mixer(a_ctx, a_lat, mix_w_in[j], mix_w_out[j], rw_mu[j], rw_w0[j],
                                             rw_w_up[j], rw_a0[j], rw_a_up[j], rw_g_up[j], rw_k_k[j],
                                             rw_k_a[j], rw_r_k[j], rw_gn_w[j], rw_gn_b[j], mb_conv_w[j],
                                             mb_conv_b[j], mb_dt_bias[j], mb_a_log[j], mb_d[j], mb_norm_w[j])
        else:
            o_ctx, o_lat = _neighbourhood_attention(a_ctx, a_lat, na_w_qkv[j], na_w_out[j], na_rpb[j], update_ctx)

        def channel_mixer(t):
            if i % 2 == 0:
                return _swiglu(t, ffn_w1[j], ffn_w3[j], ffn_w2[j])
            return _moe_swiglu(t, moe_router_w[j], moe_router_b[j], moe_w1[j], moe_w3[j], moe_w2[j])

        h_lat = h_lat + m_lat[2] * o_lat
        h_lat = h_lat + m_lat[5] * channel_mixer(_modulate(_rms_norm(h_lat, norm_ffn[i]), m_lat[3], m_lat[4]))
        if update_ctx:
            h_ctx = h_ctx + m_ctx[2] * o_ctx
            h_ctx = h_ctx + m_ctx[5] * channel_mixer(_modulate(_rms_norm(h_ctx, norm_ffn[i]), m_ctx[3], m_ctx[4]))
    return _rms_norm(h_lat, norm_final)
```

```python
from contextlib import ExitStack
import os
import numpy as np
import concourse.bass as bass
import concourse.mybir as mybir
from concourse.bass_utils import run_bass_kernel_spmd

F32 = mybir.dt.float32
BF16 = mybir.dt.bfloat16
AF = mybir.ActivationFunctionType
ALU = mybir.AluOpType
AX = mybir.AxisListType

D = 1024
TC, TL = 256, 2048
T = TC + TL
NORM_EPS = 1e-6
MIX_IN = 3312
RW_COLS = 1760
D_FF = 2816
NE = 8

ENGS = ("pe", "act", "dve", "pool", "sp")
N_DSEM = 24


class Tok:
    __slots__ = ("name", "w", "r", "psum")

    def __init__(self, name="", psum=False):
        self.name = name
        self.w = None
        self.r = {}
        self.psum = psum


def toks(n, name="", psum=False):
    return [Tok("%s%d" % (name, i), psum) for i in range(n)]


class Sched:
    def __init__(self, nc, stack, self_sync=True):
        self.nc = nc
        self.self_sync = self_sync
        self.csem = {e: stack.enter_context(nc.semaphore("c_" + e)) for e in ENGS if e != "sp"}
        self.dsem = [stack.enter_context(nc.semaphore("d%d" % i)) for i in range(N_DSEM)]
        self.cnt = {e: 0 for e in ENGS}
        self.dcount = 0
        self.dval = [0] * N_DSEM
        self.waited = {e: {} for e in ENGS}
        self.rec = {e: [] for e in ENGS}
        self.n_ops = 0
        self.fence = stack.enter_context(nc.sbuf_tensor("fence", [128, 80], F32))
        self.t_fconst = Tok("fconst")
        self.t_floc = {"dve": toks(32, "fd"), "act": toks(32, "fa")}
        self.nfence = {"dve": 0, "act": 0}

    def _fence(self, eng):
        j = self.nfence[eng] % 32
        self.nfence[eng] += 1
        f = self.fence
        if eng == "dve":
            self.op("dve", lambda e: e.memset(f[0:1, j:j + 1], 0.0), r=(), w=[self.t_floc["dve"][j]])
        else:
            self.op("act", lambda e: e.activation(out=f[0:1, 32 + j:33 + j], in_=f[0:1, 72:73], func=AF.Copy),
                    r=[self.t_fconst], w=[self.t_floc["act"][j]])
        return self.cnt[eng]

    def _need(self, eng, dep, waits):
        if dep is None:
            return
        if dep[0] == "c":
            _, e2, idx = dep
            if e2 == eng and (eng == "pe" or not self.self_sync):
                return
            key = ("c", e2)
            val = idx
        else:
            _, slot, val = dep
            key = ("d", slot)
        if self.waited[eng].get(key, 0) >= val:
            return
        self.waited[eng][key] = val
        waits.append((key, val))

    def _deps(self, eng, r, w):
        waits = []
        for t in r:
            self._need(eng, t.w, waits)
            if t.psum:
                for k, v in t.r.items():
                    if not isinstance(k, tuple) and k != eng:
                        self._need(eng, ("c", k, v), waits)
        for t in w:
            self._need(eng, t.w, waits)
            for k, v in t.r.items():
                if isinstance(k, tuple):
                    self._need(eng, ("d", k[1], v), waits)
                else:
                    self._need(eng, ("c", k, v), waits)
        return waits

    def op(self, eng, fn, r=(), w=()):
        waits = self._deps(eng, r, w)
        self.cnt[eng] += 1
        idx = self.cnt[eng]
        for t in r:
            t.r[eng] = idx
        for t in w:
            t.w = ("c", eng, idx)
            t.r = {}
        self.rec[eng].append((waits, fn, ("c", eng)))
        self.n_ops += 1
        if eng in ("dve", "act") and any(t.psum for t in r):
            fidx = self._fence(eng)
            for t in r:
                if t.psum:
                    t.r[eng] = fidx

    def dma(self, eng, out, in_, r=(), w=(), **kw):
        slot = self.dcount % N_DSEM
        self.dcount += 1
        waits = self._deps(eng, r, w)
        if self.dval[slot] > 0:
            self._need(eng, ("d", slot, self.dval[slot]), waits)
        self.dval[slot] += 16
        val = self.dval[slot]
        for t in r:
            t.r[("d", slot)] = val
        for t in w:
            t.w = ("d", slot, val)
            t.r = {}
        fn = (lambda e, out=out, in_=in_, kw=kw: e.dma_start(out=out, in_=in_, **kw))
        self.rec[eng].append((waits, fn, ("d", slot)))
        self.n_ops += 1

    def barrier(self):
        for eng in ENGS:
            waits = []
            for e2 in ENGS:
                if e2 == "sp" or self.cnt[e2] == 0:
                    continue
                self._need(eng, ("c", e2, self.cnt[e2]), waits)
            for slot in range(N_DSEM):
                if self.dval[slot] > 0:
                    self._need(eng, ("d", slot, self.dval[slot]), waits)
            if waits:
                self.rec[eng].append((waits, None, None))

    def _sem(self, key):
        return self.csem[key[1]] if key[0] == "c" else self.dsem[key[1]]

    def flush(self):
        nc = self.nc
        rec = self.rec
        self.rec = {e: [] for e in ENGS}

        def replay(eng_name):
            def body(e):
                for waits, fn, sig in rec[eng_name]:
                    for key, val in waits:
                        e.wait_ge(self._sem(key), val)
                    if fn is None:
                        continue
                    ins = fn(e)
                    if sig[0] == "c":
                        ins.then_inc(self.csem[sig[1]], 1)
                    else:
                        ins.then_inc(self.dsem[sig[1]], 16)
            return body

        with nc.Block() as block:
            if rec["pe"]:
                block.tensor(replay("pe"))
            if rec["act"]:
                block.scalar(replay("act"))
            if rec["dve"]:
                block.vector(replay("dve"))
            if rec["pool"]:
                block.gpsimd(replay("pool"))
            if rec["sp"]:
                block.sync(replay("sp"))

    def mm(self, out, lhsT, rhs, start, stop, r=(), w=()):
        self.op("pe", lambda e: e.matmul(out, lhsT=lhsT, rhs=rhs, start=start, stop=stop), r, w)

    def tr(self, out, in_, ident, r=(), w=()):
        self.op("pe", lambda e: e.transpose(out, in_, ident), r, w)

    def act(self, out, in_, func, bias=None, scale=None, accum_out=None, r=(), w=()):
        kw = {}
        if bias is not None:
            kw["bias"] = bias
        if scale is not None:
            kw["scale"] = scale
        if accum_out is not None:
            kw["accum_out"] = accum_out
        self.op("act", lambda e: e.activation(out=out, in_=in_, func=func, **kw), r, w)

    def ts(self, eng, out, in0, s1, s2, op0, op1=None, accum_out=None, r=(), w=()):
        if eng == "act_":
            return self.act(out, in0, AF.Copy, scale=s1, r=r, w=w)
        kw = {}
        if op1 is not None:
            kw["op1"] = op1
        if accum_out is not None:
            kw["accum_out"] = accum_out
        self.op(eng, lambda e: e.tensor_scalar(out=out, in0=in0, scalar1=s1, scalar2=s2, op0=op0, **kw), r, w)

    def tt(self, eng, out, in0, in1, op, r=(), w=()):
        self.op(eng, lambda e: e.tensor_tensor(out=out, in0=in0, in1=in1, op=op), r, w)

    def stt(self, out, in0, scalar, in1, op0, op1, r=(), w=()):
        self.op("dve", lambda e: e.scalar_tensor_tensor(out=out, in0=in0, scalar=scalar, in1=in1, op0=op0, op1=op1), r, w)

    def copy(self, eng, out, in_, r=(), w=()):
        if eng == "act":
            self.op("act", lambda e: e.copy(out=out, in_=in_), r, w)
        else:
            self.op(eng, lambda e: e.tensor_copy(out=out, in_=in_), r, w)

    def recip(self, out, in_, r=(), w=()):
        self.op("dve", lambda e: e.reciprocal(out=out, in_=in_), r, w)

    def reduce(self, out, in_, op, axis=None, r=(), w=()):
        self.op("dve", lambda e: e.tensor_reduce(out=out, in_=in_, axis=axis or AX.X, op=op), r, w)

    def memset(self, eng, ap, val, r=(), w=()):
        self.op(eng, lambda e: e.memset(ap, val), r, w)


class Ctx:
    pass


def build_program(dbg=(), upto=99):
    nc = bass.Bass("TRN2", target_bir_lowering=False)
    g = Ctx()
    g.nc = nc
    g.dbg = set(dbg)

    def din(name, shape):
        return nc.dram_tensor(name, list(shape), F32, kind="ExternalInput").ap()

    def dscr(name, shape, dtype=F32):
        if name in g.dbg:
            return nc.dram_tensor(name, list(shape), dtype, kind="ExternalOutput").ap()
        return nc.dram_tensor(name, list(shape), dtype).ap()

    specs = dict(
        x=(TL, D), c=(D,), ctx=(TC, D), c_ctx=(D,), w_ada=(2, D, 6 * D), b_ada=(2, 6 * D),
        norm_mix=(2, D), norm_ffn=(2, D), norm_final=(D,), mix_w_in=(D, MIX_IN), mix_w_out=(D, D),
        rw_mu=(2, RW_COLS), rw_w0=(2, 512), rw_w_up=(2, 32, 512), rw_a0=(2, 512), rw_a_up=(2, 32, 512),
        rw_g_up=(96, 512), rw_k_k=(512,), rw_k_a=(512,), rw_r_k=(2, 512), rw_gn_w=(512,), rw_gn_b=(512,),
        mb_conv_w=(5, 1024), mb_conv_b=(1024,), mb_dt_bias=(16,), mb_a_log=(16,), mb_d=(8,), mb_norm_w=(512,),
        ffn_w1=(D, D_FF), ffn_w3=(D, D_FF), ffn_w2=(D_FF, D),
        na_w_qkv=(D, 3 * D), na_w_out=(D, D), na_rpb=(16, 15, 31),
        moe_router_w=(D, NE), moe_router_b=(NE,), moe_w1=(NE, D, D_FF), moe_w3=(NE, D, D_FF), moe_w2=(NE, D_FF, D),
    )

    class LazyIn(dict):
        def __missing__(self, k):
            v = din(k, specs[k])
            self[k] = v
            return v
    I = LazyIn()
    g.I = I
    out = nc.dram_tensor("out", [TL, D], F32, kind="ExternalOutput").ap()
    g.out = out

    g.hT = dscr("hT", (D, T))
    g.zT = dscr("zT", (MIX_IN, T))

    with ExitStack() as st:
        S = Sched(nc, st)
        g.S = S
        g.ident = st.enter_context(nc.sbuf_tensor("ident", [128, 128], F32))
        g.ones = st.enter_context(nc.sbuf_tensor("ones", [128, 128], F32))
        g.modT = st.enter_context(nc.sbuf_tensor("modT", [128, 2, 2, 48], F32))
        g.Gm = st.enter_context(nc.sbuf_tensor("Gm", [128, 2, 2, 8], F32))
        g.Gf = st.enter_context(nc.sbuf_tensor("Gf", [128, 2, 2, 8], F32))
        g.nfin = st.enter_context(nc.sbuf_tensor("nfin", [128, 8], F32))
        g.t_const = Tok("const")
        g.t_mod = Tok("mod")

        S.memset("pool", g.ident[:], 0.0, w=[g.t_const])
        S.op("pool", lambda e: e.affine_select(out=g.ident[:], in_=g.ident[:], compare_op=ALU.not_equal, fill=1.0,
                                                base=0, pattern=[[-1, 128]], channel_multiplier=1),
             r=[g.t_const], w=[g.t_const])
        S.memset("pool", g.ones[:], 1.0, w=[g.t_const])
        S.memset("pool", S.fence[:], 0.0, w=[S.t_fconst])

        phase_adaln(g)
        S.barrier(); S.flush()
        if upto >= 1:
            phase_l0_pre(g)
        if upto >= 2:
            alloc_rwkv_scratch(g)
            phase_rwkv_lora(g)
        if upto >= 3:
            phase_rwkv_prep(g)
        if upto >= 4 and not os.environ.get("SKIP_RWSCAN"):
            phase_rwkv_scan(g)
        if upto >= 5:
            alloc_ssd_scratch(g)
            phase_ssd_prep(g)
        if upto >= 6:
            phase_ssd_scan(g)
        if upto >= 7 and not os.environ.get("SKIP_L0"):
            phase_l0_post(g)
        if upto >= 8:
            alloc_l1_scratch(g)
            phase_l1_qkv(g)
        if upto >= 9:
            phase_l1_attn(g)
        if upto >= 10:
            phase_l1_post(g)
        S.barrier(); S.flush()
    g.used_inputs = list(I.keys())
    nc.used_inputs = g.used_inputs
    return nc


def col1(ap1d, p=128):
    return ap1d.rearrange("(k p o) -> p k o", p=p, o=1)


def load_cols(g, st, name, srcs, ps=None, t_p=None):
    nc, S = g.nc, g.S
    R = sum(a.shape[0] for a in srcs)
    assert R <= 128
    stage = st.enter_context(nc.sbuf_tensor(name + "_stg", [128, 128], F32))
    dst = st.enter_context(nc.sbuf_tensor(name, [128, R], F32))
    if ps is None:
        ps = st.enter_context(nc.psum_tensor(name + "_ps", [128, 128], F32))
        t_p = Tok(name + "p", True)
    t_s, t_d = Tok(name + "s"), Tok(name)
    S.memset("pool", stage[:], 0.0, w=[t_s])
    r0 = 0
    for a in srcs:
        r, w = a.shape
        q0 = 0
        while q0 < r:
            rem = r - q0
            n = (rem // 16) * 16 if rem >= 16 else max(x for x in (8, 4, 2, 1) if x <= rem)
            S.dma("sp", stage[r0 + q0:r0 + q0 + n, :w], a[q0:q0 + n, :], w=[t_s])
            q0 += n
        r0 += r
    S.tr(ps[:, :R], stage[:R, :], g.ident[:R, :R], r=[t_s, g.t_const], w=[t_p])
    S.copy("dve", dst[:], ps[:, :R], r=[t_p], w=[t_d])
    return dst, t_d


def phase_adaln(g):
    nc, S, I = g.nc, g.S, g.I
    with ExitStack() as st:
        cT = st.enter_context(nc.sbuf_tensor("cT", [128, 8, 2], F32))
        wb = [st.enter_context(nc.sbuf_tensor("wada%d" % i, [128, 8, 1024], F32)) for i in range(2)]
        ps = st.enter_context(nc.psum_tensor("ps_ada", [128, 2, 48, 2], F32))
        t_c, t_ps = Tok("c"), Tok("ps", True)
        t_w = toks(2, "wada")
        v2 = lambda a: a.rearrange("(j p) -> j p", p=128)
        colA, t_b = load_cols(g, st, "colA", [v2(I["c"]), v2(I["c_ctx"]), I["norm_mix"].rearrange("i (j p) -> (i j) p", p=128),
                                              I["norm_ffn"].rearrange("i (j p) -> (i j) p", p=128), v2(I["norm_final"])])
        bada, t_b2 = load_cols(g, st, "colB", [I["b_ada"].rearrange("i (j p) -> (i j) p", p=128)])
        nm = colA[:, 16:32].rearrange("p (i j) -> p i j", i=2)
        nf = colA[:, 32:48].rearrange("p (i j) -> p i j", i=2)
        badav = bada[:].rearrange("p (i j) -> p i j", i=2)
        S.act(cT[:, :, 0], colA[:, 0:8], AF.Silu, r=[t_b], w=[t_c])
        S.act(cT[:, :, 1], colA[:, 8:16], AF.Silu, r=[t_b], w=[t_c])
        n = 0
        for i in range(2):
            wv = I["w_ada"][i].rearrange("(k p) n -> p k n", p=128)
            for blk in range(6):
                buf = wb[n % 2]; tw = t_w[n % 2]; n += 1
                S.dma("sp" if n % 2 else "act", buf[:], wv[:, :, blk * 1024:(blk + 1) * 1024], w=[tw])
                for jj in range(8):
                    j = blk * 8 + jj
                    for k in range(8):
                        S.mm(ps[:, i, j, :], buf[:, k, jj * 128:(jj + 1) * 128], cT[:, k, :], k == 0, k == 7,
                             r=[tw, t_c], w=[t_ps])
        for i in range(2):
            for w_ in range(2):
                S.tt("dve", g.modT[:, i, w_, :], ps[:, i, :, w_], badav[:, i, :], ALU.add, r=[t_ps, t_b2], w=[g.t_mod])
        for i in range(2):
            for w_ in range(2):
                S.stt(g.Gm[:, i, w_, :], g.modT[:, i, w_, 8:16], 1.0, nm[:, i, :], ALU.add, ALU.mult, r=[g.t_mod, t_b], w=[g.t_mod])
                S.stt(g.Gf[:, i, w_, :], g.modT[:, i, w_, 32:40], 1.0, nf[:, i, :], ALU.add, ALU.mult, r=[g.t_mod, t_b], w=[g.t_mod])
        S.copy("dve", g.nfin[:], colA[:, 48:56], r=[t_b], w=[g.t_mod])
        if "modT" in g.dbg:
            dd = nc.dram_tensor("modT_o", [128, 2 * 2 * 48], F32, kind="ExternalOutput").ap()
            S.dma("sp", dd[:, :], g.modT[:].rearrange("p a b c -> p (a b c)"), r=[g.t_mod])
        S.barrier(); S.flush()


def norm_mod(g, hT, n, Gsc, shift, pools, r_h, out_bf, t_out, out_f32=None):
    S = g.S
    sq, rstd, ps, tmp = pools["sq"], pools["rstd"], pools["ps"], pools["tmp"]
    t_sq, t_rstd, t_ps, t_tmp = pools["t_sq"], pools["t_rstd"], pools["t_ps"], pools["t_tmp"]
    S.act(sq[:, :, :n], hT[:, :, :n], AF.Square, r=[r_h], w=[t_sq])
    for c in range(8):
        S.mm(ps[:, :n], g.ones[:], sq[:, c, :n], c == 0, c == 7, r=[t_sq, g.t_const], w=[t_ps])
    S.ts("dve", rstd[:, :n], ps[:, :n], 1.0 / D, NORM_EPS, ALU.mult, ALU.add, r=[t_ps], w=[t_rstd])
    S.act(rstd[:, :n], rstd[:, :n], AF.Sqrt, r=[t_rstd], w=[t_rstd])
    S.op("dve", lambda e: e.reciprocal(out=rstd[:, :n], in_=rstd[:, :n]), r=[t_rstd], w=[t_rstd])
    for c in range(8):
        S.stt(tmp[:, c, :n], hT[:, c, :n], Gsc[:, c:c + 1], rstd[:, :n], ALU.mult, ALU.mult, r=[r_h, t_rstd, g.t_mod], w=[t_tmp])
    for c in range(8):
        S.act(out_bf[:, c, :n], tmp[:, c, :n], AF.Identity, bias=shift[:, c:c + 1], scale=1.0, r=[t_tmp, g.t_mod], w=[t_out])
        if out_f32 is not None:
            S.ts("pool", out_f32[:, c, :n], tmp[:, c, :n], shift[:, c:c + 1], None, ALU.add, r=[t_tmp, g.t_mod], w=[t_out])


def norm_pools(nc, st, tag):
    p = {}
    p["sq"] = st.enter_context(nc.sbuf_tensor("nsq" + tag, [128, 8, 512], F32))
    p["tmp"] = st.enter_context(nc.sbuf_tensor("ntmp" + tag, [128, 8, 512], F32))
    p["rstd"] = st.enter_context(nc.sbuf_tensor("nrstd" + tag, [128, 512], F32))
    p["ps"] = st.enter_context(nc.psum_tensor("nps" + tag, [128, 512], F32))
    for k in ("sq", "tmp", "rstd", "ps"):
        p["t_" + k] = Tok("n" + k, k == "ps")
    return p


class Caster:
    def __init__(self, g, st, name, cols, nbuf=2):
        self.S = g.S
        self.stage = [st.enter_context(g.nc.sbuf_tensor("%s_stg%d" % (name, i), [128, cols], F32)) for i in range(nbuf)]
        self.tok = toks(nbuf, name + "_stg")
        self.n = 0
        self.nbuf = nbuf
        self.engs = ("pool", "dve", "act")

    def load(self, dst, src, w_tok, rows=128, eng=None, dma_eng=None):
        i = self.n % self.nbuf
        n = dst.shape[1]
        e = eng or self.engs[self.n % 2]
        q = dma_eng or ("sp" if self.n % 2 == 0 else "act")
        self.n += 1
        self.S.dma(q, self.stage[i][:rows, :n], src, w=[self.tok[i]])
        self.S.copy(e, dst, self.stage[i][:rows, :n], r=[self.tok[i]], w=[w_tok])


CHUNKS = [(0, 256)] + [(256 + 512 * i, 512) for i in range(4)]


def phase_l0_pre(g):
    nc, S, I = g.nc, g.S, g.I
    with ExitStack() as st:
        win = st.enter_context(nc.sbuf_tensor("win", [128, 8, MIX_IN], BF16))
        xt = [st.enter_context(nc.sbuf_tensor("xt%d" % i, [128, 4, D], F32)) for i in range(2)]
        hT = [st.enter_context(nc.sbuf_tensor("hTs%d" % i, [128, 8, 512], F32)) for i in range(2)]
        aT = [st.enter_context(nc.sbuf_tensor("aT%d" % i, [128, 8, 512], BF16)) for i in range(2)]
        zt = [st.enter_context(nc.sbuf_tensor("zt%d" % i, [128, 512], F32)) for i in range(3)]
        pT = [st.enter_context(nc.psum_tensor("pT%d" % i, [128, 512], F32)) for i in range(2)]
        pz = [st.enter_context(nc.psum_tensor("pz%d" % i, [128, 512], F32)) for i in range(3)]
        npool = norm_pools(nc, st, "a")
        t_win = toks(8, "win"); t_xt = toks(2, "xt"); t_hT = toks(2, "hT"); t_aT = toks(2, "aT")
        t_zt = toks(3, "zt"); t_pT = toks(2, "pT", True); t_pz = toks(3, "pz", True)
        wv = I["mix_w_in"].rearrange("(k p) f -> p k f", p=128)
        cst = Caster(g, st, "winc", MIX_IN)
        for k in range(8):
            cst.load(win[:, k, :], wv[:, k, :], t_win[k])
        hTd = g.hT.rearrange("(c p) t -> p c t", p=128)
        nz = 0; npt = 0
        for ci, (c0, n) in enumerate(CHUNKS):
            nt = n // 128
            b = ci % 2
            src = I["ctx"] if ci == 0 else I["x"][(ci - 1) * 512:ci * 512, :]
            S.dma("sp", xt[b][:, :nt, :], src.rearrange("(t p) d -> p t d", p=128), w=[t_xt[b]])
            for c in range(8):
                pb = npt % 2; npt += 1
                for t in range(nt):
                    S.tr(pT[pb][:, t * 128:(t + 1) * 128], xt[b][:, t, c * 128:(c + 1) * 128], g.ident[:],
                         r=[t_xt[b], g.t_const], w=[t_pT[pb]])
                S.copy("act" if c % 2 else "dve", hT[b][:, c, :n], pT[pb][:, :n], r=[t_pT[pb]], w=[t_hT[b]])
            S.dma("sp", hTd[:, :, c0:c0 + n], hT[b][:, :, :n], r=[t_hT[b]])
            w_ = 1 if ci == 0 else 0
            norm_mod(g, hT[b], n, g.Gm[:, 0, w_, :], g.modT[:, 0, w_, 0:8], npool, t_hT[b], aT[b], t_aT[b])
            for ft in range(26):
                f0 = ft * 128; fs = min(128, MIX_IN - f0)
                zb = nz % 3; nz += 1
                for k in range(8):
                    S.mm(pz[zb][:fs, :n], win[:, k, f0:f0 + fs], aT[b][:, k, :n], k == 0, k == 7,
                         r=[t_win[k], t_aT[b]], w=[t_pz[zb]])
                S.copy("act" if ft % 2 else "dve", zt[zb][:fs, :n], pz[zb][:fs, :n], r=[t_pz[zb]], w=[t_zt[zb]])
                S.dma("sp", g.zT[f0:f0 + fs, c0:c0 + n], zt[zb][:fs, :n], r=[t_zt[zb]])
        S.barrier(); S.flush()


_SQUEEZE0 = ("mix_w_in", "mix_w_out", "rw_mu", "rw_w0", "rw_w_up", "rw_a0", "rw_a_up", "rw_g_up", "rw_k_k", "rw_k_a",
             "rw_gn_w", "rw_gn_b", "mb_conv_w", "mb_conv_b", "mb_d", "mb_norm_w", "ffn_w1", "ffn_w3", "ffn_w2",
             "na_w_qkv", "na_w_out", "na_rpb", "moe_router_w", "moe_router_b", "moe_w1", "moe_w3", "moe_w2")


def make_in_maps(inputs):
    shared = {}
    for k, v in inputs.items():
        if k in ("x", "c", "ctx"):
            continue
        a = np.ascontiguousarray(np.asarray(v, dtype=np.float32))
        if k in _SQUEEZE0:
            a = a[0]
        elif k == "rw_r_k":
            a = a[0].reshape(2, 512)
        elif k in ("mb_dt_bias", "mb_a_log"):
            a = a[0].reshape(16)
        shared[k] = np.ascontiguousarray(a)
    maps = []
    for b in range(8):
        m = dict(shared)
        m["x"] = np.ascontiguousarray(np.asarray(inputs["x"][b], dtype=np.float32))
        m["c"] = np.ascontiguousarray(np.asarray(inputs["c"][b], dtype=np.float32))
        m["ctx"] = np.ascontiguousarray(np.asarray(inputs["ctx"][b], dtype=np.float32))
        maps.append(m)
    return maps


def kernel(**inputs):
    nc = build_program()
    maps = make_in_maps(inputs)
    res = run_bass_kernel_spmd(nc, maps, core_ids=list(range(8)))
    return np.stack([np.asarray(r["out"], dtype=np.float32) for r in res.results], axis=0)


KAPPA = 0.6065306597126334
NCH = T // 64
SEGS = [(0, TC), (TC, T)]


def shift_tile(S, dst, src, c0, mp, mn, rows, r, w):
    S.act(dst[:rows, :], src[:rows, :], AF.Copy, scale=c0, r=r, w=w)
    for a, b in SEGS:
        S.stt(dst[:rows, a + 1:b], src[:rows, a:b - 1], mp, dst[:rows, a + 1:b], ALU.mult, ALU.add, r=r + w, w=w)
        S.stt(dst[:rows, a:b - 1], src[:rows, a + 1:b], mn, dst[:rows, a:b - 1], ALU.mult, ALU.add, r=r + w, w=w)


def alloc_rwkv_scratch(g):
    nc = g.nc
    def dscr(name, shape):
        kind = {"kind": "ExternalOutput"} if name in g.dbg else {}
        return nc.dram_tensor(name, list(shape), F32, **kind).ap()
    g.PR = [[dscr("PR%d_%d" % (d, q), (512, T)) for q in range(6)] for d in range(2)]
    g.vS = dscr("vS", (512, T))
    g.bonusT = dscr("bonusT", (512, T))
    g.gateT = dscr("gateT", (512, T))
    g.WCdd = [dscr("WCd%d" % d, (512, NCH)) for d in range(2)]
    g.sS = [dscr("sS%d" % d, (512, T)) for d in range(2)]
    g.iclrS = [dscr("iclrS%d" % d, (512, T)) for d in range(2)]
    g.catT = dscr("catT", (D, T))


def phase_rwkv_lora(g):
    nc, S, I = g.nc, g.S, g.I
    with ExitStack() as st:
        v2 = lambda a: a.rearrange("(j p) -> j p", p=128)
        mu0, mu1 = I["rw_mu"][0], I["rw_mu"][1]
        cols, t_cols = load_cols(g, st, "lcols", [
            mu0[1536:1664].rearrange("(j p) -> j p", p=128), mu1[1536:1664].rearrange("(j p) -> j p", p=128),
            mu0[1664:1760].rearrange("(j p) -> j p", p=96), mu1[1664:1760].rearrange("(j p) -> j p", p=96),
            I["rw_w0"].rearrange("d (j p) -> (d j) p", p=128), I["rw_a0"].rearrange("d (j p) -> (d j) p", p=128)])
        c0 = st.enter_context(nc.sbuf_tensor("lc0", [128, 2], F32))
        t_c0 = Tok("c0")
        for i in range(2):
            S.tt("dve", c0[:, i:i + 1], cols[:, 2 * i:2 * i + 1], cols[:, 2 * i + 1:2 * i + 2], ALU.add, r=[t_cols], w=[t_c0])
        S.ts("dve", c0[:], c0[:], -1.0, 1.0, ALU.mult, ALU.add, r=[t_c0], w=[t_c0])
        zraw = st.enter_context(nc.sbuf_tensor("lzraw", [128, T], F32))
        z12 = st.enter_context(nc.sbuf_tensor("lz12", [128, T], F32))
        z12t = st.enter_context(nc.sbuf_tensor("lz12t", [128, T], F32))
        zlg = st.enter_context(nc.sbuf_tensor("lzlg", [128, T], F32))
        wupE = st.enter_context(nc.sbuf_tensor("wupE", [128, 2, 512], F32))
        aupE = st.enter_context(nc.sbuf_tensor("aupE", [128, 2, 512], F32))
        gup = st.enter_context(nc.sbuf_tensor("gup", [96, 512], F32))
        ob = [st.enter_context(nc.sbuf_tensor("lob%d" % i, [128, T], F32)) for i in range(2)]
        pl = [st.enter_context(nc.psum_tensor("lps%d" % i, [128, 512], F32)) for i in range(3)]
        t_zr, t_12, t_12t, t_lg, t_wE = Tok("zr"), Tok("z12"), Tok("z12t"), Tok("zlg"), Tok("wE")
        t_ob = toks(2, "lob"); t_pl = toks(3, "lps", True)
        S.memset("pool", wupE[:], 0.0, w=[t_wE])
        S.memset("pool", aupE[:], 0.0, w=[t_wE])
        for d in range(2):
            S.dma("sp", wupE[d * 32:(d + 1) * 32, d, :], I["rw_w_up"][d], w=[t_wE])
            S.dma("sp", aupE[64 + d * 32:64 + (d + 1) * 32, d, :], I["rw_a_up"][d], w=[t_wE])
        S.dma("sp", gup[:], I["rw_g_up"], w=[t_wE])
        S.dma("sp", zraw[:], g.zT[1536:1664, :], w=[t_zr])
        shift_tile(S, z12, zraw, c0[:, 0:1], cols[:, 0:1], cols[:, 1:2], 128, [t_zr, t_cols, t_c0], [t_12])
        S.act(z12t[:], z12[:], AF.Tanh, r=[t_12], w=[t_12t])
        S.dma("sp", zraw[:96, :], g.zT[1664:1760, :], w=[t_zr])
        shift_tile(S, zlg, zraw, c0[:96, 1:2], cols[:96, 2:3], cols[:96, 3:4], 96, [t_zr, t_cols, t_c0], [t_lg])
        S.act(zlg[:96, :], zlg[:96, :], AF.Sigmoid, r=[t_lg], w=[t_lg])
        npl = 0; nob = 0
        jobs = []
        for tl in range(4):
            for d in range(2):
                jobs.append((wupE[:, d, tl * 128:(tl + 1) * 128], z12t, 128, t_12t, cols[:, 4 + d * 4 + tl:5 + d * 4 + tl], g.sS[d], tl))
                jobs.append((aupE[:, d, tl * 128:(tl + 1) * 128], z12, 128, t_12, cols[:, 12 + d * 4 + tl:13 + d * 4 + tl], g.iclrS[d], tl))
            jobs.append((gup[:, tl * 128:(tl + 1) * 128], zlg, 96, t_lg, None, g.gateT, tl))
        for (lhsT, rhs, K, t_rhs, bias, dst, tl) in jobs:
            o = ob[nob % 2]; to = t_ob[nob % 2]; nob += 1
            for (c0_, n) in CHUNKS:
                p = pl[npl % 3]; tp = t_pl[npl % 3]; npl += 1
                S.mm(p[:, :n], lhsT[:K, :] if K < 128 else lhsT, rhs[:K, c0_:c0_ + n], True, True, r=[t_wE, t_rhs], w=[tp])
                if bias is not None:
                    S.act(o[:, c0_:c0_ + n], p[:, :n], AF.Sigmoid, bias=bias, scale=1.0, r=[tp, t_cols], w=[to])
                else:
                    S.copy("dve", o[:, c0_:c0_ + n], p[:, :n], r=[tp], w=[to])
            S.dma("sp", dst[tl * 128:(tl + 1) * 128, :], o[:], r=[to])
        S.barrier(); S.flush()


def phase_rwkv_prep(g):
    nc, S, I = g.nc, g.S, g.I
    with ExitStack() as st:
        mu0, mu1 = I["rw_mu"][0], I["rw_mu"][1]
        r128 = lambda a: a.rearrange("(j p) -> j p", p=128)
        cols, t_cols = load_cols(g, st, "pcols", [
            r128(mu0[0:1536]), r128(mu1[0:1536]), r128(I["rw_k_k"]), r128(I["rw_k_a"]),
            I["rw_r_k"].rearrange("d (j p) -> (d j) p", p=128)])
        c0 = st.enter_context(nc.sbuf_tensor("pc0", [128, 12], F32))
        omka = st.enter_context(nc.sbuf_tensor("pomka", [128, 4], F32))
        t_c0 = Tok("pc0")
        S.tt("dve", c0[:], cols[:, 0:12], cols[:, 12:24], ALU.add, r=[t_cols], w=[t_c0])
        S.ts("dve", c0[:], c0[:], -1.0, 1.0, ALU.mult, ALU.add, r=[t_c0], w=[t_c0])
        S.ts("dve", omka[:], cols[:, 28:32], -1.0, 1.0, ALU.mult, ALU.add, r=[t_cols], w=[t_c0])
        bones = st.enter_context(nc.sbuf_tensor("bones", [128, 128], F32))
        maskC = st.enter_context(nc.sbuf_tensor("maskC", [128, T], F32))
        t_k = Tok("pconst")
        S.memset("pool", bones[:], 0.0, w=[t_k])
        S.memset("pool", bones[0:64, 0:64], 1.0, w=[t_k])
        S.memset("pool", bones[64:128, 64:128], 1.0, w=[t_k])
        S.memset("pool", maskC[:], 1.0, w=[t_k])
        S.memset("pool", maskC[:].rearrange("p (c j) -> p c j", j=64)[:, :, 0:1], 0.0, w=[t_k])
        names = ["zraw", "r", "k", "v", "kk", "bon", "s", "icl", "kdir", "b", "P", "E", "Q", "Qi", "ex0", "ex1", "o0", "o1"]
        tl_ = {n: st.enter_context(nc.sbuf_tensor("p_" + n, [128, T], F32)) for n in names}
        tk = {n: Tok("p_" + n) for n in names}
        tot = st.enter_context(nc.sbuf_tensor("ptot", [128, NCH], F32))
        wc = st.enter_context(nc.sbuf_tensor("pwc", [128, NCH], F32))
        t_tot = Tok("tot")
        pp = [st.enter_context(nc.psum_tensor("pps%d" % i, [128, 512], F32)) for i in range(2)]
        t_pp = toks(2, "pps", True)
        npp = 0
        nout = 0

        def out_tile(dst, compute):
            nonlocal nout
            o = tl_["o%d" % (nout % 2)]; to = tk["o%d" % (nout % 2)]; nout += 1
            compute(o, to)
            S.dma("sp", dst, o[:], r=[to])

        for tl in range(4):
            rows = slice(tl * 128, (tl + 1) * 128)
            for qi, q in enumerate(("r", "k", "v")):
                S.dma("sp", tl_["zraw"][:], g.zT[qi * 512 + tl * 128:qi * 512 + (tl + 1) * 128, :], w=[tk["zraw"]])
                ci = qi * 4 + tl
                shift_tile(S, tl_[q], tl_["zraw"], c0[:, ci:ci + 1], cols[:, ci:ci + 1], cols[:, 12 + ci:13 + ci], 128,
                           [tk["zraw"], t_cols, t_c0], [tk[q]])
            S.dma("sp", g.vS[rows, :], tl_["v"][:], r=[tk["v"]])
            kk = tl_["kk"]; ex0 = tl_["ex0"]; ex1 = tl_["ex1"]
            S.ts("dve", kk[:], tl_["k"][:], cols[:, 24 + tl:25 + tl], None, ALU.mult, r=[tk["k"], t_cols], w=[tk["kk"]])
            S.act(ex0[:], kk[:], AF.Square, r=[tk["kk"]], w=[tk["ex0"]])
            for (c0_, n) in CHUNKS:
                p = pp[npp % 2]; tp = t_pp[npp % 2]; npp += 1
                S.mm(p[:, :n], bones[:], ex0[:, c0_:c0_ + n], True, True, r=[t_k, tk["ex0"]], w=[tp])
                S.act(ex1[:, c0_:c0_ + n], p[:, :n], AF.Sqrt, r=[tp], w=[tk["ex1"]])
            S.ts("dve", ex1[:], ex1[:], 1e-12, None, ALU.max, r=[tk["ex1"]], w=[tk["ex1"]])
            S.op("dve", lambda e, ex1=ex1: e.reciprocal(out=ex1[:], in_=ex1[:]), r=[tk["ex1"]], w=[tk["ex1"]])
            S.tt("dve", kk[:], kk[:], ex1[:], ALU.mult, r=[tk["kk"], tk["ex1"]], w=[tk["kk"]])
            S.memset("pool", tl_["bon"][:], 0.0, w=[tk["bon"]])
            for d in range(2):
                s, icl, kdir, b = tl_["s"], tl_["icl"], tl_["kdir"], tl_["b"]
                P, E, Q, Qi = tl_["P"], tl_["E"], tl_["Q"], tl_["Qi"]
                S.dma("sp", s[:], g.sS[d][rows, :], w=[tk["s"]])
                S.dma("sp", icl[:], g.iclrS[d][rows, :], w=[tk["icl"]])
                S.ts("dve", kdir[:], icl[:], cols[:, 28 + tl:29 + tl], omka[:, tl:tl + 1], ALU.mult, ALU.add,
                     r=[tk["icl"], t_cols, t_c0], w=[tk["kdir"]])
                S.tt("dve", kdir[:], kdir[:], tl_["k"][:], ALU.mult, r=[tk["kdir"], tk["k"]], w=[tk["kdir"]])
                S.tt("pool", b[:], kk[:], icl[:], ALU.mult, r=[tk["kk"], tk["icl"]], w=[tk["b"]])
                S.stt(ex0[:], tl_["r"][:], cols[:, 32 + d * 4 + tl:33 + d * 4 + tl], kdir[:], ALU.mult, ALU.mult,
                      r=[tk["r"], tk["kdir"], t_cols], w=[tk["ex0"]])
                for (c0_, n) in CHUNKS:
                    p = pp[npp % 2]; tp = t_pp[npp % 2]; npp += 1
                    S.mm(p[:, :n], bones[:], ex0[:, c0_:c0_ + n], True, True, r=[t_k, tk["ex0"]], w=[tp])
                    S.tt("dve", tl_["bon"][:, c0_:c0_ + n], tl_["bon"][:, c0_:c0_ + n], p[:, :n], ALU.add, r=[tp, tk["bon"]], w=[tk["bon"]])
                S.op("dve", lambda e, P=P, s=s: e.tensor_tensor_scan(out=P[:], data0=maskC[:], data1=s[:], initial=0.0,
                                                                   op0=ALU.mult, op1=ALU.add), r=[t_k, tk["s"]], w=[tk["P"]])
                S.copy("dve", tot[:], P[:].rearrange("p (c j) -> p c j", j=64)[:, :, 63], r=[tk["P"]], w=[t_tot])
                S.tt("pool", E[:], P[:], s[:], ALU.subtract, r=[tk["P"], tk["s"]], w=[tk["E"]])
                v3 = lambda a: a[:].rearrange("p (c j) -> p c j", j=64)
                S.tt("dve", v3(Q), tot[:].unsqueeze(2).to_broadcast([128, NCH, 64]), v3(P), ALU.subtract, r=[tk["P"], t_tot], w=[tk["Q"]])
                S.tt("pool", Qi[:], Q[:], s[:], ALU.add, r=[tk["Q"], tk["s"]], w=[tk["Qi"]])
                S.act(wc[:], tot[:], AF.Exp, scale=-KAPPA, r=[t_tot], w=[t_tot])
                S.dma("sp", g.WCdd[d][rows, :], wc[:], r=[t_tot])
                lr, la, ln, lc = (P, E, P, Q) if d == 0 else (Qi, Q, Qi, E)
                tn = {id(P): "P", id(E): "E", id(Q): "Q", id(Qi): "Qi"}
                S.act(ex0[:], lr[:], AF.Exp, scale=-KAPPA, r=[tk[tn[id(lr)]]], w=[tk["ex0"]])
                out_tile(g.PR[d][0][rows, :], lambda o, to: S.tt("dve", o[:], tl_["r"][:], ex0[:], ALU.mult, r=[tk["r"], tk["ex0"]], w=[to]))
                S.act(ex1[:], la[:], AF.Exp, scale=-KAPPA, r=[tk[tn[id(la)]]], w=[tk["ex1"]])
                out_tile(g.PR[d][1][rows, :], lambda o, to: S.stt(o[:], kk[:], -1.0, ex1[:], ALU.mult, ALU.mult, r=[tk["kk"], tk["ex1"]], w=[to]))
                S.act(ex0[:], ln[:], AF.Exp, scale=KAPPA, r=[tk[tn[id(ln)]]], w=[tk["ex0"]])
                out_tile(g.PR[d][2][rows, :], lambda o, to: S.tt("dve", o[:], b[:], ex0[:], ALU.mult, r=[tk["b"], tk["ex0"]], w=[to]))
                out_tile(g.PR[d][3][rows, :], lambda o, to: S.tt("pool", o[:], kdir[:], ex0[:], ALU.mult, r=[tk["kdir"], tk["ex0"]], w=[to]))
                S.act(ex1[:], lc[:], AF.Exp, scale=-KAPPA, r=[tk[tn[id(lc)]]], w=[tk["ex1"]])
                out_tile(g.PR[d][4][rows, :], lambda o, to: S.tt("dve", o[:], b[:], ex1[:], ALU.mult, r=[tk["b"], tk["ex1"]], w=[to]))
                out_tile(g.PR[d][5][rows, :], lambda o, to: S.tt("pool", o[:], kdir[:], ex1[:], ALU.mult, r=[tk["kdir"], tk["ex1"]], w=[to]))
            out_tile(g.bonusT[rows, :], lambda o, to: S.tt("dve", o[:], tl_["bon"][:], tl_["v"][:], ALU.mult, r=[tk["bon"], tk["v"]], w=[to]))
        S.barrier(); S.flush()


def rwkv_chunk_order(d):
    if d == 0:
        return list(range(NCH))
    return [3, 2, 1, 0] + list(range(NCH - 1, 3, -1))


def phase_rwkv_scan(g):
    nc, S, I = g.nc, g.S, g.I
    G = int(os.environ.get("RW_G", "3"))
    with ExitStack() as st:
        sb = lambda name, shape, dt=F32: st.enter_context(nc.sbuf_tensor(name, shape, dt))
        id64 = g.ident[0:64, 0:64]
        mask = [sb("rmask%d" % d, [64, 320]) for d in range(2)]
        t_k = Tok("rconst")
        for d in range(2):
            S.memset("pool", mask[d][:], 1.0, w=[t_k])
            for blk, strict in ((0, True), (1, False), (2, True), (3, False)):
                sgn = 1 if d == 0 else -1
                S.op("pool", lambda e, d=d, blk=blk, strict=strict, sgn=sgn: e.affine_select(
                    out=mask[d][:, blk * 64:(blk + 1) * 64], in_=mask[d][:, blk * 64:(blk + 1) * 64],
                    compare_op=ALU.is_ge, fill=0.0, base=-(1 if strict else 0), pattern=[[sgn, 64]], channel_multiplier=-sgn),
                    r=[t_k], w=[t_k])
            sgn = -1 if d == 0 else 1
            S.op("pool", lambda e, d=d, sgn=sgn: e.affine_select(
                out=mask[d][:, 256:320], in_=mask[d][:, 256:320], compare_op=ALU.is_ge, fill=0.0, base=-1,
                pattern=[[sgn, 64]], channel_multiplier=-sgn), r=[t_k], w=[t_k])
        fmraw = [sb("rfm%d" % q, [128, 2 * T]) for q in range(7)]
        fm = [b[0:64, :].rearrange("p (h t) -> p h t", h=2) for b in fmraw]
        t_fm = toks(7, "rfm")
        wc2 = sb("rwc2", [64, 2, NCH]); t_wc = Tok("wc2")
        yacc = sb("ryacc", [64, NCH, 128]); t_y = toks(NCH, "yacc")
        bon = fmraw[0][:, 0:T]; gat = fmraw[1][:, 0:T]; ofm = fmraw[2][:, 0:T]
        slots = []
        for s_ in range(G):
            d_ = {}
            d_["A"] = sb("rA%d" % s_, [64, 2, 320])
            for nm in ("tm", "T0", "T1", "P0", "P1", "Q0", "Q1", "AbT", "X0", "U0", "Rb", "Y0", "GT", "ZT"):
                d_[nm] = sb("r%s%d" % (nm, s_), [64, 2, 64])
            d_["tk"] = {nm: Tok(nm + str(s_), nm.startswith("ps")) for nm in ("A", "tm", "T0", "T1", "P0", "P1", "Q0", "Q1", "AbT", "X0", "U0", "Rb", "Y0", "GT", "ZT", "ps0", "ps1")}
            d_["ps"] = [st.enter_context(nc.psum_tensor("rps%d_%d" % (s_, i), [64, 512], F32)) for i in range(2)]
            slots.append(d_)
        ptr = st.enter_context(nc.psum_tensor("rptr", [128, 512], F32)); t_ptr = Tok("ptr", True)
        gcols, t_gc = load_cols(g, st, "gncols", [I["rw_gn_w"].rearrange("(j p) -> j p", p=128), I["rw_gn_b"].rearrange("(j p) -> j p", p=128)],
                                ps=ptr[:, 0:128], t_p=t_ptr)
        prc = st.enter_context(nc.psum_tensor("rprc", [64, 512], F32)); t_prc = Tok("prc", True)
        ST = [sb("rST%d" % i, [64, 2, 64]) for i in range(2)]; t_ST = toks(2, "ST")

        def chunk_gen(sl, d, c, state):
            tk = sl["tk"]; ps = sl["ps"]
            cs = slice(c * 64, (c + 1) * 64)
            Rt, At, Bt, Kt, Bh, Kh, V = fm
            tR, tA, tB, tK, tBh, tKh, tV = t_fm
            A = sl["A"]
            tmT = {}
            for nm, src, tsrc in (("AtT", At, tA), ("BhT", Bh, tBh), ("KhT", Kh, tKh), ("vT", V, tV)):
                pass
            bufs = {"AtT": sl["tm"], "BhT": sl["P1"], "KhT": sl["Q1"], "vT": sl["T1"]}
            btok = {"AtT": tk["tm"], "BhT": tk["P1"], "KhT": tk["Q1"], "vT": tk["T1"]}
            for i, (nm, src, tsrc) in enumerate((("AtT", At, tA), ("BhT", Bh, tBh), ("KhT", Kh, tKh), ("vT", V, tV))):
                if os.environ.get("RW_SKIP_TR"):
                    break
                for hh in range(2):
                    S.tr(ptr[0:64, i * 128 + hh * 64:i * 128 + (hh + 1) * 64], src[:, hh, cs], id64, r=[tsrc, g.t_const], w=[t_ptr])
            for i, nm in enumerate(("AtT", "BhT", "KhT", "vT")):
                if os.environ.get("RW_SKIP_TR"):
                    break
                S.copy("act" if i % 2 else "dve", bufs[nm][:].rearrange("p a b -> p (a b)"), ptr[0:64, i * 128:(i + 1) * 128], r=[t_ptr], w=[btok[nm]])
            AtT, BhT, KhT, vT = bufs["AtT"], bufs["BhT"], bufs["KhT"], bufs["vT"]
            tAtT, tBhT, tKhT, tvT = btok["AtT"], btok["BhT"], btok["KhT"], btok["vT"]
            for hh in range(2):
                if os.environ.get("RW_SKIP_A"):
                    break
                ph = slice(hh * 64, (hh + 1) * 64)
                S.mm(ps[hh][:, 0:64], Bt[:, hh, cs], At[:, hh, cs], True, True, r=[tB, tA], w=[tk["ps%d" % hh]])
                S.mm(ps[hh][:, 64:128], Bt[:, hh, cs], Rt[:, hh, cs], True, True, r=[tB, tR], w=[tk["ps%d" % hh]])
                S.mm(ps[hh][:, 128:192], Kt[:, hh, cs], At[:, hh, cs], True, True, r=[tK, tA], w=[tk["ps%d" % hh]])
                S.mm(ps[hh][:, 192:256], Kt[:, hh, cs], Rt[:, hh, cs], True, True, r=[tK, tR], w=[tk["ps%d" % hh]])
                S.mm(ps[hh][:, 256:320], At[:, hh, cs], Bt[:, hh, cs], True, True, r=[tB, tA], w=[tk["ps%d" % hh]])
            for hh in range(2):
                S.tt("dve", A[:, hh, :], ps[hh][:, 0:320], mask[d][:], ALU.mult, r=[tk["ps%d" % hh], t_k], w=[tk["A"]])
                if os.environ.get("RW_FENCE"):
                    S.memset("dve", sl["GT"][0:1, 0, 0:1], 0.0, r=[tk["ps%d" % hh]], w=[tk["GT"]])
            yield
            Tc, tTc = sl["T0"], tk["T0"]
            for hh in range(2):
                S.tt("pool", Tc[:, hh, :], A[:, hh, 0:64], id64, ALU.add, r=[tk["A"], g.t_const], w=[tTc])
            Pc = (A, 0); PTc = (A, 256)
            tPc = tk["A"]; tPTc = tk["A"]
            pbuf = [(sl["P0"], tk["P0"]), (sl["Q0"], tk["Q0"])]
            pairs = [((sl["P0"], tk["P0"]), (sl["Q0"], tk["Q0"])), ((sl["AbT"], tk["AbT"]), (sl["X0"], tk["X0"]))]
            Tbufs = [(sl["T0"], tk["T0"]), (sl["U0"], tk["U0"])]
            ti = 0
            for j in range(1, 6):
                (Pn, tPn), (PTn, tPTn) = pairs[j % 2]
                def ap(x, hh):
                    t_, off = x
                    return t_[:, hh, off:off + 64]
                for hh in range(2):
                    if j < 5:
                        S.mm(ps[0][:, hh * 64:(hh + 1) * 64], ap(PTc, hh), ap(Pc, hh), True, True, r=[tPc, tPTc], w=[tk["ps0"]])
                    S.mm(ps[1][:, hh * 64:(hh + 1) * 64], ap(Pc, hh), ap(PTc, hh), True, True, r=[tPc, tPTc], w=[tk["ps1"]])
                if j < 5:
                    S.copy("dve", Pn[:].rearrange("p a b -> p (a b)"), ps[0][:, 0:128], r=[tk["ps0"]], w=[tPn])
                S.copy("act", PTn[:].rearrange("p a b -> p (a b)"), ps[1][:, 0:128], r=[tk["ps1"]], w=[tPTn])
                Pc, PTc, tPc, tPTc = (Pn, 0), (PTn, 0), tPn, tPTn
                yield
                Tcur, tTcur = Tbufs[ti]; Tnxt, tTnxt = Tbufs[1 - ti]
                for hh in range(2):
                    S.mm(ps[0][:, 128 + hh * 64:128 + (hh + 1) * 64], PTn[:, hh, :], Tcur[:, hh, :], True, True, r=[tPTn, tTcur], w=[tk["ps0"]])
                S.tt("dve", Tnxt[:].rearrange("p a b -> p (a b)"), ps[0][:, 128:256], Tcur[:].rearrange("p a b -> p (a b)"), ALU.add,
                     r=[tk["ps0"], tTcur], w=[tTnxt])
                ti = 1 - ti
                yield
            Tinv, tTinv = Tbufs[ti]
            AbT, tAbT = sl["P0"], tk["P0"]
            X0, tX0 = sl["Q0"], tk["Q0"]
            for hh in range(2):
                hc = slice(hh * 64, (hh + 1) * 64)
                S.mm(ps[0][:, 256 + hh * 64:256 + (hh + 1) * 64], Tinv[:, hh, :], AtT[:, hh, :], True, True, r=[tTinv, tAtT], w=[tk["ps0"]])
                S.mm(ps[1][:, 256 + hh * 64:256 + (hh + 1) * 64], A[:, hh, 128:192], vT[:, hh, :], True, True, r=[tk["A"], tvT], w=[tk["ps1"]])
            S.copy("dve", AbT[:].rearrange("p a b -> p (a b)"), ps[0][:, 256:384], r=[tk["ps0"]], w=[tAbT])
            S.copy("act", X0[:].rearrange("p a b -> p (a b)"), ps[1][:, 256:384], r=[tk["ps1"]], w=[tX0])
            for hh in range(2):
                S.mm(ps[0][:, hh * 64:(hh + 1) * 64], A[:, hh, 192:256], vT[:, hh, :], True, True, r=[tk["A"], tvT], w=[tk["ps0"]])
                S.mm(ps[1][:, hh * 64:(hh + 1) * 64], KhT[:, hh, :], vT[:, hh, :], True, True, r=[tKhT, tvT], w=[tk["ps1"]])
            S.copy("dve", sl["Y0"][:].rearrange("p a b -> p (a b)"), ps[0][:, 0:128], r=[tk["ps0"]], w=[tk["Y0"]])
            S.copy("act", sl["ZT"][:].rearrange("p a b -> p (a b)"), ps[1][:, 0:128], r=[tk["ps1"]], w=[tk["ZT"]])
            yield
            U0, tU0 = sl["AbT"], tk["AbT"]
            for hh in range(2):
                ph = slice(hh * 64, (hh + 1) * 64)
                S.mm(ps[0][:, 384 + hh * 64:384 + (hh + 1) * 64], Tinv[:, hh, :], X0[:, hh, :], True, True, r=[tTinv, tX0], w=[tk["ps0"]])
                S.mm(ps[1][:, 384 + hh * 64:384 + (hh + 1) * 64], AbT[:, hh, :], A[:, hh, 64:128], True, True, r=[tAbT, tk["A"]], w=[tk["ps1"]])
            S.copy("dve", U0[:].rearrange("p a b -> p (a b)"), ps[0][:, 384:512], r=[tk["ps0"]], w=[tU0])
            S.tt("dve", sl["Rb"][:], ps[1][:, 384:512].rearrange("p (a b) -> p a b", a=2), Rt[:, :, cs], ALU.add, r=[tk["ps1"], tR], w=[tk["Rb"]])
            yield
            for hh in range(2):
                o0 = hh * 64
                S.mm(ps[0][:, o0:o0 + 64], A[:, hh, 64:128], U0[:, hh, :], True, True, r=[tk["A"], tU0], w=[tk["ps0"]])
                S.mm(ps[1][:, o0:o0 + 64], AbT[:, hh, :], BhT[:, hh, :], True, True, r=[tAbT, tBhT], w=[tk["ps1"]])
                S.mm(ps[1][:, 128 + o0:128 + o0 + 64], BhT[:, hh, :], U0[:, hh, :], True, True, r=[tBhT, tU0], w=[tk["ps1"]])
            S.tt("dve", sl["Y0"][:].rearrange("p a b -> p (a b)"), ps[0][:, 0:128], sl["Y0"][:].rearrange("p a b -> p (a b)"), ALU.add,
                 r=[tk["ps0"], tk["Y0"]], w=[tk["Y0"]])
            for hh in range(2):
                S.stt(sl["GT"][:, hh, :], id64, wc2[:, hh, c:c + 1], ps[1][:, hh * 64:(hh + 1) * 64], ALU.mult, ALU.add,
                      r=[g.t_const, t_wc, tk["ps1"]], w=[tk["GT"]])
            S.tt("dve", sl["ZT"][:].rearrange("p a b -> p (a b)"), ps[1][:, 128:256], sl["ZT"][:].rearrange("p a b -> p (a b)"), ALU.add,
                 r=[tk["ps1"], tk["ZT"]], w=[tk["ZT"]])
            yield
            si = state["i"]
            Sc, tSc = ST[si], t_ST[si]; Sn, tSn = ST[1 - si], t_ST[1 - si]
            for hh in range(2):
                o0 = hh * 64
                S.mm(prc[:, o0:o0 + 64], sl["Rb"][:, hh, :], Sc[:, hh, :], True, True, r=[tk["Rb"], tSc], w=[t_prc])
                S.mm(prc[:, 128 + o0:128 + o0 + 64], sl["GT"][:, hh, :], Sc[:, hh, :], True, True, r=[tk["GT"], tSc], w=[t_prc])
            if d == 0:
                S.tt("dve", yacc[:, c, :], prc[:, 0:128], sl["Y0"][:].rearrange("p a b -> p (a b)"), ALU.add, r=[t_prc, tk["Y0"]], w=[t_y[c]])
            else:
                S.tt("dve", sl["Y0"][:].rearrange("p a b -> p (a b)"), prc[:, 0:128], sl["Y0"][:].rearrange("p a b -> p (a b)"), ALU.add,
                     r=[t_prc, tk["Y0"]], w=[tk["Y0"]])
                S.tt("pool", yacc[:, c, :], yacc[:, c, :], sl["Y0"][:].rearrange("p a b -> p (a b)"), ALU.add, r=[tk["Y0"], t_y[c]], w=[t_y[c]])
            S.tt("dve", Sn[:].rearrange("p a b -> p (a b)"), prc[:, 128:256], sl["ZT"][:].rearrange("p a b -> p (a b)"), ALU.add,
                 r=[t_prc, tk["ZT"]], w=[tSn])
            state["i"] = 1 - si
            yield

        DBG_NTL = int(os.environ.get("RW_TL", "4")); DBG_ND = int(os.environ.get("RW_D", "2"))
        DBG_NCH = int(os.environ.get("RW_NCH", "99")); DBG_STG = int(os.environ.get("RW_STG", "99"))
        for tl in range(DBG_NTL):
            rows = slice(tl * 128, (tl + 1) * 128)
            for d in range(DBG_ND):
                for q in range(6):
                    S.dma("sp" if q % 2 else "act", fm[q], g.PR[d][q][rows, :].rearrange("(h k) t -> k h t", h=2), w=[t_fm[q]])
                S.dma("sp", fm[6], g.vS[rows, :].rearrange("(h k) t -> k h t", h=2), w=[t_fm[6]])
                S.dma("sp", wc2[:], g.WCdd[d][rows, :].rearrange("(hh k) c -> k hh c", hh=2), w=[t_wc])
                state = {"i": 0}
                S.memset("pool", ST[0][:], 0.0, w=[t_ST[0]])
                order = rwkv_chunk_order(d)[:DBG_NCH]
                if os.environ.get('RW_SAME'):
                    order = [int(x) for x in os.environ['RW_SAME'].split(',')]
                gens = []
                nxt = 0
                slot_free = list(range(G))
                active = []
                while nxt < len(order) or active:
                    if nxt < len(order) and slot_free:
                        s_ = slot_free.pop(0)
                        import itertools
                        active.append((itertools.islice(chunk_gen(slots[s_], d, order[nxt], state), DBG_STG), s_))
                        nxt += 1
                    still = []
                    for gen, s_ in active:
                        try:
                            next(gen)
                            still.append((gen, s_))
                        except StopIteration:
                            slot_free.append(s_)
                    active = still
            S.dma("sp", bon, g.bonusT[rows, :], w=[t_fm[0]])
            S.dma("act", gat, g.gateT[rows, :], w=[t_fm[1]])
            stat = sb("rstat%d" % tl, [64, NCH * 2, 4]); t_st = Tok("stat")
            sqb = sb("rsq%d" % tl, [64, 128]); t_sq = Tok("sq")
            y4 = yacc[:].rearrange("p c (h v) -> p (c h) v", h=2)
            for c in range(NCH):
                S.reduce(stat[:, 2 * c:2 * c + 2, 0], y4[:, 2 * c:2 * c + 2, :], ALU.add, r=[t_y[c]], w=[t_st])
                S.act(sqb[:], yacc[:, c, :], AF.Square, r=[t_y[c]], w=[t_sq])
                S.reduce(stat[:, 2 * c:2 * c + 2, 1], sqb[:].rearrange("p (h v) -> p h v", h=2), ALU.add, r=[t_sq], w=[t_st])
            S.ts("dve", stat[:, :, 0], stat[:, :, 0], 1.0 / 64, None, ALU.mult, r=[t_st], w=[t_st])
            S.tt("dve", stat[:, :, 2], stat[:, :, 0], stat[:, :, 0], ALU.mult, r=[t_st], w=[t_st])
            S.stt(stat[:, :, 3], stat[:, :, 1], 1.0 / 64, stat[:, :, 2], ALU.mult, ALU.subtract, r=[t_st], w=[t_st])
            S.ts("dve", stat[:, :, 3], stat[:, :, 3], 64e-5, None, ALU.add, r=[t_st], w=[t_st])
            S.act(stat[:, :, 3], stat[:, :, 3], AF.Sqrt, r=[t_st], w=[t_st])
            S.recip(stat[:, :, 3], stat[:, :, 3], r=[t_st], w=[t_st])
            for c in range(NCH):
                for hh in range(2):
                    j = 2 * c + hh
                    S.ts("dve" if hh else "pool", yacc[:, c, hh * 64:(hh + 1) * 64], yacc[:, c, hh * 64:(hh + 1) * 64],
                         stat[:, j, 0:1], stat[:, j, 3:4], ALU.subtract, ALU.mult, r=[t_y[c], t_st], w=[t_y[c]])
            for c8 in range(0, NCH, 8):
                nn = min(8, NCH - c8)
                for c in range(c8, c8 + nn):
                    S.tr(ptr[:, (c - c8) * 64:(c - c8 + 1) * 64], yacc[:, c, :], id64, r=[t_y[c], g.t_const], w=[t_ptr])
                cs8 = slice(c8 * 64, (c8 + nn) * 64)
                S.ts("dve", ofm[:, cs8], ptr[:, 0:nn * 64], gcols[:, tl:tl + 1], gcols[:, 4 + tl:5 + tl], ALU.mult, ALU.add,
                     r=[t_ptr, t_gc], w=[t_fm[2]])
            S.tt("pool", ofm, ofm, bon, ALU.add, r=[t_fm[2], t_fm[0]], w=[t_fm[2]])
            S.tt("dve", ofm, ofm, gat, ALU.mult, r=[t_fm[2], t_fm[1]], w=[t_fm[2]])
            S.dma("sp", g.catT[rows, :], ofm, r=[t_fm[2]])
            if "yrw_o" in g.dbg:
                pass
        S.barrier(); S.flush()


MB0 = RW_COLS
NC2 = T // 128
NEG = -30000.0


def ssd_chunk_order(d):
    if d == 0:
        return list(range(NC2))
    return [1, 0] + list(range(NC2 - 1, 1, -1))


def alloc_ssd_scratch(g):
    nc = g.nc
    def dscr(name, shape):
        kind = {"kind": "ExternalOutput"} if name in g.dbg else {}
        return nc.dram_tensor(name, list(shape), F32, **kind).ap()
    g.xbcS = dscr("xbcS", (1024, T))
    g.ssd16 = dscr("ssd16", (4, 16, T))


def phase_ssd_prep(g):
    nc, S, I = g.nc, g.S, g.I
    with ExitStack() as st:
        sb = lambda name, shape, dt=F32: st.enter_context(nc.sbuf_tensor(name, shape, dt))
        cols, t_cols = load_cols(g, st, "ccols", [I["mb_conv_w"].rearrange("j (t p) -> (j t) p", p=128),
                                                  I["mb_conv_b"].rearrange("(t p) -> t p", p=128)])
        c16, t_c16 = load_cols(g, st, "c16", [I["mb_dt_bias"].rearrange("(o p) -> o p", o=1), I["mb_a_log"].rearrange("(o p) -> o p", o=1)])
        zin = [sb("szin%d" % i, [128, T]) for i in range(2)]; t_zin = toks(2, "szin")
        acc = [sb("sacc%d" % i, [128, T]) for i in range(2)]; t_acc = toks(2, "sacc")
        for tl in range(8):
            b = tl % 2
            S.dma("sp" if b else "act", zin[b][:], g.zT[MB0 + 512 + tl * 128:MB0 + 512 + (tl + 1) * 128, :], w=[t_zin[b]])
            S.act(acc[b][:], zin[b][:], AF.Identity, bias=cols[:, 40 + tl:41 + tl], scale=cols[:, 16 + tl:17 + tl],
                  r=[t_zin[b], t_cols], w=[t_acc[b]])
            for j in (0, 1, 3, 4):
                sh = j - 2
                for a, e_ in SEGS:
                    if sh < 0:
                        o_sl, i_sl = slice(a - sh, e_), slice(a, e_ + sh)
                    else:
                        o_sl, i_sl = slice(a, e_ - sh), slice(a + sh, e_)
                    S.stt(acc[b][:, o_sl], zin[b][:, i_sl], cols[:, j * 8 + tl:j * 8 + tl + 1], acc[b][:, o_sl], ALU.mult, ALU.add,
                          r=[t_zin[b], t_cols, t_acc[b]], w=[t_acc[b]])
            S.act(acc[b][:], acc[b][:], AF.Silu, r=[t_acc[b]], w=[t_acc[b]])
            S.dma("sp", g.xbcS[tl * 128:(tl + 1) * 128, :], acc[b][:], r=[t_acc[b]])
        x16 = sb("sx16", [16, T]); ax = sb("sax", [16, T]); dt16 = sb("sdt16", [16, T]); A16 = sb("sA16", [16, T])
        P16 = sb("sP16", [16, T]); Q16 = sb("sQ16", [16, T]); m128 = sb("sm128", [16, T]); tot = sb("stot", [16, NC2])
        aneg = sb("saneg", [16, 1])
        t16 = Tok("s16")
        S.dma("sp", x16[:], g.zT[MB0 + 1536:MB0 + 1552, :], w=[t16])
        S.act(aneg[:], c16[0:16, 1:2], AF.Exp, r=[t_c16], w=[t16])
        S.ts("dve", aneg[:], aneg[:], -1.0, None, ALU.mult, r=[t16], w=[t16])
        S.ts("dve", x16[:], x16[:], c16[0:16, 0:1], None, ALU.add, r=[t16, t_c16], w=[t16])
        S.ts("dve", ax[:], x16[:], -1.0, None, ALU.mult, r=[t16], w=[t16])
        S.tt("dve", ax[:], ax[:], x16[:], ALU.min, r=[t16], w=[t16])
        S.act(ax[:], ax[:], AF.Exp, r=[t16], w=[t16])
        S.act(ax[:], ax[:], AF.Ln, bias=1.0, scale=1.0, r=[t16], w=[t16])
        S.ts("dve", x16[:], x16[:], 0.0, None, ALU.max, r=[t16], w=[t16])
        S.tt("dve", dt16[:], x16[:], ax[:], ALU.add, r=[t16], w=[t16])
        S.ts("dve", A16[:], dt16[:], aneg[:, 0:1], None, ALU.mult, r=[t16], w=[t16])
        S.memset("pool", m128[:], 1.0, w=[t16])
        S.memset("pool", m128[:].rearrange("p (c j) -> p c j", j=128)[:, :, 0:1], 0.0, w=[t16])
        S.op("dve", lambda e: e.tensor_tensor_scan(out=P16[:], data0=m128[:], data1=A16[:], initial=0.0, op0=ALU.mult, op1=ALU.add),
             r=[t16], w=[t16])
        S.copy("dve", tot[:], P16[:].rearrange("p (c j) -> p c j", j=128)[:, :, 127], r=[t16], w=[t16])
        v3 = lambda a: a[:].rearrange("p (c j) -> p c j", j=128)
        S.tt("dve", v3(Q16), tot[:].unsqueeze(2).to_broadcast([16, NC2, 128]), v3(P16), ALU.subtract, r=[t16], w=[t16])
        S.tt("dve", Q16[:], Q16[:], A16[:], ALU.add, r=[t16], w=[t16])
        S.dma("sp", g.ssd16[0], dt16[:], r=[t16])
        S.dma("sp", g.ssd16[1], P16[:], r=[t16])
        S.dma("sp", g.ssd16[2], Q16[:], r=[t16])
        S.barrier(); S.flush()


def phase_ssd_scan(g):
    nc, S, I = g.nc, g.S, g.I
    with ExitStack() as st:
        sb = lambda name, shape, dt=F32: st.enter_context(nc.sbuf_tensor(name, shape, dt))
        pst = lambda name, shape: st.enter_context(nc.psum_tensor(name, shape, F32))
        x_tok = sb("mxtok", [128, NC2, 512]); t_xt = Tok("xtok")
        yacc = sb("myacc", [128, NC2, 512]); t_y = toks(NC2, "my")
        Bfm = sb("mBfm", [128, 2, T]); Cfm = sb("mCfm", [128, 2, T]); t_bc = Tok("bcfm")
        cum16 = sb("mcum16", [16, T]); t_cum = Tok("cum16")
        colT = sb("mcolT", [128, 3, NC2, 16]); t_colT = Tok("colT")
        Sel = sb("mSel", [16, 16, 128]); Mneg = sb("mMneg", [128, 2, 128]); Dbc = sb("mDbc", [128, 8]); t_k = Tok("mconst")
        psT = pst("mpsT", [128, 512]); t_psT = Tok("mpsT", True)
        psP = [pst("mpsP%d" % i, [128, 512]) for i in range(2)]; t_psP = toks(2, "mpsP", True)
        psG = pst("mpsG", [128, 512]); t_psG = Tok("mpsG", True)
        psY = pst("mpsY", [128, 512]); t_psY = Tok("mpsY", True)
        psS = pst("mpsS", [128, 512]); t_psS = Tok("mpsS", True)
        ncols, t_nc = load_cols(g, st, "mncols", [I["mb_norm_w"].rearrange("(j p) -> j p", p=128)], ps=psT[:, 0:128], t_p=t_psT)
        S.copy("dve", Sel[:], g.ident[0:16, 0:16].unsqueeze(2).to_broadcast([16, 16, 128]), r=[g.t_const], w=[t_k])
        S.memset("pool", Mneg[:], 0.0, w=[t_k])
        S.op("pool", lambda e: e.affine_select(out=Mneg[:, 0, :], in_=Mneg[:, 0, :], compare_op=ALU.is_ge, fill=NEG, base=0,
                                                pattern=[[1, 128]], channel_multiplier=-1), r=[t_k], w=[t_k])
        S.op("pool", lambda e: e.affine_select(out=Mneg[:, 1, :], in_=Mneg[:, 1, :], compare_op=ALU.is_ge, fill=NEG, base=0,
                                                pattern=[[-1, 128]], channel_multiplier=1), r=[t_k], w=[t_k])
        drow = sb("mdrow", [1, 8])
        S.dma("sp", drow[:], I["mb_d"].rearrange("(o h) -> o h", o=1), w=[t_k])
        S.mm(psT[:, 0:8], g.ones[0:1, :], drow[:], True, True, r=[t_k, g.t_const], w=[t_psT])
        S.copy("dve", Dbc[:], psT[:, 0:8], r=[t_psT], w=[t_k])
        S.dma("sp", Bfm[:], g.xbcS[512:768, :].rearrange("(g p) t -> p g t", p=128), w=[t_bc])
        S.dma("act", Cfm[:], g.xbcS[768:1024, :].rearrange("(g p) t -> p g t", p=128), w=[t_bc])
        S.dma("sp", cum16[0:8, :], g.ssd16[1][0:8, :], w=[t_cum])
        S.dma("sp", cum16[8:16, :], g.ssd16[2][8:16, :], w=[t_cum])
        with ExitStack() as st2:
            sb2 = lambda name, shape: st2.enter_context(nc.sbuf_tensor(name, shape, F32))
            xfm = sb2("mxfm", [128, T]); t_xfm = Tok("xfm")
            dt16 = sb2("mdt16", [16, T]); e16 = sb2("me16", [16, T]); t_16 = Tok("m16")
            tot = sb2("mtot", [16, NC2]); dm = sb2("mdm", [16, 2])
            for tl in range(4):
                S.dma("sp", xfm[:], g.xbcS[tl * 128:(tl + 1) * 128, :], w=[t_xfm])
                for c4 in range(0, NC2, 4):
                    nn = min(4, NC2 - c4)
                    for c in range(c4, c4 + nn):
                        S.tr(psT[:, (c - c4) * 128:(c - c4 + 1) * 128], xfm[:, c * 128:(c + 1) * 128], g.ident[:], r=[t_xfm, g.t_const], w=[t_psT])
                    S.copy("act" if (c4 // 4) % 2 else "dve", x_tok[:, c4:c4 + nn, tl * 128:(tl + 1) * 128],
                           psT[:, 0:nn * 128].rearrange("p (c k) -> p c k", k=128), r=[t_psT], w=[t_xt])
            S.dma("sp", dt16[:], g.ssd16[0], w=[t_16])
            S.memset("pool", dm[:], 1.0, w=[t_16])
            S.op("pool", lambda e: e.affine_select(out=dm[:, 0:1], in_=dm[:, 0:1], compare_op=ALU.is_ge, fill=0.0, base=7,
                                                    pattern=[[0, 1]], channel_multiplier=-1), r=[t_16], w=[t_16])
            S.op("pool", lambda e: e.affine_select(out=dm[:, 1:2], in_=dm[:, 1:2], compare_op=ALU.is_ge, fill=0.0, base=-8,
                                                    pattern=[[0, 1]], channel_multiplier=1), r=[t_16], w=[t_16])
            c3 = cum16[:].rearrange("p (c j) -> p c j", j=128)
            S.ts("dve", tot[:], c3[:, :, 127], dm[:, 0:1], None, ALU.mult, r=[t_cum, t_16], w=[t_16])
            S.stt(tot[:], c3[:, :, 0], dm[:, 1:2], tot[:], ALU.mult, ALU.add, r=[t_cum, t_16], w=[t_16])
            S.tt("dve", e16[:].rearrange("p (c j) -> p c j", j=128), tot[:].unsqueeze(2).to_broadcast([16, NC2, 128]), c3, ALU.subtract,
                 r=[t_cum, t_16], w=[t_16])
            S.act(e16[:], e16[:], AF.Exp, r=[t_16], w=[t_16])
            for qi, (src, tsrc) in enumerate(((dt16, t_16), (cum16, t_cum), (e16, t_16))):
                for c in range(NC2):
                    S.tr(psT[:, c * 16:(c + 1) * 16], src[:, c * 128:(c + 1) * 128], g.ident[0:16, 0:16], r=[tsrc, g.t_const], w=[t_psT])
                S.copy("dve", colT[:, qi, :, :], psT[:, 0:NC2 * 16].rearrange("p (c j) -> p c j", j=16), r=[t_psT], w=[t_colT])
            S.barrier(); S.flush()
        E = sb("mE", [128, 8, 128]); eP = sb("meP", [128, 8, 128]); M = sb("mM", [128, 8, 128]); Ct = sb("mCt", [128, 8, 128])
        t_E, t_eP, t_M, t_Ct = Tok("E"), Tok("eP"), Tok("M"), Tok("Ct")
        Xd = [sb("mXd%d" % i, [128, 8, 64]) for i in range(2)]; Xdd = [sb("mXdd%d" % i, [128, 8, 64]) for i in range(2)]
        Gsb = [sb("mG%d" % i, [128, 2, 128]) for i in range(2)]; Btok = [sb("mBt%d" % i, [128, 2, 128]) for i in range(2)]
        t_Xd, t_Xdd, t_G, t_Bt = toks(2, "Xd"), toks(2, "Xdd"), toks(2, "G"), toks(2, "Bt")
        ST = [sb("mST%d" % i, [128, 8, 64]) for i in range(2)]; t_ST = toks(2, "mST")
        tmpS = sb("mtmpS", [128, 8, 64]); t_tmpS = Tok("tmpS")
        n = 0
        for d in range(2):
            si = 0
            S.memset("pool", ST[0][:], 0.0, w=[t_ST[0]])
            hs = slice(d * 8, (d + 1) * 8)
            for c in ssd_chunk_order(d):
                b = n % 2; n += 1
                cs = slice(c * 128, (c + 1) * 128)
                for h in range(8):
                    S.mm(psP[h // 4][:, (h % 4) * 128:(h % 4 + 1) * 128], Sel[:, d * 8 + h, :], cum16[:, cs], True, True,
                         r=[t_k, t_cum], w=[t_psP[h // 4]])
                for g_ in range(2):
                    S.mm(psG[:, g_ * 128:(g_ + 1) * 128], Bfm[:, g_, cs], Cfm[:, g_, cs], True, True, r=[t_bc], w=[t_psG])
                    S.tr(psG[:, 256 + g_ * 128:256 + (g_ + 1) * 128], Bfm[:, g_, cs], g.ident[:], r=[t_bc, g.t_const], w=[t_psG])
                S.copy("act", Gsb[b][:].rearrange("p a b -> p (a b)"), psG[:, 0:256], r=[t_psG], w=[t_G[b]])
                S.copy("act", Btok[b][:].rearrange("p a b -> p (a b)"), psG[:, 256:512], r=[t_psG], w=[t_Bt[b]])
                for hb in range(2):
                    h4 = slice(hb * 4, hb * 4 + 4)
                    pv = psP[hb][:].rearrange("p (h t) -> p h t", t=128)
                    S.tt("dve", E[:, h4, :], pv, Mneg[:, d, :].unsqueeze(1).to_broadcast([128, 4, 128]), ALU.add, r=[t_psP[hb], t_k], w=[t_E])
                    S.act(eP[:, h4, :], pv, AF.Exp, r=[t_psP[hb]], w=[t_eP])
                S.tt("pool", E[:], E[:], colT[:, 1, c, hs].unsqueeze(2).to_broadcast([128, 8, 128]), ALU.subtract, r=[t_E, t_colT], w=[t_E])
                S.act(E[:], E[:], AF.Exp, r=[t_E], w=[t_E])
                for g_ in range(2):
                    h4 = slice(g_ * 4, g_ * 4 + 4)
                    S.tt("dve", M[:, h4, :], E[:, h4, :], Gsb[b][:, g_, :].unsqueeze(1).to_broadcast([128, 4, 128]), ALU.mult, r=[t_E, t_G[b]], w=[t_M])
                    S.tt("pool", Ct[:, h4, :], eP[:, h4, :], Cfm[:, g_, cs].unsqueeze(1).to_broadcast([128, 4, 128]), ALU.mult, r=[t_eP, t_bc], w=[t_Ct])
                xv = x_tok[:, c, :].rearrange("p (h k) -> p h k", k=64)
                S.tt("dve", Xd[b][:], xv, colT[:, 0, c, hs].unsqueeze(2).to_broadcast([128, 8, 64]), ALU.mult, r=[t_xt, t_colT], w=[t_Xd[b]])
                S.tt("pool", Xdd[b][:], Xd[b][:], colT[:, 2, c, hs].unsqueeze(2).to_broadcast([128, 8, 64]), ALU.mult, r=[t_Xd[b], t_colT], w=[t_Xdd[b]])
                Sc, tSc = ST[si], t_ST[si]; Sn, tSn = ST[1 - si], t_ST[1 - si]
                for h in range(8):
                    g_ = h // 4
                    S.mm(psY[:, h * 64:(h + 1) * 64], M[:, h, :], Xd[b][:, h, :], True, False, r=[t_M, t_Xd[b]], w=[t_psY])
                    S.mm(psY[:, h * 64:(h + 1) * 64], Ct[:, h, :], Sc[:, h, :], False, True, r=[t_Ct, tSc], w=[t_psY])
                    S.mm(psS[:, h * 64:(h + 1) * 64], Btok[b][:, g_, :], Xdd[b][:, h, :], True, True, r=[t_Bt[b], t_Xdd[b]], w=[t_psS])
                if d == 0:
                    S.copy("act", yacc[:, c, :], psY[:], r=[t_psY], w=[t_y[c]])
                else:
                    S.tt("dve", yacc[:, c, :], yacc[:, c, :], psY[:], ALU.add, r=[t_psY, t_y[c]], w=[t_y[c]])
                ecol = 127 if d == 0 else 0
                S.tt("pool", tmpS[:], Sc[:], eP[:, :, ecol:ecol + 1].to_broadcast([128, 8, 64]), ALU.mult, r=[tSc, t_eP], w=[t_tmpS])
                S.tt("dve", Sn[:].rearrange("p a b -> p (a b)"), psS[:], tmpS[:].rearrange("p a b -> p (a b)"), ALU.add, r=[t_psS, t_tmpS], w=[tSn])
                si = 1 - si
        zg = [sb("mzg%d" % i, [128, 4, 128]) for i in range(2)]; t_zg = toks(2, "zg")
        zs = sb("mzs", [128, 512]); sq = sb("msq", [128, 512]); ofm = [sb("mofm%d" % i, [128, 4, 128]) for i in range(2)]
        ms = sb("mms", [128, 2]); t_zs, t_sq, t_ms = Tok("zs"), Tok("sq"), Tok("ms"); t_ofm = toks(2, "mofm")
        for c in range(NC2):
            b = c % 2
            cs = slice(c * 128, (c + 1) * 128)
            S.dma("sp", zg[b][:], g.zT[MB0:MB0 + 512, cs].rearrange("(tl p) t -> p tl t", p=128), w=[t_zg[b]])
            for tl in range(4):
                S.tr(psT[:, tl * 128:(tl + 1) * 128], zg[b][:, tl, :], g.ident[:], r=[t_zg[b], g.t_const], w=[t_psT])
            S.act(zs[:], psT[:], AF.Silu, r=[t_psT], w=[t_zs])
            yv = yacc[:, c, :]
            S.tt("pool", sq[:].rearrange("p (h k) -> p h k", k=64), x_tok[:, c, :].rearrange("p (h k) -> p h k", k=64),
                 Dbc[:].unsqueeze(2).to_broadcast([128, 8, 64]), ALU.mult, r=[t_xt, t_k], w=[t_sq])
            S.tt("dve", yv, yv, sq[:], ALU.add, r=[t_y[c], t_sq], w=[t_y[c]])
            S.tt("dve", yv, yv, zs[:], ALU.mult, r=[t_y[c], t_zs], w=[t_y[c]])
            S.act(sq[:], yv, AF.Square, r=[t_y[c]], w=[t_sq])
            S.reduce(ms[:], sq[:].rearrange("p (g k) -> p g k", g=2), ALU.add, r=[t_sq], w=[t_ms])
            S.ts("dve", ms[:], ms[:], 1.0 / 256, NORM_EPS, ALU.mult, ALU.add, r=[t_ms], w=[t_ms])
            S.act(ms[:], ms[:], AF.Sqrt, r=[t_ms], w=[t_ms])
            S.recip(ms[:], ms[:], r=[t_ms], w=[t_ms])
            S.tt("dve", yv.rearrange("p (g k) -> p g k", g=2), yv.rearrange("p (g k) -> p g k", g=2),
                 ms[:].unsqueeze(2).to_broadcast([128, 2, 256]), ALU.mult, r=[t_y[c], t_ms], w=[t_y[c]])
            for tl in range(4):
                S.tr(psG[:, tl * 128:(tl + 1) * 128], yacc[:, c, tl * 128:(tl + 1) * 128], g.ident[:], r=[t_y[c], g.t_const], w=[t_psG])
            for tl in range(4):
                S.ts("dve" if tl % 2 else "act_", ofm[b][:, tl, :], psG[:, tl * 128:(tl + 1) * 128], ncols[:, tl:tl + 1], None, ALU.mult,
                     r=[t_psG, t_nc], w=[t_ofm[b]])
            S.dma("sp", g.catT[512:1024, cs].rearrange("(tl p) t -> p tl t", p=128), ofm[b][:], r=[t_ofm[b]])
        S.barrier(); S.flush()


FGROUPS = [(i * 3, min(3, 22 - i * 3)) for i in range(8)]


def tok_chunks(n_tok):
    out = []
    c0 = 0
    while c0 < n_tok:
        n = min(512, n_tok - c0)
        out.append((c0, n))
        c0 += n
    return out


def swiglu_pass(g, st, aT, t_aT, h, t_h, NT, w1d, w3d, w2d, gate_col, tag, gb=None, t_gb=None, pools=None, chunks=None):
    nc, S = g.nc, g.S
    if pools is None:
        pools = {}
        sb = lambda name, shape, dt=F32: st.enter_context(nc.sbuf_tensor(name + tag, shape, dt))
        pools["w1"] = [sb("fw1_%d" % i, [128, 8, 384], BF16) for i in range(2)]
        pools["w3"] = [sb("fw3_%d" % i, [128, 8, 384], BF16) for i in range(2)]
        pools["w2"] = [sb("fw2_%d" % i, [128, 3, 1024], BF16) for i in range(2)]
        pools["t_w"] = toks(2, "fw")
        pools["gT"] = [sb("fgT%d" % i, [128, 3, 512], BF16) for i in range(2)]
        pools["t_gT"] = toks(2, "fgT")
        pools["sl"] = [sb("fsl%d" % i, [128, 512]) for i in range(2)]
        pools["t_sl"] = toks(2, "fsl")
        pools["cst"] = Caster(g, st, "fcst" + tag, 1024, nbuf=3)
        pools["ph"] = [st.enter_context(nc.psum_tensor("fph%d%s" % (i, tag), [128, 512], F32)) for i in range(4)]
        pools["t_ph"] = toks(4, "fph", True)
        pools["po"] = [st.enter_context(nc.psum_tensor("fpo%d%s" % (i, tag), [128, 512], F32)) for i in range(3)]
        pools["t_po"] = toks(3, "fpo", True)
        pools["n"] = {"w": 0, "g": 0, "ph": 0, "po": 0, "sl": 0}
    P = pools
    cst = P["cst"]
    w1v = w1d.rearrange("(k p) f -> p k f", p=128)
    w3v = w3d.rearrange("(k p) f -> p k f", p=128)
    w2v = w2d.rearrange("(k p) f -> p k f", p=128)
    chunks = chunks or tok_chunks(NT)
    for (f0, nf) in FGROUPS:
        wb = P["n"]["w"] % 2; P["n"]["w"] += 1
        tw = P["t_w"][wb]
        for k in range(8):
            cst.load(P["w1"][wb][:, k, :nf * 128], w1v[:, k, f0 * 128:(f0 + nf) * 128], tw)
            cst.load(P["w3"][wb][:, k, :nf * 128], w3v[:, k, f0 * 128:(f0 + nf) * 128], tw)
        for j in range(nf):
            cst.load(P["w2"][wb][:, j, :], w2v[:, f0 + j, :], tw)
        for (c0, n) in chunks:
            gi = P["n"]["g"] % 2; P["n"]["g"] += 1
            gT, tgT = P["gT"][gi], P["t_gT"][gi]
            for j in range(nf):
                i1 = P["n"]["ph"] % 4; i3 = (P["n"]["ph"] + 1) % 4; P["n"]["ph"] += 2
                p1, p3, t1, t3 = P["ph"][i1], P["ph"][i3], P["t_ph"][i1], P["t_ph"][i3]
                for k in range(8):
                    S.mm(p1[:, :n], P["w1"][wb][:, k, j * 128:(j + 1) * 128], aT[:, k, c0:c0 + n], k == 0, k == 7, r=[tw, t_aT], w=[t1])
                for k in range(8):
                    S.mm(p3[:, :n], P["w3"][wb][:, k, j * 128:(j + 1) * 128], aT[:, k, c0:c0 + n], k == 0, k == 7, r=[tw, t_aT], w=[t3])
                si = P["n"]["sl"] % 2; P["n"]["sl"] += 1
                sl, tsl = P["sl"][si], P["t_sl"][si]
                S.act(sl[:, :n], p1[:, :n], AF.Silu, r=[t1], w=[tsl])
                if gb is not None:
                    S.tt("pool", sl[:, :n], sl[:, :n], gb[:, c0:c0 + n], ALU.mult, r=[tsl, t_gb], w=[tsl])
                S.tt("dve", gT[:, j, :n], p3[:, :n], sl[:, :n], ALU.mult, r=[t3, tsl], w=[tgT])
            for dt_ in range(8):
                oi = P["n"]["po"] % 3; P["n"]["po"] += 1
                po, tpo = P["po"][oi], P["t_po"][oi]
                for j in range(nf):
                    S.mm(po[:, :n], P["w2"][wb][:, j, dt_ * 128:(dt_ + 1) * 128], gT[:, j, :n], j == 0, j == nf - 1, r=[tw, tgT], w=[tpo])
                S.stt(h[:, dt_, c0:c0 + n], po[:, :n], gate_col(c0)[:, dt_:dt_ + 1], h[:, dt_, c0:c0 + n], ALU.mult, ALU.add,
                      r=[tpo, t_h, g.t_mod], w=[t_h])
    return pools


def phase_l0_post(g):
    nc, S, I = g.nc, g.S, g.I
    with ExitStack() as st:
        sb = lambda name, shape, dt=F32: st.enter_context(nc.sbuf_tensor(name, shape, dt))
        h = sb("qh", [128, 8, T]); t_h = Tok("qh")
        aT = sb("qaT", [128, 8, T], BF16); t_aT = Tok("qaT")
        hTd = g.hT.rearrange("(c p) t -> p c t", p=128)
        for c in range(8):
            S.dma("sp" if c % 2 else "act", h[:, c, :], hTd[:, c, :], w=[t_h])
        with ExitStack() as st2:
            sb2 = lambda name, shape, dt=F32: st2.enter_context(nc.sbuf_tensor(name, shape, dt))
            wout = sb2("qwout", [128, 8, D], BF16); t_wout = Tok("qwout")
            cst = Caster(g, st2, "qcst", D, nbuf=2)
            catb = [sb2("qcatb%d" % i, [128, 8, 512], BF16) for i in range(2)]; t_catb = toks(2, "qcatb")
            cst2 = Caster(g, st2, "qcst2", 512, nbuf=3)
            po = [st2.enter_context(nc.psum_tensor("qpo%d" % i, [128, 512], F32)) for i in range(3)]; t_po = toks(3, "qpo", True)
            npool = norm_pools(nc, st2, "q")
            wv = I["mix_w_out"].rearrange("(k p) f -> p k f", p=128)
            for k in range(8):
                cst.load(wout[:, k, :], wv[:, k, :], t_wout)
            catv = g.catT.rearrange("(k p) t -> p k t", p=128)
            npo = 0
            for ci, (c0, n) in enumerate(CHUNKS):
                b = ci % 2
                w_ = 1 if ci == 0 else 0
                for k in range(8):
                    cst2.load(catb[b][:, k, :n], catv[:, k, c0:c0 + n], t_catb[b])
                for ft in range(8):
                    p = po[npo % 3]; tp = t_po[npo % 3]; npo += 1
                    for k in range(8):
                        S.mm(p[:, :n], wout[:, k, ft * 128:(ft + 1) * 128], catb[b][:, k, :n], k == 0, k == 7, r=[t_wout, t_catb[b]], w=[tp])
                    S.stt(h[:, ft, c0:c0 + n], p[:, :n], g.modT[:, 0, w_, 16 + ft:17 + ft], h[:, ft, c0:c0 + n], ALU.mult, ALU.add,
                          r=[tp, t_h, g.t_mod], w=[t_h])
                norm_mod(g, h[:, :, c0:c0 + n], n, g.Gf[:, 0, w_, :], g.modT[:, 0, w_, 24:32], npool, t_h, aT[:, :, c0:c0 + n], t_aT)
            if "h0mix_o" in g.dbg:
                dd = nc.dram_tensor("h0mixT", [D, T], F32, kind="ExternalOutput").ap()
                S.dma("sp", dd.rearrange("(c p) t -> p c t", p=128), h[:], r=[t_h])
            S.barrier(); S.flush()
        gate_col = lambda c0: g.modT[:, 0, 1 if c0 < TC else 0, 40:48]
        swiglu_pass(g, st, aT, t_aT, h, t_h, T, I["ffn_w1"], I["ffn_w3"], I["ffn_w2"], gate_col, "q", chunks=CHUNKS)
        for c in range(8):
            S.dma("sp" if c % 2 else "act", hTd[:, c, :], h[:, c, :], r=[t_h])
        S.barrier(); S.flush()


def alloc_l1_scratch(g):
    nc = g.nc
    def dscr(name, shape, dt):
        kind = {"kind": "ExternalOutput"} if name in g.dbg else {}
        return nc.dram_tensor(name, list(shape), dt, **kind).ap()
    g.qT = dscr("qT", (D, TL), BF16)
    g.kT = dscr("kT", (D, T), BF16)
    g.vtok = dscr("vtok", (T, D), BF16)
    g.oT = dscr("oT", (D, TL), BF16)
    g.rpbpad = dscr("rpbpad", (16, 15, 127), F32)


def phase_l1_qkv(g):
    nc, S, I = g.nc, g.S, g.I
    with ExitStack() as st:
        sb = lambda name, shape, dt=F32: st.enter_context(nc.sbuf_tensor(name, shape, dt))
        wq = sb("awq", [128, 8, 3 * D], BF16); t_wq = toks(8, "awq")
        cst = Caster(g, st, "acst", 3 * D, nbuf=2)
        hT = [sb("ahT%d" % i, [128, 8, 512]) for i in range(2)]; t_hT = toks(2, "ahT")
        aT = [sb("aaT%d" % i, [128, 8, 512], BF16) for i in range(2)]; t_aT = toks(2, "aaT")
        ob = [sb("aob%d" % i, [128, 512], BF16) for i in range(4)]; t_ob = toks(4, "aob")
        pz = [st.enter_context(nc.psum_tensor("apz%d" % i, [128, 512], F32)) for i in range(4)]; t_pz = toks(4, "apz", True)
        npool = norm_pools(nc, st, "b")
        wv = I["na_w_qkv"].rearrange("(k p) f -> p k f", p=128)
        for k in range(8):
            cst.load(wq[:, k, :], wv[:, k, :], t_wq[k])
        rp = sb("arp", [16, 15, 127]); t_rp = Tok("arp")
        S.memset("pool", rp[:], 0.0, w=[t_rp])
        S.dma("sp", rp[:, :, 48:79], I["na_rpb"], w=[t_rp])
        S.dma("sp", g.rpbpad, rp[:], r=[t_rp])
        hTd = g.hT.rearrange("(c p) t -> p c t", p=128)
        nz = 0
        for ci, (c0, n) in enumerate(CHUNKS):
            b = ci % 2
            w_ = 1 if ci == 0 else 0
            S.dma("sp", hT[b][:, :, :n], hTd[:, :, c0:c0 + n], w=[t_hT[b]])
            norm_mod(g, hT[b], n, g.Gm[:, 1, w_, :], g.modT[:, 1, w_, 0:8], npool, t_hT[b], aT[b], t_aT[b])
            for ft in range(16):
                if ft < 8 and ci == 0:
                    continue
                zb = nz % 4; nz += 1
                for k in range(8):
                    S.mm(pz[zb][:, :n], wq[:, k, ft * 128:(ft + 1) * 128], aT[b][:, k, :n], k == 0, k == 7, r=[t_wq[k], t_aT[b]], w=[t_pz[zb]])
                S.copy("act" if ft % 2 else "dve", ob[zb][:, :n], pz[zb][:, :n], r=[t_pz[zb]], w=[t_ob[zb]])
                if ft < 8:
                    S.dma("sp", g.qT[ft * 128:(ft + 1) * 128, c0 - TC:c0 - TC + n], ob[zb][:, :n], r=[t_ob[zb]])
                else:
                    S.dma("sp", g.kT[(ft - 8) * 128:(ft - 7) * 128, c0:c0 + n], ob[zb][:, :n], r=[t_ob[zb]])
            for tt_ in range(n // 128):
                for half in range(2):
                    zb = nz % 4; nz += 1
                    for k in range(8):
                        S.mm(pz[zb][:, :], aT[b][:, k, tt_ * 128:(tt_ + 1) * 128], wq[:, k, 2048 + half * 512:2048 + (half + 1) * 512],
                             k == 0, k == 7, r=[t_wq[k], t_aT[b]], w=[t_pz[zb]])
                    S.copy("act" if half else "dve", ob[zb][:, :], pz[zb][:, :], r=[t_pz[zb]], w=[t_ob[zb]])
                    S.dma("act", g.vtok[c0 + tt_ * 128:c0 + (tt_ + 1) * 128, half * 512:(half + 1) * 512], ob[zb][:, :], r=[t_ob[zb]])
        S.barrier(); S.flush()


def phase_l1_attn(g):
    nc, S, I = g.nc, g.S, g.I
    GW = 64
    with ExitStack() as st:
        sb = lambda name, shape, dt=F32: st.enter_context(nc.sbuf_tensor(name, shape, dt))
        V0 = sb("nV0", [128, 18, D], BF16)
        V1 = sb("nV1", [128, 16, D], BF16)
        t_V = Tok("nV")
        S.dma("sp", V0[:], g.vtok.rearrange("(j p) f -> p j f", p=128), w=[t_V])
        S.dma("act", V1[:, 0:15, :], g.vtok[TC + 64:TC + 64 + 15 * 128, :].rearrange("(j p) f -> p j f", p=128), w=[t_V])
        S.dma("sp", V1[0:64, 15, :], g.vtok[TC + 64 + 15 * 128:T, :], w=[t_V])
        identb = sb("nidb", [128, 128], BF16); t_k = Tok("nconst")
        S.copy("dve", identb[:], g.ident[:], r=[g.t_const], w=[t_k])
        kcr = sb("nkcr", [64, 64]); qc = sb("nqc", [64, 2]); Mcol = sb("nMcol", [64, 64]); m2 = sb("nm2", [64, 64])
        S.op("pool", lambda e: e.iota(kcr[:], [[1, 64]], base=0, channel_multiplier=0, allow_small_or_imprecise_dtypes=True), w=[t_k])
        S.op("pool", lambda e: e.iota(qc[:, 0:1], [[0, 1]], base=-8, channel_multiplier=1, allow_small_or_imprecise_dtypes=True), w=[t_k])
        S.ts("dve", qc[:, 0:1], qc[:, 0:1], 0.0, 48.0, ALU.max, ALU.min, r=[t_k], w=[t_k])
        S.ts("dve", qc[:, 1:2], qc[:, 0:1], 16.0, None, ALU.add, r=[t_k], w=[t_k])
        S.ts("dve", Mcol[:], kcr[:], qc[:, 0:1], None, ALU.is_ge, r=[t_k], w=[t_k])
        S.ts("dve", m2[:], kcr[:], qc[:, 1:2], None, ALU.is_lt, r=[t_k], w=[t_k])
        S.tt("dve", Mcol[:], Mcol[:], m2[:], ALU.mult, r=[t_k], w=[t_k])
        S.ts("dve", Mcol[:], Mcol[:], -NEG, NEG, ALU.mult, ALU.add, r=[t_k], w=[t_k])
        J = sb("nJ", [64, 64]); Bhr = sb("nBhr", [64, 15, 64]); t_Bhr = Tok("nBhr")
        S.memset("pool", J[:], 0.0, w=[t_k])
        S.op("pool", lambda e: e.affine_select(out=J[:], in_=J[:], compare_op=ALU.not_equal, fill=1.0, base=-63,
                                                pattern=[[1, 64]], channel_multiplier=1), r=[t_k], w=[t_k])
        qh = [sb("nqh%d" % i, [64, TL], BF16) for i in range(2)]
        kh = [sb("nkh%d" % i, [64, T], BF16) for i in range(2)]
        Bh = [sb("nBh%d" % i, [64, 15, 64]) for i in range(2)]
        oTh = [sb("noT%d" % i, [64, TL], BF16) for i in range(2)]
        t_qk = toks(2, "nqk"); t_Bh = toks(2, "nBh"); t_oT = toks(2, "noT")
        Ssb = [sb("nS%d" % i, [64, 768]) for i in range(2)]; t_S = toks(2, "nS")
        Pn = [sb("nPn%d" % i, [64, 768], BF16) for i in range(2)]; t_Pn = toks(2, "nPn")
        PnT = [sb("nPnT%d" % i, [128, 6, 64], BF16) for i in range(2)]; t_PnT = toks(2, "nPnT")
        stt_ = [sb("nst%d" % i, [64, 4]) for i in range(2)]; t_st = toks(2, "nst")
        ps1 = [st.enter_context(nc.psum_tensor("nps1_%d" % i, [64, 512], F32)) for i in range(2)]; t_ps1 = toks(2, "nps1", True)
        ps2 = [st.enter_context(nc.psum_tensor("nps2_%d" % i, [64, 512], F32)) for i in range(2)]; t_ps2 = toks(2, "nps2", True)
        psT = [st.enter_context(nc.psum_tensor("npsT%d" % i, [128, 6, 64], BF16)) for i in range(2)]; t_psT = toks(2, "npsT", True)
        psO = [st.enter_context(nc.psum_tensor("npsO%d" % i, [64, 64], F32)) for i in range(2)]; t_psO = toks(2, "npsO", True)
        n = 0
        for hd in range(16):
            hb = hd % 2
            S.dma("sp", qh[hb][:], g.qT[hd * 64:(hd + 1) * 64, :], w=[t_qk[hb]])
            S.dma("act", kh[hb][:], g.kT[hd * 64:(hd + 1) * 64, :], w=[t_qk[hb]])
            src = bass.AP(tensor=g.rpbpad.tensor, offset=hd * 15 * 127, ap=[[1, 64], [127, 15], [1, 64]])
            S.dma("sp", Bhr[:], src, w=[t_Bhr])
            Bflat = Bhr[:].rearrange("p a b -> p (a b)")
            S.mm(ps1[0][:, :], J[:], Bflat[:, 0:512], True, True, r=[t_k, t_Bhr], w=[t_ps1[0]])
            S.mm(ps2[0][:, 0:448], J[:], Bflat[:, 512:960], True, True, r=[t_k, t_Bhr], w=[t_ps2[0]])
            Mc = Mcol[:].unsqueeze(1)
            S.tt("dve", Bh[hb][:, 0:8, :], ps1[0][:, :].rearrange("p (a b) -> p a b", b=64), Mc.to_broadcast([64, 8, 64]), ALU.add,
                 r=[t_ps1[0], t_k], w=[t_Bh[hb]])
            S.tt("dve", Bh[hb][:, 8:15, :], ps2[0][:, 0:448].rearrange("p (a b) -> p a b", b=64), Mc.to_broadcast([64, 7, 64]), ALU.add,
                 r=[t_ps2[0], t_k], w=[t_Bh[hb]])
            for r in range(32):
                b = n % 2; n += 1
                rs = min(max(r - 4, 0), 24)
                ri0 = rs - r + 7
                qs = slice(r * GW, (r + 1) * GW)
                k0 = TC + rs * GW
                S.mm(ps1[b][:, :], qh[hb][:, qs], kh[hb][:, k0:k0 + 512], True, True, r=[t_qk[hb]], w=[t_ps1[b]])
                S.mm(ps2[b][:, 0:256], qh[hb][:, qs], kh[hb][:, 0:TC], True, True, r=[t_qk[hb]], w=[t_ps2[b]])
                S.stt(Ssb[b][:, 0:512], ps1[b][:, :], 0.125, Bh[hb][:, ri0:ri0 + 8, :].rearrange("p a b -> p (a b)"), ALU.mult, ALU.add,
                      r=[t_ps1[b], t_Bh[hb]], w=[t_S[b]])
                S.act(Ssb[b][:, 512:768], ps2[b][:, 0:256], AF.Copy, scale=0.125, r=[t_ps2[b]], w=[t_S[b]])
                S.reduce(stt_[b][:, 0:1], Ssb[b][:], ALU.max, r=[t_S[b]], w=[t_st[b]])
                S.ts("dve", stt_[b][:, 1:2], stt_[b][:, 0:1], -1.0, None, ALU.mult, r=[t_st[b]], w=[t_st[b]])
                S.act(Ssb[b][:], Ssb[b][:], AF.Exp, bias=stt_[b][:, 1:2], scale=1.0, accum_out=stt_[b][:, 2:3], r=[t_S[b], t_st[b]], w=[t_S[b], t_st[b]])
                S.recip(stt_[b][:, 3:4], stt_[b][:, 2:3], r=[t_st[b]], w=[t_st[b]])
                S.ts("dve", Pn[b][:], Ssb[b][:], stt_[b][:, 3:4], None, ALU.mult, r=[t_S[b], t_st[b]], w=[t_Pn[b]])
                for blk in range(6):
                    S.tr(psT[b][:, blk, :], Pn[b][:, blk * 128:(blk + 1) * 128], identb[0:64, 0:64], r=[t_Pn[b], t_k], w=[t_psT[b]])
                S.copy("act", PnT[b][:].rearrange("p a b -> p (a b)"), psT[b][:].rearrange("p a b -> p (a b)"), r=[t_psT[b]], w=[t_PnT[b]])
                hc = slice(hd * 64, (hd + 1) * 64)
                for blk in range(6):
                    if blk < 4:
                        if rs % 2 == 0:
                            vb = V0[:, 2 + rs // 2 + blk, hc]
                        else:
                            vb = V1[:, (rs - 1) // 2 + blk, hc]
                    else:
                        vb = V0[:, blk - 4, hc]
                    S.mm(psO[b][:, :], vb, PnT[b][:, blk, :], blk == 0, blk == 5, r=[t_V, t_PnT[b]], w=[t_psO[b]])
                S.copy("dve", oTh[hb][:, qs], psO[b][:, :], r=[t_psO[b]], w=[t_oT[hb]])
            S.dma("sp", g.oT[hd * 64:(hd + 1) * 64, :], oTh[hb][:], r=[t_oT[hb]])
        S.barrier(); S.flush()


def phase_l1_post(g):
    nc, S, I = g.nc, g.S, g.I
    NT = TL
    with ExitStack() as st:
        sb = lambda name, shape, dt=F32: st.enter_context(nc.sbuf_tensor(name, shape, dt))
        h = sb("ph", [128, 8, NT]); t_h = Tok("ph")
        aT = sb("paT", [128, 8, NT], BF16); t_aT = Tok("paT")
        gatesT = sb("pgatesT", [8, NT]); t_gates = Tok("gatesT")
        Sel8 = sb("pSel8", [8, 8, 128]); t_k = Tok("pconst")
        S.copy("dve", Sel8[:], g.ident[0:8, 0:8].unsqueeze(2).to_broadcast([8, 8, 128]), r=[g.t_const], w=[t_k])
        hTd = g.hT.rearrange("(c p) t -> p c t", p=128)
        for c in range(8):
            S.dma("sp" if c % 2 else "act", h[:, c, :], hTd[:, c, TC:T], w=[t_h])
        chunks = tok_chunks(NT)
        with ExitStack() as st2:
            sb2 = lambda name, shape, dt=F32: st2.enter_context(nc.sbuf_tensor(name, shape, dt))
            wout = sb2("pwout", [128, 8, D], BF16); t_wout = Tok("pwout")
            cst = Caster(g, st2, "pcst", D, nbuf=1)
            ob = [sb2("pob%d" % i, [128, 8, 512], BF16) for i in range(1)] * 2; t_ob = toks(1, "pob") * 2
            a32 = sb2("pa32", [128, 8, 512]); t_a32 = Tok("pa32")
            wr = sb2("pwr", [128, 8, 128]); t_wr = Tok("pwr")
            logT = sb2("plogT", [8, NT]); t_log = Tok("plogT")
            L = sb2("pL", [128, 16, 8]); W = sb2("pW", [128, 16, 8]); tmp = sb2("ptmp", [128, 16, 8]); t_L = Tok("pL")
            m1 = sb2("pm1", [128, 16]); m2_ = sb2("pm2", [128, 16]); den = sb2("pden", [128, 16])
            po = [st2.enter_context(nc.psum_tensor("ppo%d" % i, [128, 512], F32)) for i in range(3)]; t_po = toks(3, "ppo", True)
            pl = st2.enter_context(nc.psum_tensor("ppl", [128, 512], F32)); t_pl = Tok("ppl", True)
            npool = norm_pools(nc, st2, "p")
            rb, t_rb = load_cols(g, st2, "prb", [I["moe_router_b"].rearrange("(o p) -> o p", o=1)], ps=pl[:, 0:128], t_p=t_pl)
            wv = I["na_w_out"].rearrange("(k p) f -> p k f", p=128)
            for k in range(8):
                cst.load(wout[:, k, :], wv[:, k, :], t_wout)
            S.memset("pool", wr[:], 0.0, w=[t_wr])
            S.dma("sp", wr[:, :, 0:8], I["moe_router_w"].rearrange("(k p) e -> p k e", p=128), w=[t_wr])
            oTv = g.oT.rearrange("(k p) t -> p k t", p=128)
            npo = 0
            for ci, (c0, n) in enumerate(chunks):
                b = ci % 2
                S.dma("sp", ob[b][:, :, :n], oTv[:, :, c0:c0 + n], w=[t_ob[b]])
                for ft in range(8):
                    p = po[npo % 3]; tp = t_po[npo % 3]; npo += 1
                    for k in range(8):
                        S.mm(p[:, :n], wout[:, k, ft * 128:(ft + 1) * 128], ob[b][:, k, :n], k == 0, k == 7, r=[t_wout, t_ob[b]], w=[tp])
                    S.stt(h[:, ft, c0:c0 + n], p[:, :n], g.modT[:, 1, 0, 16 + ft:17 + ft], h[:, ft, c0:c0 + n], ALU.mult, ALU.add,
                          r=[tp, t_h, g.t_mod], w=[t_h])
                norm_mod(g, h[:, :, c0:c0 + n], n, g.Gf[:, 1, 0, :], g.modT[:, 1, 0, 24:32], npool, t_h, aT[:, :, c0:c0 + n], t_aT,
                         out_f32=a32)
                for k in range(8):
                    S.mm(pl[:, :n], wr[:, k, :], a32[:, k, :n], k == 0, k == 7, r=[t_wr, t_aT], w=[t_pl])
                S.ts("dve", logT[:, c0:c0 + n], pl[0:8, :n], rb[0:8, 0:1], None, ALU.add, r=[t_pl, t_rb], w=[t_log])
            if "h1att_o" in g.dbg:
                dd = nc.dram_tensor("h1attT", [D, NT], F32, kind="ExternalOutput").ap()
                S.dma("sp", dd.rearrange("(c p) t -> p c t", p=128), h[:], r=[t_h])
            for j in range(16):
                S.tr(pl[:, j * 8:(j + 1) * 8], logT[:, j * 128:(j + 1) * 128], g.ident[0:8, 0:8], r=[t_log, g.t_const], w=[t_pl])
            S.copy("dve", L[:].rearrange("p a b -> p (a b)"), pl[:, 0:128], r=[t_pl], w=[t_L])
            if "logits_o" in g.dbg:
                dd = nc.dram_tensor("logitsL", [128, 128], F32, kind="ExternalOutput").ap()
                S.dma("sp", dd[:, :], L[:].rearrange("p a b -> p (a b)"), r=[t_L])
            b3 = lambda a: a[:].unsqueeze(2).to_broadcast([128, 16, 8])
            S.reduce(m1[:], L[:], ALU.max, r=[t_L], w=[t_L])
            S.tt("dve", tmp[:], L[:], b3(m1), ALU.is_equal, r=[t_L], w=[t_L])
            S.stt(tmp[:], tmp[:], NEG, L[:], ALU.mult, ALU.add, r=[t_L], w=[t_L])
            S.reduce(m2_[:], tmp[:], ALU.max, r=[t_L], w=[t_L])
            S.tt("dve", tmp[:], L[:], b3(m2_), ALU.is_ge, r=[t_L], w=[t_L])
            S.tt("dve", W[:], L[:], b3(m1), ALU.subtract, r=[t_L], w=[t_L])
            S.act(W[:], W[:], AF.Exp, r=[t_L], w=[t_L])
            S.tt("dve", W[:], W[:], tmp[:], ALU.mult, r=[t_L], w=[t_L])
            S.reduce(den[:], W[:], ALU.add, r=[t_L], w=[t_L])
            S.recip(den[:], den[:], r=[t_L], w=[t_L])
            S.tt("dve", W[:], W[:], b3(den), ALU.mult, r=[t_L], w=[t_L])
            for j4 in range(4):
                for j in range(4):
                    S.tr(pl[0:8, j * 128:(j + 1) * 128], W[:, j4 * 4 + j, :], g.ident[:], r=[t_L, g.t_const], w=[t_pl])
                S.copy("dve", gatesT[:, j4 * 512:(j4 + 1) * 512], pl[0:8, :], r=[t_pl], w=[t_gates])
            S.barrier(); S.flush()
        gbs = [sb("pgb%d" % i, [128, NT]) for i in range(2)]; t_gb = toks(2, "pgb")
        pg = st.enter_context(nc.psum_tensor("ppg", [128, 512], F32)); t_pg = Tok("ppg", True)
        gate_col = lambda c0: g.modT[:, 1, 0, 40:48]
        pools = None
        NEXP = int(os.environ.get("MOE_NE", str(NE)))
        for e in range(NEXP):
            gb, tgb = gbs[e % 2], t_gb[e % 2]
            for (c0, n) in chunks:
                S.mm(pg[:, :n], Sel8[:, e, :], gatesT[:, c0:c0 + n], True, True, r=[t_k, t_gates], w=[t_pg])
                S.copy("act", gb[:, c0:c0 + n], pg[:, :n], r=[t_pg], w=[tgb])
            pools = swiglu_pass(g, st, aT, t_aT, h, t_h, NT, I["moe_w1"][e], I["moe_w3"][e], I["moe_w2"][e], gate_col, "p",
                                gb=gb, t_gb=tgb, pools=pools)
        if "h1_o" in g.dbg:
            dd = nc.dram_tensor("h1T", [D, NT], F32, kind="ExternalOutput").ap()
            S.dma("sp", dd.rearrange("(c p) t -> p c t", p=128), h[:], r=[t_h])
        with ExitStack() as st3:
            sb3 = lambda name, shape, dt=F32: st3.enter_context(nc.sbuf_tensor(name, shape, dt))
            sq = sb3("psq", [128, 8, 512]); rstd = sb3("prstd", [128, 512]); y = sq
            ot = [gbs[i][:, 0:D] for i in range(2)]
            t_sq, t_rstd = Tok("psq"), Tok("prstd"); t_yy = t_sq; t_ot = t_gb
            pt = [pools["ph"][i] for i in range(4)]; t_pt = [pools["t_ph"][i] for i in range(4)]
            pss = pools["po"][0]; t_pss = pools["t_po"][0]
            no = 0
            for (c0, n) in chunks:
                S.act(sq[:, :, :n], h[:, :, c0:c0 + n], AF.Square, r=[t_h], w=[t_sq])
                for c in range(8):
                    S.mm(pss[:, :n], g.ones[:], sq[:, c, :n], c == 0, c == 7, r=[t_sq, g.t_const], w=[t_pss])
                S.ts("dve", rstd[:, :n], pss[:, :n], 1.0 / D, NORM_EPS, ALU.mult, ALU.add, r=[t_pss], w=[t_rstd])
                S.act(rstd[:, :n], rstd[:, :n], AF.Sqrt, r=[t_rstd], w=[t_rstd])
                S.recip(rstd[:, :n], rstd[:, :n], r=[t_rstd], w=[t_rstd])
                for c in range(8):
                    S.stt(y[:, c, :n], h[:, c, c0:c0 + n], g.nfin[:, c:c + 1], rstd[:, :n], ALU.mult, ALU.mult, r=[t_h, t_rstd, g.t_mod], w=[t_yy])
                for tt_ in range(n // 128):
                    o_ = ot[no % 2]; to = t_ot[no % 2]; no += 1
                    for half in range(2):
                        p = pt[(2 * no + half) % 4]; tp = t_pt[(2 * no + half) % 4]
                        for c in range(4):
                            S.tr(p[:, c * 128:(c + 1) * 128], y[:, half * 4 + c, tt_ * 128:(tt_ + 1) * 128], g.ident[:], r=[t_yy, g.t_const], w=[tp])
                        S.copy("act" if half else "dve", o_[:, half * 512:(half + 1) * 512], p[:, :], r=[tp], w=[to])
                    S.dma("sp", g.out[c0 + tt_ * 128:c0 + (tt_ + 1) * 128, :], o_[:], r=[to])
            S.barrier(); S.flush()
        S.barrier(); S.flush()
```

```python
from contextlib import ExitStack
import os
import numpy as np
import concourse.bass as bass
import concourse.mybir as mybir
from concourse.bass_utils import run_bass_kernel_spmd

F32 = mybir.dt.float32
BF16 = mybir.dt.bfloat16
AF = mybir.ActivationFunctionType
ALU = mybir.AluOpType
AX = mybir.AxisListType

D = 1024
TC, TL = 256, 2048
T = TC + TL
NORM_EPS = 1e-6
MIX_IN = 3312
RW_COLS = 1760
D_FF = 2816
NE = 8

ENGS = ("pe", "act", "dve", "pool", "sp")
N_DSEM = 24


class Tok:
    __slots__ = ("name", "w", "r", "psum")

    def __init__(self, name="", psum=False):
        self.name = name
        self.w = None
        self.r = {}
        self.psum = psum


def toks(n, name="", psum=False):
    return [Tok("%s%d" % (name, i), psum) for i in range(n)]


class Sched:
    def __init__(self, nc, stack, self_sync=True):
        self.nc = nc
        self.self_sync = self_sync
        self.csem = {e: stack.enter_context(nc.semaphore("c_" + e)) for e in ENGS if e != "sp"}
        self.dsem = [stack.enter_context(nc.semaphore("d%d" % i)) for i in range(N_DSEM)]
        self.cnt = {e: 0 for e in ENGS}
        self.dcount = 0
        self.dval = [0] * N_DSEM
        self.waited = {e: {} for e in ENGS}
        self.rec = {e: [] for e in ENGS}
        self.n_ops = 0
        self.fence = stack.enter_context(nc.sbuf_tensor("fence", [128, 80], F32))
        self.t_fconst = Tok("fconst")
        self.t_floc = {"dve": toks(32, "fd"), "act": toks(32, "fa")}
        self.nfence = {"dve": 0, "act": 0}

    def _fence(self, eng):
        j = self.nfence[eng] % 32
        self.nfence[eng] += 1
        f = self.fence
        if eng == "dve":
            self.op("dve", lambda e: e.memset(f[0:1, j:j + 1], 0.0), r=(), w=[self.t_floc["dve"][j]])
        else:
            self.op("act", lambda e: e.activation(out=f[0:1, 32 + j:33 + j], in_=f[0:1, 72:73], func=AF.Copy),
                    r=[self.t_fconst], w=[self.t_floc["act"][j]])
        return self.cnt[eng]

    def _need(self, eng, dep, waits):
        if dep is None:
            return
        if dep[0] == "c":
            _, e2, idx = dep
            if e2 == eng and (eng == "pe" or not self.self_sync):
                return
            key = ("c", e2)
            val = idx
        else:
            _, slot, val = dep
            key = ("d", slot)
        if self.waited[eng].get(key, 0) >= val:
            return
        self.waited[eng][key] = val
        waits.append((key, val))

    def _deps(self, eng, r, w):
        waits = []
        for t in r:
            self._need(eng, t.w, waits)
            if t.psum:
                for k, v in t.r.items():
                    if not isinstance(k, tuple) and k != eng:
                        self._need(eng, ("c", k, v), waits)
        for t in w:
            self._need(eng, t.w, waits)
            for k, v in t.r.items():
                if isinstance(k, tuple):
                    self._need(eng, ("d", k[1], v), waits)
                else:
                    self._need(eng, ("c", k, v), waits)
        return waits

    def op(self, eng, fn, r=(), w=()):
        waits = self._deps(eng, r, w)
        self.cnt[eng] += 1
        idx = self.cnt[eng]
        for t in r:
            t.r[eng] = idx
        for t in w:
            t.w = ("c", eng, idx)
            t.r = {}
        self.rec[eng].append((waits, fn, ("c", eng)))
        self.n_ops += 1
        if eng in ("dve", "act") and any(t.psum for t in r):
            fidx = self._fence(eng)
            for t in r:
                if t.psum:
                    t.r[eng] = fidx

    def dma(self, eng, out, in_, r=(), w=(), **kw):
        slot = self.dcount % N_DSEM
        self.dcount += 1
        waits = self._deps(eng, r, w)
        if self.dval[slot] > 0:
            self._need(eng, ("d", slot, self.dval[slot]), waits)
        self.dval[slot] += 16
        val = self.dval[slot]
        for t in r:
            t.r[("d", slot)] = val
        for t in w:
            t.w = ("d", slot, val)
            t.r = {}
        fn = (lambda e, out=out, in_=in_, kw=kw: e.dma_start(out=out, in_=in_, **kw))
        self.rec[eng].append((waits, fn, ("d", slot)))
        self.n_ops += 1

    def barrier(self):
        for eng in ENGS:
            waits = []
            for e2 in ENGS:
                if e2 == "sp" or self.cnt[e2] == 0:
                    continue
                self._need(eng, ("c", e2, self.cnt[e2]), waits)
            for slot in range(N_DSEM):
                if self.dval[slot] > 0:
                    self._need(eng, ("d", slot, self.dval[slot]), waits)
            if waits:
                self.rec[eng].append((waits, None, None))

    def _sem(self, key):
        return self.csem[key[1]] if key[0] == "c" else self.dsem[key[1]]

    def flush(self):
        nc = self.nc
        rec = self.rec
        self.rec = {e: [] for e in ENGS}

        def replay(eng_name):
            def body(e):
                for waits, fn, sig in rec[eng_name]:
                    for key, val in waits:
                        e.wait_ge(self._sem(key), val)
                    if fn is None:
                        continue
                    ins = fn(e)
                    if sig[0] == "c":
                        ins.then_inc(self.csem[sig[1]], 1)
                    else:
                        ins.then_inc(self.dsem[sig[1]], 16)
            return body

        with nc.Block() as block:
            if rec["pe"]:
                block.tensor(replay("pe"))
            if rec["act"]:
                block.scalar(replay("act"))
            if rec["dve"]:
                block.vector(replay("dve"))
            if rec["pool"]:
                block.gpsimd(replay("pool"))
            if rec["sp"]:
                block.sync(replay("sp"))

    def mm(self, out, lhsT, rhs, start, stop, r=(), w=(), f32r=False):
        if (f32r or getattr(self, "default_f32r", False)) and os.environ.get("NO_F32R") is None and lhsT.dtype == F32 and rhs.dtype == F32:
            lhsT = lhsT.bitcast(mybir.dt.float32r)
            rhs = rhs.bitcast(mybir.dt.float32r)
        self.op("pe", lambda e: e.matmul(out, lhsT=lhsT, rhs=rhs, start=start, stop=stop), r, w)

    def tr(self, out, in_, ident, r=(), w=()):
        self.op("pe", lambda e: e.transpose(out, in_, ident), r, w)

    def act(self, out, in_, func, bias=None, scale=None, accum_out=None, r=(), w=()):
        kw = {}
        if bias is not None:
            kw["bias"] = bias
        if scale is not None:
            kw["scale"] = scale
        if accum_out is not None:
            kw["accum_out"] = accum_out
        self.op("act", lambda e: e.activation(out=out, in_=in_, func=func, **kw), r, w)

    def ts(self, eng, out, in0, s1, s2, op0, op1=None, accum_out=None, r=(), w=()):
        if eng == "act_":
            return self.act(out, in0, AF.Copy, scale=s1, r=r, w=w)
        kw = {}
        if op1 is not None:
            kw["op1"] = op1
        if accum_out is not None:
            kw["accum_out"] = accum_out
        self.op(eng, lambda e: e.tensor_scalar(out=out, in0=in0, scalar1=s1, scalar2=s2, op0=op0, **kw), r, w)

    def tt(self, eng, out, in0, in1, op, r=(), w=()):
        self.op(eng, lambda e: e.tensor_tensor(out=out, in0=in0, in1=in1, op=op), r, w)

    def stt(self, out, in0, scalar, in1, op0, op1, r=(), w=()):
        self.op("dve", lambda e: e.scalar_tensor_tensor(out=out, in0=in0, scalar=scalar, in1=in1, op0=op0, op1=op1), r, w)

    def copy(self, eng, out, in_, r=(), w=()):
        if eng == "act":
            self.op("act", lambda e: e.copy(out=out, in_=in_), r, w)
        else:
            self.op(eng, lambda e: e.tensor_copy(out=out, in_=in_), r, w)

    def recip(self, out, in_, r=(), w=()):
        self.op("dve", lambda e: e.reciprocal(out=out, in_=in_), r, w)

    def reduce(self, out, in_, op, axis=None, r=(), w=()):
        self.op("dve", lambda e: e.tensor_reduce(out=out, in_=in_, axis=axis or AX.X, op=op), r, w)

    def memset(self, eng, ap, val, r=(), w=()):
        self.op(eng, lambda e: e.memset(ap, val), r, w)


class Ctx:
    pass


def build_program(dbg=(), upto=99):
    nc = bass.Bass("TRN2", target_bir_lowering=False)
    g = Ctx()
    g.nc = nc
    g.dbg = set(dbg)

    def din(name, shape):
        return nc.dram_tensor(name, list(shape), F32, kind="ExternalInput").ap()

    def dscr(name, shape, dtype=F32):
        if name in g.dbg:
            return nc.dram_tensor(name, list(shape), dtype, kind="ExternalOutput").ap()
        return nc.dram_tensor(name, list(shape), dtype).ap()

    specs = dict(
        x=(TL, D), c=(D,), ctx=(TC, D), c_ctx=(D,), w_ada=(2, D, 6 * D), b_ada=(2, 6 * D),
        norm_mix=(2, D), norm_ffn=(2, D), norm_final=(D,), mix_w_in=(D, MIX_IN), mix_w_out=(D, D),
        rw_mu=(2, RW_COLS), rw_w0=(2, 512), rw_w_up=(2, 32, 512), rw_a0=(2, 512), rw_a_up=(2, 32, 512),
        rw_g_up=(96, 512), rw_k_k=(512,), rw_k_a=(512,), rw_r_k=(2, 512), rw_gn_w=(512,), rw_gn_b=(512,),
        mb_conv_w=(5, 1024), mb_conv_b=(1024,), mb_dt_bias=(16,), mb_a_log=(16,), mb_d=(8,), mb_norm_w=(512,),
        ffn_w1=(D, D_FF), ffn_w3=(D, D_FF), ffn_w2=(D_FF, D),
        na_w_qkv=(D, 3 * D), na_w_out=(D, D), na_rpb=(16, 15, 31),
        moe_router_w=(D, NE), moe_router_b=(NE,), moe_w1=(NE, D, D_FF), moe_w3=(NE, D, D_FF), moe_w2=(NE, D_FF, D),
    )

    class LazyIn(dict):
        def __missing__(self, k):
            v = din(k, specs[k])
            self[k] = v
            return v
    I = LazyIn()
    g.I = I
    out = nc.dram_tensor("out", [TL, D], F32, kind="ExternalOutput").ap()
    g.out = out

    g.hT = dscr("hT", (D, T))
    g.zT = dscr("zT", (MIX_IN, T))

    with ExitStack() as st:
        S = Sched(nc, st)
        g.S = S
        g.ident = st.enter_context(nc.sbuf_tensor("ident", [128, 128], F32))
        g.ones = st.enter_context(nc.sbuf_tensor("ones", [128, 128], F32))
        g.modT = st.enter_context(nc.sbuf_tensor("modT", [128, 2, 2, 48], F32))
        g.Gm = st.enter_context(nc.sbuf_tensor("Gm", [128, 2, 2, 8], F32))
        g.Gf = st.enter_context(nc.sbuf_tensor("Gf", [128, 2, 2, 8], F32))
        g.nfin = st.enter_context(nc.sbuf_tensor("nfin", [128, 8], F32))
        g.t_const = Tok("const")
        g.t_mod = Tok("mod")

        S.memset("pool", g.ident[:], 0.0, w=[g.t_const])
        S.op("pool", lambda e: e.affine_select(out=g.ident[:], in_=g.ident[:], compare_op=ALU.not_equal, fill=1.0,
                                                base=0, pattern=[[-1, 128]], channel_multiplier=1),
             r=[g.t_const], w=[g.t_const])
        S.memset("pool", g.ones[:], 1.0, w=[g.t_const])
        S.memset("pool", S.fence[:], 0.0, w=[S.t_fconst])

        phase_adaln(g)
        S.barrier(); S.flush()
        if upto >= 1:
            phase_l0_pre(g)
        if upto >= 2:
            alloc_rwkv_scratch(g)
            phase_rwkv_lora(g)
        if upto >= 3:
            phase_rwkv_prep(g)
        if upto >= 4 and not os.environ.get("SKIP_RWSCAN"):
            phase_rwkv_scan(g)
        if upto >= 5:
            alloc_ssd_scratch(g)
            phase_ssd_prep(g)
        if upto >= 6:
            phase_ssd_scan(g)
        if upto >= 7 and not os.environ.get("SKIP_L0"):
            phase_l0_post(g)
        if upto >= 8:
            alloc_l1_scratch(g)
            phase_l1_qkv(g)
        if upto >= 9:
            phase_l1_attn(g)
        if upto >= 10:
            phase_l1_post(g)
        S.barrier(); S.flush()
    g.used_inputs = list(I.keys())
    nc.used_inputs = g.used_inputs
    return nc


def col1(ap1d, p=128):
    return ap1d.rearrange("(k p o) -> p k o", p=p, o=1)


def load_cols(g, st, name, srcs, ps=None, t_p=None):
    nc, S = g.nc, g.S
    R = sum(a.shape[0] for a in srcs)
    assert R <= 128
    stage = st.enter_context(nc.sbuf_tensor(name + "_stg", [128, 128], F32))
    dst = st.enter_context(nc.sbuf_tensor(name, [128, R], F32))
    if ps is None:
        ps = st.enter_context(nc.psum_tensor(name + "_ps", [128, 128], F32))
        t_p = Tok(name + "p", True)
    t_s, t_d = Tok(name + "s"), Tok(name)
    S.memset("pool", stage[:], 0.0, w=[t_s])
    r0 = 0
    for a in srcs:
        r, w = a.shape
        q0 = 0
        while q0 < r:
            rem = r - q0
            n = (rem // 16) * 16 if rem >= 16 else max(x for x in (8, 4, 2, 1) if x <= rem)
            S.dma("sp", stage[r0 + q0:r0 + q0 + n, :w], a[q0:q0 + n, :], w=[t_s])
            q0 += n
        r0 += r
    S.tr(ps[:, :R], stage[:R, :], g.ident[:R, :R], r=[t_s, g.t_const], w=[t_p])
    S.copy("dve", dst[:], ps[:, :R], r=[t_p], w=[t_d])
    return dst, t_d


def phase_adaln(g):
    nc, S, I = g.nc, g.S, g.I
    with ExitStack() as st:
        cT = st.enter_context(nc.sbuf_tensor("cT", [128, 8, 2], F32))
        wb = [st.enter_context(nc.sbuf_tensor("wada%d" % i, [128, 8, 1024], F32)) for i in range(2)]
        ps = st.enter_context(nc.psum_tensor("ps_ada", [128, 2, 48, 2], F32))
        t_c, t_ps = Tok("c"), Tok("ps", True)
        t_w = toks(2, "wada")
        v2 = lambda a: a.rearrange("(j p) -> j p", p=128)
        colA, t_b = load_cols(g, st, "colA", [v2(I["c"]), v2(I["c_ctx"]), I["norm_mix"].rearrange("i (j p) -> (i j) p", p=128),
                                              I["norm_ffn"].rearrange("i (j p) -> (i j) p", p=128), v2(I["norm_final"])])
        bada, t_b2 = load_cols(g, st, "colB", [I["b_ada"].rearrange("i (j p) -> (i j) p", p=128)])
        nm = colA[:, 16:32].rearrange("p (i j) -> p i j", i=2)
        nf = colA[:, 32:48].rearrange("p (i j) -> p i j", i=2)
        badav = bada[:].rearrange("p (i j) -> p i j", i=2)
        S.act(cT[:, :, 0], colA[:, 0:8], AF.Silu, r=[t_b], w=[t_c])
        S.act(cT[:, :, 1], colA[:, 8:16], AF.Silu, r=[t_b], w=[t_c])
        n = 0
        for i in range(2):
            wv = I["w_ada"][i].rearrange("(k p) n -> p k n", p=128)
            for blk in range(6):
                buf = wb[n % 2]; tw = t_w[n % 2]; n += 1
                S.dma("sp" if n % 2 else "act", buf[:], wv[:, :, blk * 1024:(blk + 1) * 1024], w=[tw])
                for jj in range(8):
                    j = blk * 8 + jj
                    for k in range(8):
                        S.mm(ps[:, i, j, :], buf[:, k, jj * 128:(jj + 1) * 128], cT[:, k, :], k == 0, k == 7,
                             r=[tw, t_c], w=[t_ps])
        for i in range(2):
            for w_ in range(2):
                S.tt("dve", g.modT[:, i, w_, :], ps[:, i, :, w_], badav[:, i, :], ALU.add, r=[t_ps, t_b2], w=[g.t_mod])
        for i in range(2):
            for w_ in range(2):
                S.stt(g.Gm[:, i, w_, :], g.modT[:, i, w_, 8:16], 1.0, nm[:, i, :], ALU.add, ALU.mult, r=[g.t_mod, t_b], w=[g.t_mod])
                S.stt(g.Gf[:, i, w_, :], g.modT[:, i, w_, 32:40], 1.0, nf[:, i, :], ALU.add, ALU.mult, r=[g.t_mod, t_b], w=[g.t_mod])
        S.copy("dve", g.nfin[:], colA[:, 48:56], r=[t_b], w=[g.t_mod])
        if "modT" in g.dbg:
            dd = nc.dram_tensor("modT_o", [128, 2 * 2 * 48], F32, kind="ExternalOutput").ap()
            S.dma("sp", dd[:, :], g.modT[:].rearrange("p a b c -> p (a b c)"), r=[g.t_mod])
        S.barrier(); S.flush()


def norm_mod(g, hT, n, Gsc, shift, pools, r_h, out_bf, t_out, out_f32=None):
    S = g.S
    sq, rstd, ps, tmp = pools["sq"], pools["rstd"], pools["ps"], pools["tmp"]
    t_sq, t_rstd, t_ps, t_tmp = pools["t_sq"], pools["t_rstd"], pools["t_ps"], pools["t_tmp"]
    S.act(sq[:, :, :n], hT[:, :, :n], AF.Square, r=[r_h], w=[t_sq])
    for c in range(8):
        S.mm(ps[:, :n], g.ones[:], sq[:, c, :n], c == 0, c == 7, r=[t_sq, g.t_const], w=[t_ps])
    S.ts("dve", rstd[:, :n], ps[:, :n], 1.0 / D, NORM_EPS, ALU.mult, ALU.add, r=[t_ps], w=[t_rstd])
    S.act(rstd[:, :n], rstd[:, :n], AF.Sqrt, r=[t_rstd], w=[t_rstd])
    S.op("dve", lambda e: e.reciprocal(out=rstd[:, :n], in_=rstd[:, :n]), r=[t_rstd], w=[t_rstd])
    for c in range(8):
        S.stt(tmp[:, c, :n], hT[:, c, :n], Gsc[:, c:c + 1], rstd[:, :n], ALU.mult, ALU.mult, r=[r_h, t_rstd, g.t_mod], w=[t_tmp])
    for c in range(8):
        S.act(out_bf[:, c, :n], tmp[:, c, :n], AF.Identity, bias=shift[:, c:c + 1], scale=1.0, r=[t_tmp, g.t_mod], w=[t_out])
        if out_f32 is not None:
            S.ts("pool", out_f32[:, c, :n], tmp[:, c, :n], shift[:, c:c + 1], None, ALU.add, r=[t_tmp, g.t_mod], w=[t_out])


def norm_pools(nc, st, tag):
    p = {}
    p["sq"] = st.enter_context(nc.sbuf_tensor("nsq" + tag, [128, 8, 512], F32))
    p["tmp"] = st.enter_context(nc.sbuf_tensor("ntmp" + tag, [128, 8, 512], F32))
    p["rstd"] = st.enter_context(nc.sbuf_tensor("nrstd" + tag, [128, 512], F32))
    p["ps"] = st.enter_context(nc.psum_tensor("nps" + tag, [128, 512], F32))
    for k in ("sq", "tmp", "rstd", "ps"):
        p["t_" + k] = Tok("n" + k, k == "ps")
    return p


class Caster:
    def __init__(self, g, st, name, cols, nbuf=2):
        self.S = g.S
        self.stage = [st.enter_context(g.nc.sbuf_tensor("%s_stg%d" % (name, i), [128, cols], F32)) for i in range(nbuf)]
        self.tok = toks(nbuf, name + "_stg")
        self.n = 0
        self.nbuf = nbuf
        self.engs = ("pool", "dve", "act")

    def load(self, dst, src, w_tok, rows=128, eng=None, dma_eng=None):
        i = self.n % self.nbuf
        n = dst.shape[1]
        e = eng or self.engs[self.n % 2]
        q = dma_eng or ("sp" if self.n % 2 == 0 else "act")
        self.n += 1
        self.S.dma(q, self.stage[i][:rows, :n], src, w=[self.tok[i]])
        self.S.copy(e, dst, self.stage[i][:rows, :n], r=[self.tok[i]], w=[w_tok])


CHUNKS = [(0, 256)] + [(256 + 512 * i, 512) for i in range(4)]


def phase_l0_pre(g):
    nc, S, I = g.nc, g.S, g.I
    with ExitStack() as st:
        win = st.enter_context(nc.sbuf_tensor("win", [128, 8, MIX_IN], BF16))
        xt = [st.enter_context(nc.sbuf_tensor("xt%d" % i, [128, 4, D], F32)) for i in range(2)]
        hT = [st.enter_context(nc.sbuf_tensor("hTs%d" % i, [128, 8, 512], F32)) for i in range(2)]
        aT = [st.enter_context(nc.sbuf_tensor("aT%d" % i, [128, 8, 512], BF16)) for i in range(2)]
        zt = [st.enter_context(nc.sbuf_tensor("zt%d" % i, [128, 512], F32)) for i in range(3)]
        pT = [st.enter_context(nc.psum_tensor("pT%d" % i, [128, 512], F32)) for i in range(2)]
        pz = [st.enter_context(nc.psum_tensor("pz%d" % i, [128, 512], F32)) for i in range(3)]
        npool = norm_pools(nc, st, "a")
        t_win = toks(8, "win"); t_xt = toks(2, "xt"); t_hT = toks(2, "hT"); t_aT = toks(2, "aT")
        t_zt = toks(3, "zt"); t_pT = toks(2, "pT", True); t_pz = toks(3, "pz", True)
        wv = I["mix_w_in"].rearrange("(k p) f -> p k f", p=128)
        cst = Caster(g, st, "winc", MIX_IN)
        for k in range(8):
            cst.load(win[:, k, :], wv[:, k, :], t_win[k])
        hTd = g.hT.rearrange("(c p) t -> p c t", p=128)
        nz = 0; npt = 0
        for ci, (c0, n) in enumerate(CHUNKS):
            nt = n // 128
            b = ci % 2
            src = I["ctx"] if ci == 0 else I["x"][(ci - 1) * 512:ci * 512, :]
            S.dma("sp", xt[b][:, :nt, :], src.rearrange("(t p) d -> p t d", p=128), w=[t_xt[b]])
            for c in range(8):
                pb = npt % 2; npt += 1
                for t in range(nt):
                    S.tr(pT[pb][:, t * 128:(t + 1) * 128], xt[b][:, t, c * 128:(c + 1) * 128], g.ident[:],
                         r=[t_xt[b], g.t_const], w=[t_pT[pb]])
                S.copy("act" if c % 2 else "dve", hT[b][:, c, :n], pT[pb][:, :n], r=[t_pT[pb]], w=[t_hT[b]])
            S.dma("sp", hTd[:, :, c0:c0 + n], hT[b][:, :, :n], r=[t_hT[b]])
            w_ = 1 if ci == 0 else 0
            norm_mod(g, hT[b], n, g.Gm[:, 0, w_, :], g.modT[:, 0, w_, 0:8], npool, t_hT[b], aT[b], t_aT[b])
            for ft in range(26):
                f0 = ft * 128; fs = min(128, MIX_IN - f0)
                zb = nz % 3; nz += 1
                for k in range(8):
                    S.mm(pz[zb][:fs, :n], win[:, k, f0:f0 + fs], aT[b][:, k, :n], k == 0, k == 7,
                         r=[t_win[k], t_aT[b]], w=[t_pz[zb]])
                S.copy("act" if ft % 2 else "dve", zt[zb][:fs, :n], pz[zb][:fs, :n], r=[t_pz[zb]], w=[t_zt[zb]])
                S.dma("sp", g.zT[f0:f0 + fs, c0:c0 + n], zt[zb][:fs, :n], r=[t_zt[zb]])
        S.barrier(); S.flush()


_SQUEEZE0 = ("mix_w_in", "mix_w_out", "rw_mu", "rw_w0", "rw_w_up", "rw_a0", "rw_a_up", "rw_g_up", "rw_k_k", "rw_k_a",
             "rw_gn_w", "rw_gn_b", "mb_conv_w", "mb_conv_b", "mb_d", "mb_norm_w", "ffn_w1", "ffn_w3", "ffn_w2",
             "na_w_qkv", "na_w_out", "na_rpb", "moe_router_w", "moe_router_b", "moe_w1", "moe_w3", "moe_w2")


def make_in_maps(inputs):
    shared = {}
    for k, v in inputs.items():
        if k in ("x", "c", "ctx"):
            continue
        a = np.ascontiguousarray(np.asarray(v, dtype=np.float32))
        if k in _SQUEEZE0:
            a = a[0]
        elif k == "rw_r_k":
            a = a[0].reshape(2, 512)
        elif k in ("mb_dt_bias", "mb_a_log"):
            a = a[0].reshape(16)
        shared[k] = np.ascontiguousarray(a)
    maps = []
    for b in range(8):
        m = dict(shared)
        m["x"] = np.ascontiguousarray(np.asarray(inputs["x"][b], dtype=np.float32))
        m["c"] = np.ascontiguousarray(np.asarray(inputs["c"][b], dtype=np.float32))
        m["ctx"] = np.ascontiguousarray(np.asarray(inputs["ctx"][b], dtype=np.float32))
        maps.append(m)
    return maps


def kernel(**inputs):
    nc = build_program()
    maps = make_in_maps(inputs)
    res = run_bass_kernel_spmd(nc, maps, core_ids=list(range(8)))
    return np.stack([np.asarray(r["out"], dtype=np.float32) for r in res.results], axis=0)


KAPPA = 0.6065306597126334
NCH = T // 64
SEGS = [(0, TC), (TC, T)]


def shift_tile(S, dst, src, c0, mp, mn, rows, r, w):
    S.act(dst[:rows, :], src[:rows, :], AF.Copy, scale=c0, r=r, w=w)
    for a, b in SEGS:
        S.stt(dst[:rows, a + 1:b], src[:rows, a:b - 1], mp, dst[:rows, a + 1:b], ALU.mult, ALU.add, r=r + w, w=w)
        S.stt(dst[:rows, a:b - 1], src[:rows, a + 1:b], mn, dst[:rows, a:b - 1], ALU.mult, ALU.add, r=r + w, w=w)


def alloc_rwkv_scratch(g):
    nc = g.nc
    def dscr(name, shape):
        kind = {"kind": "ExternalOutput"} if name in g.dbg else {}
        return nc.dram_tensor(name, list(shape), F32, **kind).ap()
    g.PR = [[dscr("PR%d_%d" % (d, q), (512, T)) for q in range(6)] for d in range(2)]
    g.vS = dscr("vS", (512, T))
    g.bonusT = dscr("bonusT", (512, T))
    g.gateT = dscr("gateT", (512, T))
    g.WCdd = [dscr("WCd%d" % d, (512, NCH)) for d in range(2)]
    g.sS = [dscr("sS%d" % d, (512, T)) for d in range(2)]
    g.iclrS = [dscr("iclrS%d" % d, (512, T)) for d in range(2)]
    g.catT = dscr("catT", (D, T))


def phase_rwkv_lora(g):
    nc, S, I = g.nc, g.S, g.I
    with ExitStack() as st:
        v2 = lambda a: a.rearrange("(j p) -> j p", p=128)
        mu0, mu1 = I["rw_mu"][0], I["rw_mu"][1]
        cols, t_cols = load_cols(g, st, "lcols", [
            mu0[1536:1664].rearrange("(j p) -> j p", p=128), mu1[1536:1664].rearrange("(j p) -> j p", p=128),
            mu0[1664:1760].rearrange("(j p) -> j p", p=96), mu1[1664:1760].rearrange("(j p) -> j p", p=96),
            I["rw_w0"].rearrange("d (j p) -> (d j) p", p=128), I["rw_a0"].rearrange("d (j p) -> (d j) p", p=128)])
        c0 = st.enter_context(nc.sbuf_tensor("lc0", [128, 2], F32))
        t_c0 = Tok("c0")
        for i in range(2):
            S.tt("dve", c0[:, i:i + 1], cols[:, 2 * i:2 * i + 1], cols[:, 2 * i + 1:2 * i + 2], ALU.add, r=[t_cols], w=[t_c0])
        S.ts("dve", c0[:], c0[:], -1.0, 1.0, ALU.mult, ALU.add, r=[t_c0], w=[t_c0])
        zraw = st.enter_context(nc.sbuf_tensor("lzraw", [128, T], F32))
        z12 = st.enter_context(nc.sbuf_tensor("lz12", [128, T], F32))
        z12t = st.enter_context(nc.sbuf_tensor("lz12t", [128, T], F32))
        zlg = st.enter_context(nc.sbuf_tensor("lzlg", [128, T], F32))
        wupE = st.enter_context(nc.sbuf_tensor("wupE", [128, 2, 512], F32))
        aupE = st.enter_context(nc.sbuf_tensor("aupE", [128, 2, 512], F32))
        gup = st.enter_context(nc.sbuf_tensor("gup", [96, 512], F32))
        ob = [st.enter_context(nc.sbuf_tensor("lob%d" % i, [128, T], F32)) for i in range(2)]
        pl = [st.enter_context(nc.psum_tensor("lps%d" % i, [128, 512], F32)) for i in range(3)]
        t_zr, t_12, t_12t, t_lg, t_wE = Tok("zr"), Tok("z12"), Tok("z12t"), Tok("zlg"), Tok("wE")
        t_ob = toks(2, "lob"); t_pl = toks(3, "lps", True)
        S.memset("pool", wupE[:], 0.0, w=[t_wE])
        S.memset("pool", aupE[:], 0.0, w=[t_wE])
        for d in range(2):
            S.dma("sp", wupE[d * 32:(d + 1) * 32, d, :], I["rw_w_up"][d], w=[t_wE])
            S.dma("sp", aupE[64 + d * 32:64 + (d + 1) * 32, d, :], I["rw_a_up"][d], w=[t_wE])
        S.dma("sp", gup[:], I["rw_g_up"], w=[t_wE])
        S.dma("sp", zraw[:], g.zT[1536:1664, :], w=[t_zr])
        shift_tile(S, z12, zraw, c0[:, 0:1], cols[:, 0:1], cols[:, 1:2], 128, [t_zr, t_cols, t_c0], [t_12])
        S.act(z12t[:], z12[:], AF.Tanh, r=[t_12], w=[t_12t])
        S.dma("sp", zraw[:96, :], g.zT[1664:1760, :], w=[t_zr])
        shift_tile(S, zlg, zraw, c0[:96, 1:2], cols[:96, 2:3], cols[:96, 3:4], 96, [t_zr, t_cols, t_c0], [t_lg])
        S.act(zlg[:96, :], zlg[:96, :], AF.Sigmoid, r=[t_lg], w=[t_lg])
        npl = 0; nob = 0
        jobs = []
        for tl in range(4):
            for d in range(2):
                jobs.append((wupE[:, d, tl * 128:(tl + 1) * 128], z12t, 128, t_12t, cols[:, 4 + d * 4 + tl:5 + d * 4 + tl], g.sS[d], tl))
                jobs.append((aupE[:, d, tl * 128:(tl + 1) * 128], z12, 128, t_12, cols[:, 12 + d * 4 + tl:13 + d * 4 + tl], g.iclrS[d], tl))
            jobs.append((gup[:, tl * 128:(tl + 1) * 128], zlg, 96, t_lg, None, g.gateT, tl))
        for (lhsT, rhs, K, t_rhs, bias, dst, tl) in jobs:
            o = ob[nob % 2]; to = t_ob[nob % 2]; nob += 1
            for (c0_, n) in CHUNKS:
                p = pl[npl % 3]; tp = t_pl[npl % 3]; npl += 1
                S.mm(p[:, :n], lhsT[:K, :] if K < 128 else lhsT, rhs[:K, c0_:c0_ + n], True, True, r=[t_wE, t_rhs], w=[tp])
                if bias is not None:
                    S.act(o[:, c0_:c0_ + n], p[:, :n], AF.Sigmoid, bias=bias, scale=1.0, r=[tp, t_cols], w=[to])
                else:
                    S.copy("dve", o[:, c0_:c0_ + n], p[:, :n], r=[tp], w=[to])
            S.dma("sp", dst[tl * 128:(tl + 1) * 128, :], o[:], r=[to])
        S.barrier(); S.flush()


def phase_rwkv_prep(g):
    nc, S, I = g.nc, g.S, g.I
    with ExitStack() as st:
        mu0, mu1 = I["rw_mu"][0], I["rw_mu"][1]
        r128 = lambda a: a.rearrange("(j p) -> j p", p=128)
        cols, t_cols = load_cols(g, st, "pcols", [
            r128(mu0[0:1536]), r128(mu1[0:1536]), r128(I["rw_k_k"]), r128(I["rw_k_a"]),
            I["rw_r_k"].rearrange("d (j p) -> (d j) p", p=128)])
        c0 = st.enter_context(nc.sbuf_tensor("pc0", [128, 12], F32))
        omka = st.enter_context(nc.sbuf_tensor("pomka", [128, 4], F32))
        t_c0 = Tok("pc0")
        S.tt("dve", c0[:], cols[:, 0:12], cols[:, 12:24], ALU.add, r=[t_cols], w=[t_c0])
        S.ts("dve", c0[:], c0[:], -1.0, 1.0, ALU.mult, ALU.add, r=[t_c0], w=[t_c0])
        S.ts("dve", omka[:], cols[:, 28:32], -1.0, 1.0, ALU.mult, ALU.add, r=[t_cols], w=[t_c0])
        bones = st.enter_context(nc.sbuf_tensor("bones", [128, 128], F32))
        maskC = st.enter_context(nc.sbuf_tensor("maskC", [128, T], F32))
        t_k = Tok("pconst")
        S.memset("pool", bones[:], 0.0, w=[t_k])
        S.memset("pool", bones[0:64, 0:64], 1.0, w=[t_k])
        S.memset("pool", bones[64:128, 64:128], 1.0, w=[t_k])
        S.memset("pool", maskC[:], 1.0, w=[t_k])
        S.memset("pool", maskC[:].rearrange("p (c j) -> p c j", j=64)[:, :, 0:1], 0.0, w=[t_k])
        names = ["zraw", "r", "k", "v", "kk", "bon", "s", "icl", "kdir", "b", "P", "E", "Q", "Qi", "ex0", "ex1", "o0", "o1"]
        tl_ = {n: st.enter_context(nc.sbuf_tensor("p_" + n, [128, T], F32)) for n in names}
        tk = {n: Tok("p_" + n) for n in names}
        tot = st.enter_context(nc.sbuf_tensor("ptot", [128, NCH], F32))
        wc = st.enter_context(nc.sbuf_tensor("pwc", [128, NCH], F32))
        t_tot = Tok("tot")
        pp = [st.enter_context(nc.psum_tensor("pps%d" % i, [128, 512], F32)) for i in range(2)]
        t_pp = toks(2, "pps", True)
        npp = 0
        nout = 0

        def out_tile(dst, compute):
            nonlocal nout
            o = tl_["o%d" % (nout % 2)]; to = tk["o%d" % (nout % 2)]; nout += 1
            compute(o, to)
            S.dma("sp", dst, o[:], r=[to])

        for tl in range(4):
            rows = slice(tl * 128, (tl + 1) * 128)
            for qi, q in enumerate(("r", "k", "v")):
                S.dma("sp", tl_["zraw"][:], g.zT[qi * 512 + tl * 128:qi * 512 + (tl + 1) * 128, :], w=[tk["zraw"]])
                ci = qi * 4 + tl
                shift_tile(S, tl_[q], tl_["zraw"], c0[:, ci:ci + 1], cols[:, ci:ci + 1], cols[:, 12 + ci:13 + ci], 128,
                           [tk["zraw"], t_cols, t_c0], [tk[q]])
            S.dma("sp", g.vS[rows, :], tl_["v"][:], r=[tk["v"]])
            kk = tl_["kk"]; ex0 = tl_["ex0"]; ex1 = tl_["ex1"]
            S.ts("dve", kk[:], tl_["k"][:], cols[:, 24 + tl:25 + tl], None, ALU.mult, r=[tk["k"], t_cols], w=[tk["kk"]])
            S.act(ex0[:], kk[:], AF.Square, r=[tk["kk"]], w=[tk["ex0"]])
            for (c0_, n) in CHUNKS:
                p = pp[npp % 2]; tp = t_pp[npp % 2]; npp += 1
                S.mm(p[:, :n], bones[:], ex0[:, c0_:c0_ + n], True, True, r=[t_k, tk["ex0"]], w=[tp])
                S.act(ex1[:, c0_:c0_ + n], p[:, :n], AF.Sqrt, r=[tp], w=[tk["ex1"]])
            S.ts("dve", ex1[:], ex1[:], 1e-12, None, ALU.max, r=[tk["ex1"]], w=[tk["ex1"]])
            S.op("dve", lambda e, ex1=ex1: e.reciprocal(out=ex1[:], in_=ex1[:]), r=[tk["ex1"]], w=[tk["ex1"]])
            S.tt("dve", kk[:], kk[:], ex1[:], ALU.mult, r=[tk["kk"], tk["ex1"]], w=[tk["kk"]])
            S.memset("pool", tl_["bon"][:], 0.0, w=[tk["bon"]])
            for d in range(2):
                s, icl, kdir, b = tl_["s"], tl_["icl"], tl_["kdir"], tl_["b"]
                P, E, Q, Qi = tl_["P"], tl_["E"], tl_["Q"], tl_["Qi"]
                S.dma("sp", s[:], g.sS[d][rows, :], w=[tk["s"]])
                S.dma("sp", icl[:], g.iclrS[d][rows, :], w=[tk["icl"]])
                S.ts("dve", kdir[:], icl[:], cols[:, 28 + tl:29 + tl], omka[:, tl:tl + 1], ALU.mult, ALU.add,
                     r=[tk["icl"], t_cols, t_c0], w=[tk["kdir"]])
                S.tt("dve", kdir[:], kdir[:], tl_["k"][:], ALU.mult, r=[tk["kdir"], tk["k"]], w=[tk["kdir"]])
                S.tt("pool", b[:], kk[:], icl[:], ALU.mult, r=[tk["kk"], tk["icl"]], w=[tk["b"]])
                S.stt(ex0[:], tl_["r"][:], cols[:, 32 + d * 4 + tl:33 + d * 4 + tl], kdir[:], ALU.mult, ALU.mult,
                      r=[tk["r"], tk["kdir"], t_cols], w=[tk["ex0"]])
                for (c0_, n) in CHUNKS:
                    p = pp[npp % 2]; tp = t_pp[npp % 2]; npp += 1
                    S.mm(p[:, :n], bones[:], ex0[:, c0_:c0_ + n], True, True, r=[t_k, tk["ex0"]], w=[tp])
                    S.tt("dve", tl_["bon"][:, c0_:c0_ + n], tl_["bon"][:, c0_:c0_ + n], p[:, :n], ALU.add, r=[tp, tk["bon"]], w=[tk["bon"]])
                S.op("dve", lambda e, P=P, s=s: e.tensor_tensor_scan(out=P[:], data0=maskC[:], data1=s[:], initial=0.0,
                                                                   op0=ALU.mult, op1=ALU.add), r=[t_k, tk["s"]], w=[tk["P"]])
                S.copy("dve", tot[:], P[:].rearrange("p (c j) -> p c j", j=64)[:, :, 63], r=[tk["P"]], w=[t_tot])
                S.tt("pool", E[:], P[:], s[:], ALU.subtract, r=[tk["P"], tk["s"]], w=[tk["E"]])
                v3 = lambda a: a[:].rearrange("p (c j) -> p c j", j=64)
                S.tt("dve", v3(Q), tot[:].unsqueeze(2).to_broadcast([128, NCH, 64]), v3(P), ALU.subtract, r=[tk["P"], t_tot], w=[tk["Q"]])
                S.tt("pool", Qi[:], Q[:], s[:], ALU.add, r=[tk["Q"], tk["s"]], w=[tk["Qi"]])
                S.act(wc[:], tot[:], AF.Exp, scale=-KAPPA, r=[t_tot], w=[t_tot])
                S.dma("sp", g.WCdd[d][rows, :], wc[:], r=[t_tot])
                lr, la, ln, lc = (P, E, P, Q) if d == 0 else (Qi, Q, Qi, E)
                tn = {id(P): "P", id(E): "E", id(Q): "Q", id(Qi): "Qi"}
                S.act(ex0[:], lr[:], AF.Exp, scale=-KAPPA, r=[tk[tn[id(lr)]]], w=[tk["ex0"]])
                out_tile(g.PR[d][0][rows, :], lambda o, to: S.tt("dve", o[:], tl_["r"][:], ex0[:], ALU.mult, r=[tk["r"], tk["ex0"]], w=[to]))
                S.act(ex1[:], la[:], AF.Exp, scale=-KAPPA, r=[tk[tn[id(la)]]], w=[tk["ex1"]])
                out_tile(g.PR[d][1][rows, :], lambda o, to: S.stt(o[:], kk[:], -1.0, ex1[:], ALU.mult, ALU.mult, r=[tk["kk"], tk["ex1"]], w=[to]))
                S.act(ex0[:], ln[:], AF.Exp, scale=KAPPA, r=[tk[tn[id(ln)]]], w=[tk["ex0"]])
                out_tile(g.PR[d][2][rows, :], lambda o, to: S.tt("dve", o[:], b[:], ex0[:], ALU.mult, r=[tk["b"], tk["ex0"]], w=[to]))
                out_tile(g.PR[d][3][rows, :], lambda o, to: S.tt("pool", o[:], kdir[:], ex0[:], ALU.mult, r=[tk["kdir"], tk["ex0"]], w=[to]))
                S.act(ex1[:], lc[:], AF.Exp, scale=-KAPPA, r=[tk[tn[id(lc)]]], w=[tk["ex1"]])
                out_tile(g.PR[d][4][rows, :], lambda o, to: S.tt("dve", o[:], b[:], ex1[:], ALU.mult, r=[tk["b"], tk["ex1"]], w=[to]))
                out_tile(g.PR[d][5][rows, :], lambda o, to: S.tt("pool", o[:], kdir[:], ex1[:], ALU.mult, r=[tk["kdir"], tk["ex1"]], w=[to]))
            out_tile(g.bonusT[rows, :], lambda o, to: S.tt("dve", o[:], tl_["bon"][:], tl_["v"][:], ALU.mult, r=[tk["bon"], tk["v"]], w=[to]))
        S.barrier(); S.flush()


def rwkv_chunk_order(d):
    if d == 0:
        return list(range(NCH))
    return [3, 2, 1, 0] + list(range(NCH - 1, 3, -1))


def phase_rwkv_scan(g):
    nc, S, I = g.nc, g.S, g.I
    G = int(os.environ.get("RW_G", "3"))
    with ExitStack() as st:
        sb = lambda name, shape, dt=F32: st.enter_context(nc.sbuf_tensor(name, shape, dt))
        id64 = g.ident[0:64, 0:64]
        mask = [sb("rmask%d" % d, [64, 320]) for d in range(2)]
        t_k = Tok("rconst")
        for d in range(2):
            S.memset("pool", mask[d][:], 1.0, w=[t_k])
            for blk, strict in ((0, True), (1, False), (2, True), (3, False)):
                sgn = 1 if d == 0 else -1
                S.op("pool", lambda e, d=d, blk=blk, strict=strict, sgn=sgn: e.affine_select(
                    out=mask[d][:, blk * 64:(blk + 1) * 64], in_=mask[d][:, blk * 64:(blk + 1) * 64],
                    compare_op=ALU.is_ge, fill=0.0, base=-(1 if strict else 0), pattern=[[sgn, 64]], channel_multiplier=-sgn),
                    r=[t_k], w=[t_k])
            sgn = -1 if d == 0 else 1
            S.op("pool", lambda e, d=d, sgn=sgn: e.affine_select(
                out=mask[d][:, 256:320], in_=mask[d][:, 256:320], compare_op=ALU.is_ge, fill=0.0, base=-1,
                pattern=[[sgn, 64]], channel_multiplier=-sgn), r=[t_k], w=[t_k])
        fmraw = [sb("rfm%d" % q, [128, 2 * T]) for q in range(7)]
        fm = [b[0:64, :].rearrange("p (h t) -> p h t", h=2) for b in fmraw]
        t_fm = toks(7, "rfm")
        wc2 = sb("rwc2", [64, 2, NCH]); t_wc = Tok("wc2")
        yacc = sb("ryacc", [64, NCH, 128]); t_y = toks(NCH, "yacc")
        bon = fmraw[0][:, 0:T]; gat = fmraw[1][:, 0:T]; ofm = fmraw[2][:, 0:T]
        slots = []
        for s_ in range(G):
            d_ = {}
            d_["A"] = sb("rA%d" % s_, [64, 2, 320])
            for nm in ("tm", "T0", "T1", "P0", "P1", "Q0", "Q1", "AbT", "X0", "U0", "Rb", "Y0", "GT", "ZT"):
                d_[nm] = sb("r%s%d" % (nm, s_), [64, 2, 64])
            d_["tk"] = {nm: Tok(nm + str(s_), nm.startswith("ps")) for nm in ("A", "tm", "T0", "T1", "P0", "P1", "Q0", "Q1", "AbT", "X0", "U0", "Rb", "Y0", "GT", "ZT", "ps0", "ps1")}
            d_["ps"] = [st.enter_context(nc.psum_tensor("rps%d_%d" % (s_, i), [64, 512], F32)) for i in range(2)]
            slots.append(d_)
        ptr = st.enter_context(nc.psum_tensor("rptr", [128, 512], F32)); t_ptr = Tok("ptr", True)
        gcols, t_gc = load_cols(g, st, "gncols", [I["rw_gn_w"].rearrange("(j p) -> j p", p=128), I["rw_gn_b"].rearrange("(j p) -> j p", p=128)],
                                ps=ptr[:, 0:128], t_p=t_ptr)
        prc = st.enter_context(nc.psum_tensor("rprc", [64, 512], F32)); t_prc = Tok("prc", True)
        ST = [sb("rST%d" % i, [64, 2, 64]) for i in range(2)]; t_ST = toks(2, "ST")

        def chunk_gen(sl, d, c, state):
            tk = sl["tk"]; ps = sl["ps"]
            cs = slice(c * 64, (c + 1) * 64)
            Rt, At, Bt, Kt, Bh, Kh, V = fm
            tR, tA, tB, tK, tBh, tKh, tV = t_fm
            A = sl["A"]
            tmT = {}
            for nm, src, tsrc in (("AtT", At, tA), ("BhT", Bh, tBh), ("KhT", Kh, tKh), ("vT", V, tV)):
                pass
            bufs = {"AtT": sl["tm"], "BhT": sl["P1"], "KhT": sl["Q1"], "vT": sl["T1"]}
            btok = {"AtT": tk["tm"], "BhT": tk["P1"], "KhT": tk["Q1"], "vT": tk["T1"]}
            for i, (nm, src, tsrc) in enumerate((("AtT", At, tA), ("BhT", Bh, tBh), ("KhT", Kh, tKh), ("vT", V, tV))):
                if os.environ.get("RW_SKIP_TR"):
                    break
                for hh in range(2):
                    S.tr(ptr[0:64, i * 128 + hh * 64:i * 128 + (hh + 1) * 64], src[:, hh, cs], id64, r=[tsrc, g.t_const], w=[t_ptr])
            for i, nm in enumerate(("AtT", "BhT", "KhT", "vT")):
                if os.environ.get("RW_SKIP_TR"):
                    break
                S.copy("act" if i % 2 else "dve", bufs[nm][:].rearrange("p a b -> p (a b)"), ptr[0:64, i * 128:(i + 1) * 128], r=[t_ptr], w=[btok[nm]])
            AtT, BhT, KhT, vT = bufs["AtT"], bufs["BhT"], bufs["KhT"], bufs["vT"]
            tAtT, tBhT, tKhT, tvT = btok["AtT"], btok["BhT"], btok["KhT"], btok["vT"]
            for hh in range(2):
                if os.environ.get("RW_SKIP_A"):
                    break
                ph = slice(hh * 64, (hh + 1) * 64)
                S.mm(ps[hh][:, 0:64], Bt[:, hh, cs], At[:, hh, cs], True, True, r=[tB, tA], w=[tk["ps%d" % hh]])
                S.mm(ps[hh][:, 64:128], Bt[:, hh, cs], Rt[:, hh, cs], True, True, r=[tB, tR], w=[tk["ps%d" % hh]])
                S.mm(ps[hh][:, 128:192], Kt[:, hh, cs], At[:, hh, cs], True, True, r=[tK, tA], w=[tk["ps%d" % hh]])
                S.mm(ps[hh][:, 192:256], Kt[:, hh, cs], Rt[:, hh, cs], True, True, r=[tK, tR], w=[tk["ps%d" % hh]])
                S.mm(ps[hh][:, 256:320], At[:, hh, cs], Bt[:, hh, cs], True, True, r=[tB, tA], w=[tk["ps%d" % hh]])
            for hh in range(2):
                S.tt("dve", A[:, hh, :], ps[hh][:, 0:320], mask[d][:], ALU.mult, r=[tk["ps%d" % hh], t_k], w=[tk["A"]])
                if os.environ.get("RW_FENCE"):
                    S.memset("dve", sl["GT"][0:1, 0, 0:1], 0.0, r=[tk["ps%d" % hh]], w=[tk["GT"]])
            yield
            Tc, tTc = sl["T0"], tk["T0"]
            for hh in range(2):
                S.tt("pool", Tc[:, hh, :], A[:, hh, 0:64], id64, ALU.add, r=[tk["A"], g.t_const], w=[tTc])
            Pc = (A, 0); PTc = (A, 256)
            tPc = tk["A"]; tPTc = tk["A"]
            pbuf = [(sl["P0"], tk["P0"]), (sl["Q0"], tk["Q0"])]
            pairs = [((sl["P0"], tk["P0"]), (sl["Q0"], tk["Q0"])), ((sl["AbT"], tk["AbT"]), (sl["X0"], tk["X0"]))]
            Tbufs = [(sl["T0"], tk["T0"]), (sl["U0"], tk["U0"])]
            ti = 0
            for j in range(1, 6):
                (Pn, tPn), (PTn, tPTn) = pairs[j % 2]
                def ap(x, hh):
                    t_, off = x
                    return t_[:, hh, off:off + 64]
                for hh in range(2):
                    if j < 5:
                        S.mm(ps[0][:, hh * 64:(hh + 1) * 64], ap(PTc, hh), ap(Pc, hh), True, True, r=[tPc, tPTc], w=[tk["ps0"]])
                    S.mm(ps[1][:, hh * 64:(hh + 1) * 64], ap(Pc, hh), ap(PTc, hh), True, True, r=[tPc, tPTc], w=[tk["ps1"]])
                if j < 5:
                    S.copy("dve", Pn[:].rearrange("p a b -> p (a b)"), ps[0][:, 0:128], r=[tk["ps0"]], w=[tPn])
                S.copy("act", PTn[:].rearrange("p a b -> p (a b)"), ps[1][:, 0:128], r=[tk["ps1"]], w=[tPTn])
                Pc, PTc, tPc, tPTc = (Pn, 0), (PTn, 0), tPn, tPTn
                yield
                Tcur, tTcur = Tbufs[ti]; Tnxt, tTnxt = Tbufs[1 - ti]
                for hh in range(2):
                    S.mm(ps[0][:, 128 + hh * 64:128 + (hh + 1) * 64], PTn[:, hh, :], Tcur[:, hh, :], True, True, r=[tPTn, tTcur], w=[tk["ps0"]])
                S.tt("dve", Tnxt[:].rearrange("p a b -> p (a b)"), ps[0][:, 128:256], Tcur[:].rearrange("p a b -> p (a b)"), ALU.add,
                     r=[tk["ps0"], tTcur], w=[tTnxt])
                ti = 1 - ti
                yield
            Tinv, tTinv = Tbufs[ti]
            AbT, tAbT = sl["P0"], tk["P0"]
            X0, tX0 = sl["Q0"], tk["Q0"]
            for hh in range(2):
                hc = slice(hh * 64, (hh + 1) * 64)
                S.mm(ps[0][:, 256 + hh * 64:256 + (hh + 1) * 64], Tinv[:, hh, :], AtT[:, hh, :], True, True, r=[tTinv, tAtT], w=[tk["ps0"]])
                S.mm(ps[1][:, 256 + hh * 64:256 + (hh + 1) * 64], A[:, hh, 128:192], vT[:, hh, :], True, True, r=[tk["A"], tvT], w=[tk["ps1"]])
            S.copy("dve", AbT[:].rearrange("p a b -> p (a b)"), ps[0][:, 256:384], r=[tk["ps0"]], w=[tAbT])
            S.copy("act", X0[:].rearrange("p a b -> p (a b)"), ps[1][:, 256:384], r=[tk["ps1"]], w=[tX0])
            for hh in range(2):
                S.mm(ps[0][:, hh * 64:(hh + 1) * 64], A[:, hh, 192:256], vT[:, hh, :], True, True, r=[tk["A"], tvT], w=[tk["ps0"]])
                S.mm(ps[1][:, hh * 64:(hh + 1) * 64], KhT[:, hh, :], vT[:, hh, :], True, True, r=[tKhT, tvT], w=[tk["ps1"]])
            S.copy("dve", sl["Y0"][:].rearrange("p a b -> p (a b)"), ps[0][:, 0:128], r=[tk["ps0"]], w=[tk["Y0"]])
            S.copy("act", sl["ZT"][:].rearrange("p a b -> p (a b)"), ps[1][:, 0:128], r=[tk["ps1"]], w=[tk["ZT"]])
            yield
            U0, tU0 = sl["AbT"], tk["AbT"]
            for hh in range(2):
                ph = slice(hh * 64, (hh + 1) * 64)
                S.mm(ps[0][:, 384 + hh * 64:384 + (hh + 1) * 64], Tinv[:, hh, :], X0[:, hh, :], True, True, r=[tTinv, tX0], w=[tk["ps0"]])
                S.mm(ps[1][:, 384 + hh * 64:384 + (hh + 1) * 64], AbT[:, hh, :], A[:, hh, 64:128], True, True, r=[tAbT, tk["A"]], w=[tk["ps1"]])
            S.copy("dve", U0[:].rearrange("p a b -> p (a b)"), ps[0][:, 384:512], r=[tk["ps0"]], w=[tU0])
            S.tt("dve", sl["Rb"][:], ps[1][:, 384:512].rearrange("p (a b) -> p a b", a=2), Rt[:, :, cs], ALU.add, r=[tk["ps1"], tR], w=[tk["Rb"]])
            yield
            for hh in range(2):
                o0 = hh * 64
                S.mm(ps[0][:, o0:o0 + 64], A[:, hh, 64:128], U0[:, hh, :], True, True, r=[tk["A"], tU0], w=[tk["ps0"]])
                S.mm(ps[1][:, o0:o0 + 64], AbT[:, hh, :], BhT[:, hh, :], True, True, r=[tAbT, tBhT], w=[tk["ps1"]])
                S.mm(ps[1][:, 128 + o0:128 + o0 + 64], BhT[:, hh, :], U0[:, hh, :], True, True, r=[tBhT, tU0], w=[tk["ps1"]])
            S.tt("dve", sl["Y0"][:].rearrange("p a b -> p (a b)"), ps[0][:, 0:128], sl["Y0"][:].rearrange("p a b -> p (a b)"), ALU.add,
                 r=[tk["ps0"], tk["Y0"]], w=[tk["Y0"]])
            for hh in range(2):
                S.stt(sl["GT"][:, hh, :], id64, wc2[:, hh, c:c + 1], ps[1][:, hh * 64:(hh + 1) * 64], ALU.mult, ALU.add,
                      r=[g.t_const, t_wc, tk["ps1"]], w=[tk["GT"]])
            S.tt("dve", sl["ZT"][:].rearrange("p a b -> p (a b)"), ps[1][:, 128:256], sl["ZT"][:].rearrange("p a b -> p (a b)"), ALU.add,
                 r=[tk["ps1"], tk["ZT"]], w=[tk["ZT"]])
            yield
            si = state["i"]
            Sc, tSc = ST[si], t_ST[si]; Sn, tSn = ST[1 - si], t_ST[1 - si]
            for hh in range(2):
                o0 = hh * 64
                S.mm(prc[:, o0:o0 + 64], sl["Rb"][:, hh, :], Sc[:, hh, :], True, True, r=[tk["Rb"], tSc], w=[t_prc])
                S.mm(prc[:, 128 + o0:128 + o0 + 64], sl["GT"][:, hh, :], Sc[:, hh, :], True, True, r=[tk["GT"], tSc], w=[t_prc])
            if d == 0:
                S.tt("dve", yacc[:, c, :], prc[:, 0:128], sl["Y0"][:].rearrange("p a b -> p (a b)"), ALU.add, r=[t_prc, tk["Y0"]], w=[t_y[c]])
            else:
                S.tt("dve", sl["Y0"][:].rearrange("p a b -> p (a b)"), prc[:, 0:128], sl["Y0"][:].rearrange("p a b -> p (a b)"), ALU.add,
                     r=[t_prc, tk["Y0"]], w=[tk["Y0"]])
                S.tt("pool", yacc[:, c, :], yacc[:, c, :], sl["Y0"][:].rearrange("p a b -> p (a b)"), ALU.add, r=[tk["Y0"], t_y[c]], w=[t_y[c]])
            S.tt("dve", Sn[:].rearrange("p a b -> p (a b)"), prc[:, 128:256], sl["ZT"][:].rearrange("p a b -> p (a b)"), ALU.add,
                 r=[t_prc, tk["ZT"]], w=[tSn])
            state["i"] = 1 - si
            yield

        DBG_NTL = int(os.environ.get("RW_TL", "4")); DBG_ND = int(os.environ.get("RW_D", "2"))
        DBG_NCH = int(os.environ.get("RW_NCH", "99")); DBG_STG = int(os.environ.get("RW_STG", "99"))
        for tl in range(DBG_NTL):
            rows = slice(tl * 128, (tl + 1) * 128)
            for d in range(DBG_ND):
                for q in range(6):
                    S.dma("sp" if q % 2 else "act", fm[q], g.PR[d][q][rows, :].rearrange("(h k) t -> k h t", h=2), w=[t_fm[q]])
                S.dma("sp", fm[6], g.vS[rows, :].rearrange("(h k) t -> k h t", h=2), w=[t_fm[6]])
                S.dma("sp", wc2[:], g.WCdd[d][rows, :].rearrange("(hh k) c -> k hh c", hh=2), w=[t_wc])
                state = {"i": 0}
                S.memset("pool", ST[0][:], 0.0, w=[t_ST[0]])
                order = rwkv_chunk_order(d)[:DBG_NCH]
                if os.environ.get('RW_SAME'):
                    order = [int(x) for x in os.environ['RW_SAME'].split(',')]
                gens = []
                nxt = 0
                slot_free = list(range(G))
                active = []
                while nxt < len(order) or active:
                    if nxt < len(order) and slot_free:
                        s_ = slot_free.pop(0)
                        import itertools
                        active.append((itertools.islice(chunk_gen(slots[s_], d, order[nxt], state), DBG_STG), s_))
                        nxt += 1
                    still = []
                    for gen, s_ in active:
                        try:
                            next(gen)
                            still.append((gen, s_))
                        except StopIteration:
                            slot_free.append(s_)
                    active = still
            S.dma("sp", bon, g.bonusT[rows, :], w=[t_fm[0]])
            S.dma("act", gat, g.gateT[rows, :], w=[t_fm[1]])
            stat = sb("rstat%d" % tl, [64, NCH * 2, 4]); t_st = Tok("stat")
            sqb = sb("rsq%d" % tl, [64, 128]); t_sq = Tok("sq")
            y4 = yacc[:].rearrange("p c (h v) -> p (c h) v", h=2)
            for c in range(NCH):
                S.reduce(stat[:, 2 * c:2 * c + 2, 0], y4[:, 2 * c:2 * c + 2, :], ALU.add, r=[t_y[c]], w=[t_st])
                S.act(sqb[:], yacc[:, c, :], AF.Square, r=[t_y[c]], w=[t_sq])
                S.reduce(stat[:, 2 * c:2 * c + 2, 1], sqb[:].rearrange("p (h v) -> p h v", h=2), ALU.add, r=[t_sq], w=[t_st])
            S.ts("dve", stat[:, :, 0], stat[:, :, 0], 1.0 / 64, None, ALU.mult, r=[t_st], w=[t_st])
            S.tt("dve", stat[:, :, 2], stat[:, :, 0], stat[:, :, 0], ALU.mult, r=[t_st], w=[t_st])
            S.stt(stat[:, :, 3], stat[:, :, 1], 1.0 / 64, stat[:, :, 2], ALU.mult, ALU.subtract, r=[t_st], w=[t_st])
            S.ts("dve", stat[:, :, 3], stat[:, :, 3], 64e-5, None, ALU.add, r=[t_st], w=[t_st])
            S.act(stat[:, :, 3], stat[:, :, 3], AF.Sqrt, r=[t_st], w=[t_st])
            S.recip(stat[:, :, 3], stat[:, :, 3], r=[t_st], w=[t_st])
            for c in range(NCH):
                for hh in range(2):
                    j = 2 * c + hh
                    S.ts("dve" if hh else "pool", yacc[:, c, hh * 64:(hh + 1) * 64], yacc[:, c, hh * 64:(hh + 1) * 64],
                         stat[:, j, 0:1], stat[:, j, 3:4], ALU.subtract, ALU.mult, r=[t_y[c], t_st], w=[t_y[c]])
            for c8 in range(0, NCH, 8):
                nn = min(8, NCH - c8)
                for c in range(c8, c8 + nn):
                    S.tr(ptr[:, (c - c8) * 64:(c - c8 + 1) * 64], yacc[:, c, :], id64, r=[t_y[c], g.t_const], w=[t_ptr])
                cs8 = slice(c8 * 64, (c8 + nn) * 64)
                S.ts("dve", ofm[:, cs8], ptr[:, 0:nn * 64], gcols[:, tl:tl + 1], gcols[:, 4 + tl:5 + tl], ALU.mult, ALU.add,
                     r=[t_ptr, t_gc], w=[t_fm[2]])
            S.tt("pool", ofm, ofm, bon, ALU.add, r=[t_fm[2], t_fm[0]], w=[t_fm[2]])
            S.tt("dve", ofm, ofm, gat, ALU.mult, r=[t_fm[2], t_fm[1]], w=[t_fm[2]])
            S.dma("sp", g.catT[rows, :], ofm, r=[t_fm[2]])
            if "yrw_o" in g.dbg:
                pass
        S.barrier(); S.flush()


MB0 = RW_COLS
NC2 = T // 128
NEG = -30000.0


def ssd_chunk_order(d):
    if d == 0:
        return list(range(NC2))
    return [1, 0] + list(range(NC2 - 1, 1, -1))


def alloc_ssd_scratch(g):
    nc = g.nc
    def dscr(name, shape):
        kind = {"kind": "ExternalOutput"} if name in g.dbg else {}
        return nc.dram_tensor(name, list(shape), F32, **kind).ap()
    g.xbcS = dscr("xbcS", (1024, T))
    g.ssd16 = dscr("ssd16", (4, 16, T))


def phase_ssd_prep(g):
    nc, S, I = g.nc, g.S, g.I
    with ExitStack() as st:
        sb = lambda name, shape, dt=F32: st.enter_context(nc.sbuf_tensor(name, shape, dt))
        cols, t_cols = load_cols(g, st, "ccols", [I["mb_conv_w"].rearrange("j (t p) -> (j t) p", p=128),
                                                  I["mb_conv_b"].rearrange("(t p) -> t p", p=128)])
        c16, t_c16 = load_cols(g, st, "c16", [I["mb_dt_bias"].rearrange("(o p) -> o p", o=1), I["mb_a_log"].rearrange("(o p) -> o p", o=1)])
        zin = [sb("szin%d" % i, [128, T]) for i in range(2)]; t_zin = toks(2, "szin")
        acc = [sb("sacc%d" % i, [128, T]) for i in range(2)]; t_acc = toks(2, "sacc")
        for tl in range(8):
            b = tl % 2
            S.dma("sp" if b else "act", zin[b][:], g.zT[MB0 + 512 + tl * 128:MB0 + 512 + (tl + 1) * 128, :], w=[t_zin[b]])
            S.act(acc[b][:], zin[b][:], AF.Identity, bias=cols[:, 40 + tl:41 + tl], scale=cols[:, 16 + tl:17 + tl],
                  r=[t_zin[b], t_cols], w=[t_acc[b]])
            for j in (0, 1, 3, 4):
                sh = j - 2
                for a, e_ in SEGS:
                    if sh < 0:
                        o_sl, i_sl = slice(a - sh, e_), slice(a, e_ + sh)
                    else:
                        o_sl, i_sl = slice(a, e_ - sh), slice(a + sh, e_)
                    S.stt(acc[b][:, o_sl], zin[b][:, i_sl], cols[:, j * 8 + tl:j * 8 + tl + 1], acc[b][:, o_sl], ALU.mult, ALU.add,
                          r=[t_zin[b], t_cols, t_acc[b]], w=[t_acc[b]])
            S.act(acc[b][:], acc[b][:], AF.Silu, r=[t_acc[b]], w=[t_acc[b]])
            S.dma("sp", g.xbcS[tl * 128:(tl + 1) * 128, :], acc[b][:], r=[t_acc[b]])
        x16 = sb("sx16", [16, T]); ax = sb("sax", [16, T]); dt16 = sb("sdt16", [16, T]); A16 = sb("sA16", [16, T])
        P16 = sb("sP16", [16, T]); Q16 = sb("sQ16", [16, T]); m128 = sb("sm128", [16, T]); tot = sb("stot", [16, NC2])
        aneg = sb("saneg", [16, 1])
        t16 = Tok("s16")
        S.dma("sp", x16[:], g.zT[MB0 + 1536:MB0 + 1552, :], w=[t16])
        S.act(aneg[:], c16[0:16, 1:2], AF.Exp, r=[t_c16], w=[t16])
        S.ts("dve", aneg[:], aneg[:], -1.0, None, ALU.mult, r=[t16], w=[t16])
        S.ts("dve", x16[:], x16[:], c16[0:16, 0:1], None, ALU.add, r=[t16, t_c16], w=[t16])
        S.ts("dve", ax[:], x16[:], -1.0, None, ALU.mult, r=[t16], w=[t16])
        S.tt("dve", ax[:], ax[:], x16[:], ALU.min, r=[t16], w=[t16])
        S.act(ax[:], ax[:], AF.Exp, r=[t16], w=[t16])
        S.act(ax[:], ax[:], AF.Ln, bias=1.0, scale=1.0, r=[t16], w=[t16])
        S.ts("dve", x16[:], x16[:], 0.0, None, ALU.max, r=[t16], w=[t16])
        S.tt("dve", dt16[:], x16[:], ax[:], ALU.add, r=[t16], w=[t16])
        S.ts("dve", A16[:], dt16[:], aneg[:, 0:1], None, ALU.mult, r=[t16], w=[t16])
        S.memset("pool", m128[:], 1.0, w=[t16])
        S.memset("pool", m128[:].rearrange("p (c j) -> p c j", j=128)[:, :, 0:1], 0.0, w=[t16])
        S.op("dve", lambda e: e.tensor_tensor_scan(out=P16[:], data0=m128[:], data1=A16[:], initial=0.0, op0=ALU.mult, op1=ALU.add),
             r=[t16], w=[t16])
        S.copy("dve", tot[:], P16[:].rearrange("p (c j) -> p c j", j=128)[:, :, 127], r=[t16], w=[t16])
        v3 = lambda a: a[:].rearrange("p (c j) -> p c j", j=128)
        S.tt("dve", v3(Q16), tot[:].unsqueeze(2).to_broadcast([16, NC2, 128]), v3(P16), ALU.subtract, r=[t16], w=[t16])
        S.tt("dve", Q16[:], Q16[:], A16[:], ALU.add, r=[t16], w=[t16])
        S.dma("sp", g.ssd16[0], dt16[:], r=[t16])
        S.dma("sp", g.ssd16[1], P16[:], r=[t16])
        S.dma("sp", g.ssd16[2], Q16[:], r=[t16])
        S.barrier(); S.flush()


def phase_ssd_scan(g):
    nc, S, I = g.nc, g.S, g.I
    with ExitStack() as st:
        sb = lambda name, shape, dt=F32: st.enter_context(nc.sbuf_tensor(name, shape, dt))
        pst = lambda name, shape: st.enter_context(nc.psum_tensor(name, shape, F32))
        x_tok = sb("mxtok", [128, NC2, 512]); t_xt = Tok("xtok")
        yacc = sb("myacc", [128, NC2, 512]); t_y = toks(NC2, "my")
        Bfm = sb("mBfm", [128, 2, T]); Cfm = sb("mCfm", [128, 2, T]); t_bc = Tok("bcfm")
        cum16 = sb("mcum16", [16, T]); t_cum = Tok("cum16")
        colT = sb("mcolT", [128, 3, NC2, 16]); t_colT = Tok("colT")
        Sel = sb("mSel", [16, 16, 128]); Mneg = sb("mMneg", [128, 2, 128]); Dbc = sb("mDbc", [128, 8]); t_k = Tok("mconst")
        psT = pst("mpsT", [128, 512]); t_psT = Tok("mpsT", True)
        psP = [pst("mpsP%d" % i, [128, 512]) for i in range(2)]; t_psP = toks(2, "mpsP", True)
        psG = pst("mpsG", [128, 512]); t_psG = Tok("mpsG", True)
        psY = pst("mpsY", [128, 512]); t_psY = Tok("mpsY", True)
        psS = pst("mpsS", [128, 512]); t_psS = Tok("mpsS", True)
        ncols, t_nc = load_cols(g, st, "mncols", [I["mb_norm_w"].rearrange("(j p) -> j p", p=128)], ps=psT[:, 0:128], t_p=t_psT)
        S.copy("dve", Sel[:], g.ident[0:16, 0:16].unsqueeze(2).to_broadcast([16, 16, 128]), r=[g.t_const], w=[t_k])
        S.memset("pool", Mneg[:], 0.0, w=[t_k])
        S.op("pool", lambda e: e.affine_select(out=Mneg[:, 0, :], in_=Mneg[:, 0, :], compare_op=ALU.is_ge, fill=NEG, base=0,
                                                pattern=[[1, 128]], channel_multiplier=-1), r=[t_k], w=[t_k])
        S.op("pool", lambda e: e.affine_select(out=Mneg[:, 1, :], in_=Mneg[:, 1, :], compare_op=ALU.is_ge, fill=NEG, base=0,
                                                pattern=[[-1, 128]], channel_multiplier=1), r=[t_k], w=[t_k])
        drow = sb("mdrow", [1, 8])
        S.dma("sp", drow[:], I["mb_d"].rearrange("(o h) -> o h", o=1), w=[t_k])
        S.mm(psT[:, 0:8], g.ones[0:1, :], drow[:], True, True, r=[t_k, g.t_const], w=[t_psT])
        S.copy("dve", Dbc[:], psT[:, 0:8], r=[t_psT], w=[t_k])
        S.dma("sp", Bfm[:], g.xbcS[512:768, :].rearrange("(g p) t -> p g t", p=128), w=[t_bc])
        S.dma("act", Cfm[:], g.xbcS[768:1024, :].rearrange("(g p) t -> p g t", p=128), w=[t_bc])
        S.dma("sp", cum16[0:8, :], g.ssd16[1][0:8, :], w=[t_cum])
        S.dma("sp", cum16[8:16, :], g.ssd16[2][8:16, :], w=[t_cum])
        with ExitStack() as st2:
            sb2 = lambda name, shape: st2.enter_context(nc.sbuf_tensor(name, shape, F32))
            xfm = sb2("mxfm", [128, T]); t_xfm = Tok("xfm")
            dt16 = sb2("mdt16", [16, T]); e16 = sb2("me16", [16, T]); t_16 = Tok("m16")
            tot = sb2("mtot", [16, NC2]); dm = sb2("mdm", [16, 2])
            for tl in range(4):
                S.dma("sp", xfm[:], g.xbcS[tl * 128:(tl + 1) * 128, :], w=[t_xfm])
                for c4 in range(0, NC2, 4):
                    nn = min(4, NC2 - c4)
                    for c in range(c4, c4 + nn):
                        S.tr(psT[:, (c - c4) * 128:(c - c4 + 1) * 128], xfm[:, c * 128:(c + 1) * 128], g.ident[:], r=[t_xfm, g.t_const], w=[t_psT])
                    S.copy("act" if (c4 // 4) % 2 else "dve", x_tok[:, c4:c4 + nn, tl * 128:(tl + 1) * 128],
                           psT[:, 0:nn * 128].rearrange("p (c k) -> p c k", k=128), r=[t_psT], w=[t_xt])
            S.dma("sp", dt16[:], g.ssd16[0], w=[t_16])
            S.memset("pool", dm[:], 1.0, w=[t_16])
            S.op("pool", lambda e: e.affine_select(out=dm[:, 0:1], in_=dm[:, 0:1], compare_op=ALU.is_ge, fill=0.0, base=7,
                                                    pattern=[[0, 1]], channel_multiplier=-1), r=[t_16], w=[t_16])
            S.op("pool", lambda e: e.affine_select(out=dm[:, 1:2], in_=dm[:, 1:2], compare_op=ALU.is_ge, fill=0.0, base=-8,
                                                    pattern=[[0, 1]], channel_multiplier=1), r=[t_16], w=[t_16])
            c3 = cum16[:].rearrange("p (c j) -> p c j", j=128)
            S.ts("dve", tot[:], c3[:, :, 127], dm[:, 0:1], None, ALU.mult, r=[t_cum, t_16], w=[t_16])
            S.stt(tot[:], c3[:, :, 0], dm[:, 1:2], tot[:], ALU.mult, ALU.add, r=[t_cum, t_16], w=[t_16])
            S.tt("dve", e16[:].rearrange("p (c j) -> p c j", j=128), tot[:].unsqueeze(2).to_broadcast([16, NC2, 128]), c3, ALU.subtract,
                 r=[t_cum, t_16], w=[t_16])
            S.act(e16[:], e16[:], AF.Exp, r=[t_16], w=[t_16])
            for qi, (src, tsrc) in enumerate(((dt16, t_16), (cum16, t_cum), (e16, t_16))):
                for c in range(NC2):
                    S.tr(psT[:, c * 16:(c + 1) * 16], src[:, c * 128:(c + 1) * 128], g.ident[0:16, 0:16], r=[tsrc, g.t_const], w=[t_psT])
                S.copy("dve", colT[:, qi, :, :], psT[:, 0:NC2 * 16].rearrange("p (c j) -> p c j", j=16), r=[t_psT], w=[t_colT])
            S.barrier(); S.flush()
        E = sb("mE", [128, 8, 128]); eP = sb("meP", [128, 8, 128]); M = sb("mM", [128, 8, 128]); Ct = sb("mCt", [128, 8, 128])
        t_E, t_eP, t_M, t_Ct = Tok("E"), Tok("eP"), Tok("M"), Tok("Ct")
        Xd = [sb("mXd%d" % i, [128, 8, 64]) for i in range(2)]; Xdd = [sb("mXdd%d" % i, [128, 8, 64]) for i in range(2)]
        Gsb = [sb("mG%d" % i, [128, 2, 128]) for i in range(2)]; Btok = [sb("mBt%d" % i, [128, 2, 128]) for i in range(2)]
        t_Xd, t_Xdd, t_G, t_Bt = toks(2, "Xd"), toks(2, "Xdd"), toks(2, "G"), toks(2, "Bt")
        ST = [sb("mST%d" % i, [128, 8, 64]) for i in range(2)]; t_ST = toks(2, "mST")
        tmpS = sb("mtmpS", [128, 8, 64]); t_tmpS = Tok("tmpS")
        n = 0
        for d in range(2):
            si = 0
            S.memset("pool", ST[0][:], 0.0, w=[t_ST[0]])
            hs = slice(d * 8, (d + 1) * 8)
            for c in ssd_chunk_order(d):
                b = n % 2; n += 1
                cs = slice(c * 128, (c + 1) * 128)
                for h in range(8):
                    S.mm(psP[h // 4][:, (h % 4) * 128:(h % 4 + 1) * 128], Sel[:, d * 8 + h, :], cum16[:, cs], True, True,
                         r=[t_k, t_cum], w=[t_psP[h // 4]])
                for g_ in range(2):
                    S.mm(psG[:, g_ * 128:(g_ + 1) * 128], Bfm[:, g_, cs], Cfm[:, g_, cs], True, True, r=[t_bc], w=[t_psG])
                    S.tr(psG[:, 256 + g_ * 128:256 + (g_ + 1) * 128], Bfm[:, g_, cs], g.ident[:], r=[t_bc, g.t_const], w=[t_psG])
                S.copy("act", Gsb[b][:].rearrange("p a b -> p (a b)"), psG[:, 0:256], r=[t_psG], w=[t_G[b]])
                S.copy("act", Btok[b][:].rearrange("p a b -> p (a b)"), psG[:, 256:512], r=[t_psG], w=[t_Bt[b]])
                for hb in range(2):
                    h4 = slice(hb * 4, hb * 4 + 4)
                    pv = psP[hb][:].rearrange("p (h t) -> p h t", t=128)
                    S.tt("dve", E[:, h4, :], pv, Mneg[:, d, :].unsqueeze(1).to_broadcast([128, 4, 128]), ALU.add, r=[t_psP[hb], t_k], w=[t_E])
                    S.act(eP[:, h4, :], pv, AF.Exp, r=[t_psP[hb]], w=[t_eP])
                S.tt("pool", E[:], E[:], colT[:, 1, c, hs].unsqueeze(2).to_broadcast([128, 8, 128]), ALU.subtract, r=[t_E, t_colT], w=[t_E])
                S.act(E[:], E[:], AF.Exp, r=[t_E], w=[t_E])
                for g_ in range(2):
                    h4 = slice(g_ * 4, g_ * 4 + 4)
                    S.tt("dve", M[:, h4, :], E[:, h4, :], Gsb[b][:, g_, :].unsqueeze(1).to_broadcast([128, 4, 128]), ALU.mult, r=[t_E, t_G[b]], w=[t_M])
                    S.tt("pool", Ct[:, h4, :], eP[:, h4, :], Cfm[:, g_, cs].unsqueeze(1).to_broadcast([128, 4, 128]), ALU.mult, r=[t_eP, t_bc], w=[t_Ct])
                xv = x_tok[:, c, :].rearrange("p (h k) -> p h k", k=64)
                S.tt("dve", Xd[b][:], xv, colT[:, 0, c, hs].unsqueeze(2).to_broadcast([128, 8, 64]), ALU.mult, r=[t_xt, t_colT], w=[t_Xd[b]])
                S.tt("pool", Xdd[b][:], Xd[b][:], colT[:, 2, c, hs].unsqueeze(2).to_broadcast([128, 8, 64]), ALU.mult, r=[t_Xd[b], t_colT], w=[t_Xdd[b]])
                Sc, tSc = ST[si], t_ST[si]; Sn, tSn = ST[1 - si], t_ST[1 - si]
                for h in range(8):
                    g_ = h // 4
                    S.mm(psY[:, h * 64:(h + 1) * 64], M[:, h, :], Xd[b][:, h, :], True, False, r=[t_M, t_Xd[b]], w=[t_psY])
                    S.mm(psY[:, h * 64:(h + 1) * 64], Ct[:, h, :], Sc[:, h, :], False, True, r=[t_Ct, tSc], w=[t_psY])
                    S.mm(psS[:, h * 64:(h + 1) * 64], Btok[b][:, g_, :], Xdd[b][:, h, :], True, True, r=[t_Bt[b], t_Xdd[b]], w=[t_psS])
                if d == 0:
                    S.copy("act", yacc[:, c, :], psY[:], r=[t_psY], w=[t_y[c]])
                else:
                    S.tt("dve", yacc[:, c, :], yacc[:, c, :], psY[:], ALU.add, r=[t_psY, t_y[c]], w=[t_y[c]])
                ecol = 127 if d == 0 else 0
                S.tt("pool", tmpS[:], Sc[:], eP[:, :, ecol:ecol + 1].to_broadcast([128, 8, 64]), ALU.mult, r=[tSc, t_eP], w=[t_tmpS])
                S.tt("dve", Sn[:].rearrange("p a b -> p (a b)"), psS[:], tmpS[:].rearrange("p a b -> p (a b)"), ALU.add, r=[t_psS, t_tmpS], w=[tSn])
                si = 1 - si
        zg = [sb("mzg%d" % i, [128, 4, 128]) for i in range(2)]; t_zg = toks(2, "zg")
        zs = sb("mzs", [128, 512]); sq = sb("msq", [128, 512]); ofm = [sb("mofm%d" % i, [128, 4, 128]) for i in range(2)]
        ms = sb("mms", [128, 2]); t_zs, t_sq, t_ms = Tok("zs"), Tok("sq"), Tok("ms"); t_ofm = toks(2, "mofm")
        for c in range(NC2):
            b = c % 2
            cs = slice(c * 128, (c + 1) * 128)
            S.dma("sp", zg[b][:], g.zT[MB0:MB0 + 512, cs].rearrange("(tl p) t -> p tl t", p=128), w=[t_zg[b]])
            for tl in range(4):
                S.tr(psT[:, tl * 128:(tl + 1) * 128], zg[b][:, tl, :], g.ident[:], r=[t_zg[b], g.t_const], w=[t_psT])
            S.act(zs[:], psT[:], AF.Silu, r=[t_psT], w=[t_zs])
            yv = yacc[:, c, :]
            S.tt("pool", sq[:].rearrange("p (h k) -> p h k", k=64), x_tok[:, c, :].rearrange("p (h k) -> p h k", k=64),
                 Dbc[:].unsqueeze(2).to_broadcast([128, 8, 64]), ALU.mult, r=[t_xt, t_k], w=[t_sq])
            S.tt("dve", yv, yv, sq[:], ALU.add, r=[t_y[c], t_sq], w=[t_y[c]])
            S.tt("dve", yv, yv, zs[:], ALU.mult, r=[t_y[c], t_zs], w=[t_y[c]])
            S.act(sq[:], yv, AF.Square, r=[t_y[c]], w=[t_sq])
            S.reduce(ms[:], sq[:].rearrange("p (g k) -> p g k", g=2), ALU.add, r=[t_sq], w=[t_ms])
            S.ts("dve", ms[:], ms[:], 1.0 / 256, NORM_EPS, ALU.mult, ALU.add, r=[t_ms], w=[t_ms])
            S.act(ms[:], ms[:], AF.Sqrt, r=[t_ms], w=[t_ms])
            S.recip(ms[:], ms[:], r=[t_ms], w=[t_ms])
            S.tt("dve", yv.rearrange("p (g k) -> p g k", g=2), yv.rearrange("p (g k) -> p g k", g=2),
                 ms[:].unsqueeze(2).to_broadcast([128, 2, 256]), ALU.mult, r=[t_y[c], t_ms], w=[t_y[c]])
            for tl in range(4):
                S.tr(psG[:, tl * 128:(tl + 1) * 128], yacc[:, c, tl * 128:(tl + 1) * 128], g.ident[:], r=[t_y[c], g.t_const], w=[t_psG])
            for tl in range(4):
                S.ts("dve" if tl % 2 else "act_", ofm[b][:, tl, :], psG[:, tl * 128:(tl + 1) * 128], ncols[:, tl:tl + 1], None, ALU.mult,
                     r=[t_psG, t_nc], w=[t_ofm[b]])
            S.dma("sp", g.catT[512:1024, cs].rearrange("(tl p) t -> p tl t", p=128), ofm[b][:], r=[t_ofm[b]])
        S.barrier(); S.flush()


FGROUPS = [(i * 3, min(3, 22 - i * 3)) for i in range(8)]


def tok_chunks(n_tok):
    out = []
    c0 = 0
    while c0 < n_tok:
        n = min(512, n_tok - c0)
        out.append((c0, n))
        c0 += n
    return out


def swiglu_pass(g, st, aT, t_aT, h, t_h, NT, w1d, w3d, w2d, gate_col, tag, gb=None, t_gb=None, pools=None, chunks=None):
    nc, S = g.nc, g.S
    if pools is None:
        pools = {}
        sb = lambda name, shape, dt=F32: st.enter_context(nc.sbuf_tensor(name + tag, shape, dt))
        pools["w1"] = [sb("fw1_%d" % i, [128, 8, 384], BF16) for i in range(2)]
        pools["w3"] = [sb("fw3_%d" % i, [128, 8, 384], BF16) for i in range(2)]
        pools["w2"] = [sb("fw2_%d" % i, [128, 3, 1024], BF16) for i in range(2)]
        pools["t_w"] = toks(2, "fw")
        pools["gT"] = [sb("fgT%d" % i, [128, 3, 512], BF16) for i in range(2)]
        pools["t_gT"] = toks(2, "fgT")
        pools["sl"] = [sb("fsl%d" % i, [128, 512]) for i in range(2)]
        pools["t_sl"] = toks(2, "fsl")
        pools["cst"] = Caster(g, st, "fcst" + tag, 1024, nbuf=3)
        pools["ph"] = [st.enter_context(nc.psum_tensor("fph%d%s" % (i, tag), [128, 512], F32)) for i in range(4)]
        pools["t_ph"] = toks(4, "fph", True)
        pools["po"] = [st.enter_context(nc.psum_tensor("fpo%d%s" % (i, tag), [128, 512], F32)) for i in range(3)]
        pools["t_po"] = toks(3, "fpo", True)
        pools["n"] = {"w": 0, "g": 0, "ph": 0, "po": 0, "sl": 0}
    P = pools
    cst = P["cst"]
    w1v = w1d.rearrange("(k p) f -> p k f", p=128)
    w3v = w3d.rearrange("(k p) f -> p k f", p=128)
    w2v = w2d.rearrange("(k p) f -> p k f", p=128)
    chunks = chunks or tok_chunks(NT)
    for (f0, nf) in FGROUPS:
        wb = P["n"]["w"] % 2; P["n"]["w"] += 1
        tw = P["t_w"][wb]
        for k in range(8):
            cst.load(P["w1"][wb][:, k, :nf * 128], w1v[:, k, f0 * 128:(f0 + nf) * 128], tw)
            cst.load(P["w3"][wb][:, k, :nf * 128], w3v[:, k, f0 * 128:(f0 + nf) * 128], tw)
        for j in range(nf):
            cst.load(P["w2"][wb][:, j, :], w2v[:, f0 + j, :], tw)
        for (c0, n) in chunks:
            gi = P["n"]["g"] % 2; P["n"]["g"] += 1
            gT, tgT = P["gT"][gi], P["t_gT"][gi]
            for j in range(nf):
                i1 = P["n"]["ph"] % 4; i3 = (P["n"]["ph"] + 1) % 4; P["n"]["ph"] += 2
                p1, p3, t1, t3 = P["ph"][i1], P["ph"][i3], P["t_ph"][i1], P["t_ph"][i3]
                for k in range(8):
                    S.mm(p1[:, :n], P["w1"][wb][:, k, j * 128:(j + 1) * 128], aT[:, k, c0:c0 + n], k == 0, k == 7, r=[tw, t_aT], w=[t1])
                for k in range(8):
                    S.mm(p3[:, :n], P["w3"][wb][:, k, j * 128:(j + 1) * 128], aT[:, k, c0:c0 + n], k == 0, k == 7, r=[tw, t_aT], w=[t3])
                si = P["n"]["sl"] % 2; P["n"]["sl"] += 1
                sl, tsl = P["sl"][si], P["t_sl"][si]
                S.act(sl[:, :n], p1[:, :n], AF.Silu, r=[t1], w=[tsl])
                if gb is not None:
                    S.tt("pool", sl[:, :n], sl[:, :n], gb[:, c0:c0 + n], ALU.mult, r=[tsl, t_gb], w=[tsl])
                S.tt("dve", gT[:, j, :n], p3[:, :n], sl[:, :n], ALU.mult, r=[t3, tsl], w=[tgT])
            for dt_ in range(8):
                oi = P["n"]["po"] % 3; P["n"]["po"] += 1
                po, tpo = P["po"][oi], P["t_po"][oi]
                for j in range(nf):
                    S.mm(po[:, :n], P["w2"][wb][:, j, dt_ * 128:(dt_ + 1) * 128], gT[:, j, :n], j == 0, j == nf - 1, r=[tw, tgT], w=[tpo])
                S.stt(h[:, dt_, c0:c0 + n], po[:, :n], gate_col(c0)[:, dt_:dt_ + 1], h[:, dt_, c0:c0 + n], ALU.mult, ALU.add,
                      r=[tpo, t_h, g.t_mod], w=[t_h])
    return pools


def phase_l0_post(g):
    nc, S, I = g.nc, g.S, g.I
    with ExitStack() as st:
        sb = lambda name, shape, dt=F32: st.enter_context(nc.sbuf_tensor(name, shape, dt))
        h = sb("qh", [128, 8, T]); t_h = Tok("qh")
        aT = sb("qaT", [128, 8, T], BF16); t_aT = Tok("qaT")
        hTd = g.hT.rearrange("(c p) t -> p c t", p=128)
        for c in range(8):
            S.dma("sp" if c % 2 else "act", h[:, c, :], hTd[:, c, :], w=[t_h])
        with ExitStack() as st2:
            sb2 = lambda name, shape, dt=F32: st2.enter_context(nc.sbuf_tensor(name, shape, dt))
            wout = sb2("qwout", [128, 8, D], BF16); t_wout = Tok("qwout")
            cst = Caster(g, st2, "qcst", D, nbuf=2)
            catb = [sb2("qcatb%d" % i, [128, 8, 512], BF16) for i in range(2)]; t_catb = toks(2, "qcatb")
            cst2 = Caster(g, st2, "qcst2", 512, nbuf=3)
            po = [st2.enter_context(nc.psum_tensor("qpo%d" % i, [128, 512], F32)) for i in range(3)]; t_po = toks(3, "qpo", True)
            npool = norm_pools(nc, st2, "q")
            wv = I["mix_w_out"].rearrange("(k p) f -> p k f", p=128)
            for k in range(8):
                cst.load(wout[:, k, :], wv[:, k, :], t_wout)
            catv = g.catT.rearrange("(k p) t -> p k t", p=128)
            npo = 0
            for ci, (c0, n) in enumerate(CHUNKS):
                b = ci % 2
                w_ = 1 if ci == 0 else 0
                for k in range(8):
                    cst2.load(catb[b][:, k, :n], catv[:, k, c0:c0 + n], t_catb[b])
                for ft in range(8):
                    p = po[npo % 3]; tp = t_po[npo % 3]; npo += 1
                    for k in range(8):
                        S.mm(p[:, :n], wout[:, k, ft * 128:(ft + 1) * 128], catb[b][:, k, :n], k == 0, k == 7, r=[t_wout, t_catb[b]], w=[tp])
                    S.stt(h[:, ft, c0:c0 + n], p[:, :n], g.modT[:, 0, w_, 16 + ft:17 + ft], h[:, ft, c0:c0 + n], ALU.mult, ALU.add,
                          r=[tp, t_h, g.t_mod], w=[t_h])
                norm_mod(g, h[:, :, c0:c0 + n], n, g.Gf[:, 0, w_, :], g.modT[:, 0, w_, 24:32], npool, t_h, aT[:, :, c0:c0 + n], t_aT)
            if "h0mix_o" in g.dbg:
                dd = nc.dram_tensor("h0mixT", [D, T], F32, kind="ExternalOutput").ap()
                S.dma("sp", dd.rearrange("(c p) t -> p c t", p=128), h[:], r=[t_h])
            S.barrier(); S.flush()
        gate_col = lambda c0: g.modT[:, 0, 1 if c0 < TC else 0, 40:48]
        swiglu_pass(g, st, aT, t_aT, h, t_h, T, I["ffn_w1"], I["ffn_w3"], I["ffn_w2"], gate_col, "q", chunks=CHUNKS)
        for c in range(8):
            S.dma("sp" if c % 2 else "act", hTd[:, c, :], h[:, c, :], r=[t_h])
        S.barrier(); S.flush()


def alloc_l1_scratch(g):
    nc = g.nc
    def dscr(name, shape, dt):
        kind = {"kind": "ExternalOutput"} if name in g.dbg else {}
        return nc.dram_tensor(name, list(shape), dt, **kind).ap()
    g.qT = dscr("qT", (D, TL), BF16)
    g.kT = dscr("kT", (D, T), BF16)
    g.vtok = dscr("vtok", (T, D), BF16)
    g.oT = dscr("oT", (D, TL), BF16)
    g.rpbpad = dscr("rpbpad", (16, 15, 127), F32)


def phase_l1_qkv(g):
    nc, S, I = g.nc, g.S, g.I
    with ExitStack() as st:
        sb = lambda name, shape, dt=F32: st.enter_context(nc.sbuf_tensor(name, shape, dt))
        wq = sb("awq", [128, 8, 3 * D], BF16); t_wq = toks(8, "awq")
        cst = Caster(g, st, "acst", 3 * D, nbuf=2)
        hT = [sb("ahT%d" % i, [128, 8, 512]) for i in range(2)]; t_hT = toks(2, "ahT")
        aT = [sb("aaT%d" % i, [128, 8, 512], BF16) for i in range(2)]; t_aT = toks(2, "aaT")
        ob = [sb("aob%d" % i, [128, 512], BF16) for i in range(4)]; t_ob = toks(4, "aob")
        pz = [st.enter_context(nc.psum_tensor("apz%d" % i, [128, 512], F32)) for i in range(4)]; t_pz = toks(4, "apz", True)
        npool = norm_pools(nc, st, "b")
        wv = I["na_w_qkv"].rearrange("(k p) f -> p k f", p=128)
        for k in range(8):
            cst.load(wq[:, k, :], wv[:, k, :], t_wq[k])
        rp = sb("arp", [16, 15, 127]); t_rp = Tok("arp")
        S.memset("pool", rp[:], 0.0, w=[t_rp])
        S.dma("sp", rp[:, :, 48:79], I["na_rpb"], w=[t_rp])
        S.dma("sp", g.rpbpad, rp[:], r=[t_rp])
        hTd = g.hT.rearrange("(c p) t -> p c t", p=128)
        nz = 0
        for ci, (c0, n) in enumerate(CHUNKS):
            b = ci % 2
            w_ = 1 if ci == 0 else 0
            S.dma("sp", hT[b][:, :, :n], hTd[:, :, c0:c0 + n], w=[t_hT[b]])
            norm_mod(g, hT[b], n, g.Gm[:, 1, w_, :], g.modT[:, 1, w_, 0:8], npool, t_hT[b], aT[b], t_aT[b])
            for ft in range(16):
                if ft < 8 and ci == 0:
                    continue
                zb = nz % 4; nz += 1
                for k in range(8):
                    S.mm(pz[zb][:, :n], wq[:, k, ft * 128:(ft + 1) * 128], aT[b][:, k, :n], k == 0, k == 7, r=[t_wq[k], t_aT[b]], w=[t_pz[zb]])
                S.copy("act" if ft % 2 else "dve", ob[zb][:, :n], pz[zb][:, :n], r=[t_pz[zb]], w=[t_ob[zb]])
                if ft < 8:
                    S.dma("sp", g.qT[ft * 128:(ft + 1) * 128, c0 - TC:c0 - TC + n], ob[zb][:, :n], r=[t_ob[zb]])
                else:
                    S.dma("sp", g.kT[(ft - 8) * 128:(ft - 7) * 128, c0:c0 + n], ob[zb][:, :n], r=[t_ob[zb]])
            for tt_ in range(n // 128):
                for half in range(2):
                    zb = nz % 4; nz += 1
                    for k in range(8):
                        S.mm(pz[zb][:, :], aT[b][:, k, tt_ * 128:(tt_ + 1) * 128], wq[:, k, 2048 + half * 512:2048 + (half + 1) * 512],
                             k == 0, k == 7, r=[t_wq[k], t_aT[b]], w=[t_pz[zb]])
                    S.copy("act" if half else "dve", ob[zb][:, :], pz[zb][:, :], r=[t_pz[zb]], w=[t_ob[zb]])
                    S.dma("act", g.vtok[c0 + tt_ * 128:c0 + (tt_ + 1) * 128, half * 512:(half + 1) * 512], ob[zb][:, :], r=[t_ob[zb]])
        S.barrier(); S.flush()


def phase_l1_attn(g):
    nc, S, I = g.nc, g.S, g.I
    GW = 64
    NB = 3
    with ExitStack() as st:
        sb = lambda name, shape, dt=F32: st.enter_context(nc.sbuf_tensor(name, shape, dt))
        V0 = sb("nV0", [128, 18, D], BF16)
        V1 = sb("nV1", [128, 16, D], BF16)
        t_V = Tok("nV")
        S.dma("sp", V0[:], g.vtok.rearrange("(j p) f -> p j f", p=128), w=[t_V])
        S.dma("act", V1[:, 0:15, :], g.vtok[TC + 64:TC + 64 + 15 * 128, :].rearrange("(j p) f -> p j f", p=128), w=[t_V])
        S.dma("sp", V1[0:64, 15, :], g.vtok[TC + 64 + 15 * 128:T, :], w=[t_V])
        identb = sb("nidb", [128, 128], BF16); t_k = Tok("nconst")
        S.copy("dve", identb[:], g.ident[:], r=[g.t_const], w=[t_k])
        kcr = sb("nkcr", [128, 64]); qc = sb("nqc", [128, 2]); Mcol = sb("nMcol", [128, 64]); m2 = sb("nm2", [128, 64])
        S.op("pool", lambda e: e.iota(kcr[:], [[1, 64]], base=0, channel_multiplier=0, allow_small_or_imprecise_dtypes=True), w=[t_k])
        S.op("pool", lambda e: e.iota(qc[0:64, 0:1], [[0, 1]], base=-8, channel_multiplier=1, allow_small_or_imprecise_dtypes=True), w=[t_k])
        S.op("pool", lambda e: e.iota(qc[64:128, 0:1], [[0, 1]], base=-8, channel_multiplier=1, allow_small_or_imprecise_dtypes=True), w=[t_k])
        S.ts("dve", qc[:, 0:1], qc[:, 0:1], 0.0, 48.0, ALU.max, ALU.min, r=[t_k], w=[t_k])
        S.ts("dve", qc[:, 1:2], qc[:, 0:1], 16.0, None, ALU.add, r=[t_k], w=[t_k])
        S.ts("dve", Mcol[:], kcr[:], qc[:, 0:1], None, ALU.is_ge, r=[t_k], w=[t_k])
        S.ts("dve", m2[:], kcr[:], qc[:, 1:2], None, ALU.is_lt, r=[t_k], w=[t_k])
        S.tt("dve", Mcol[:], Mcol[:], m2[:], ALU.mult, r=[t_k], w=[t_k])
        S.ts("dve", Mcol[:], Mcol[:], -NEG, NEG, ALU.mult, ALU.add, r=[t_k], w=[t_k])
        J = sb("nJ", [128, 128]); Bhr = sb("nBhr", [128, 15, 64]); t_Bhr = Tok("nBhr")
        S.memset("pool", J[:], 0.0, w=[t_k])
        for hh in range(2):
            S.op("pool", lambda e, hh=hh: e.affine_select(out=J[hh * 64:(hh + 1) * 64, hh * 64:(hh + 1) * 64], in_=J[hh * 64:(hh + 1) * 64, hh * 64:(hh + 1) * 64],
                                                        compare_op=ALU.not_equal, fill=1.0, base=-63, pattern=[[1, 64]], channel_multiplier=1),
                 r=[t_k], w=[t_k])
        qh = [sb("nqh%d" % i, [64, 2, TL], BF16) for i in range(2)]
        kh = [sb("nkh%d" % i, [64, 2, T], BF16) for i in range(2)]
        Bh = [sb("nBh%d" % i, [128, 15, 64]) for i in range(2)]
        oTh = [sb("noT%d" % i, [64, 2, TL], BF16) for i in range(2)]
        t_qk = toks(2, "nqk"); t_Bh = toks(2, "nBh"); t_oT = toks(2, "noT")
        Ssb = [sb("nS%d" % i, [128, 768]) for i in range(NB)]; t_S = toks(NB, "nS")
        Pn = [sb("nPn%d" % i, [128, 768], BF16) for i in range(NB)]; t_Pn = toks(NB, "nPn")
        PnT = [sb("nPnT%d" % i, [128, 6, 128], BF16) for i in range(NB)]; t_PnT = toks(NB, "nPnT")
        stt_ = [sb("nst%d" % i, [128, 4]) for i in range(NB)]; t_st = toks(NB, "nst")
        ps1 = [st.enter_context(nc.psum_tensor("nps1_%d" % i, [128, 512], F32)) for i in range(NB)]; t_ps1 = toks(NB, "nps1", True)
        ps2 = [st.enter_context(nc.psum_tensor("nps2_%d" % i, [128, 512], F32)) for i in range(NB)]; t_ps2 = toks(NB, "nps2", True)
        psT = [st.enter_context(nc.psum_tensor("npsT%d" % i, [128, 6, 128], BF16)) for i in range(2)]; t_psT = toks(2, "npsT", True)
        n = 0
        for hp in range(8):
            hb = hp % 2
            S.dma("sp", qh[hb][:], g.qT[hp * 128:(hp + 1) * 128, :].rearrange("(h k) t -> k h t", h=2), w=[t_qk[hb]])
            S.dma("act", kh[hb][:], g.kT[hp * 128:(hp + 1) * 128, :].rearrange("(h k) t -> k h t", h=2), w=[t_qk[hb]])
            for hh in range(2):
                src = bass.AP(tensor=g.rpbpad.tensor, offset=(2 * hp + hh) * 15 * 127, ap=[[1, 64], [127, 15], [1, 64]])
                S.dma("sp", Bhr[hh * 64:(hh + 1) * 64, :, :], src, w=[t_Bhr])
            Bflat = Bhr[:].rearrange("p a b -> p (a b)")
            S.mm(ps1[0][:, :], J[:], Bflat[:, 0:512], True, True, r=[t_k, t_Bhr], w=[t_ps1[0]])
            S.mm(ps2[0][:, 0:448], J[:], Bflat[:, 512:960], True, True, r=[t_k, t_Bhr], w=[t_ps2[0]])
            Mc = Mcol[:].unsqueeze(1)
            S.tt("dve", Bh[hb][:, 0:8, :], ps1[0][:, :].rearrange("p (a b) -> p a b", b=64), Mc.to_broadcast([128, 8, 64]), ALU.add,
                 r=[t_ps1[0], t_k], w=[t_Bh[hb]])
            S.tt("dve", Bh[hb][:, 8:15, :], ps2[0][:, 0:448].rearrange("p (a b) -> p a b", b=64), Mc.to_broadcast([128, 7, 64]), ALU.add,
                 r=[t_ps2[0], t_k], w=[t_Bh[hb]])
            for r in range(32):
                b = n % NB; bt = n % 2; n += 1
                rs = min(max(r - 4, 0), 24)
                ri0 = rs - r + 7
                qs = slice(r * GW, (r + 1) * GW)
                k0 = TC + rs * GW
                for hh in range(2):
                    po = slice(hh * 64, (hh + 1) * 64)
                    S.mm(ps1[b][po, :], qh[hb][:, hh, qs], kh[hb][:, hh, k0:k0 + 512], True, True, r=[t_qk[hb]], w=[t_ps1[b]])
                    S.mm(ps2[b][po, 0:256], qh[hb][:, hh, qs], kh[hb][:, hh, 0:TC], True, True, r=[t_qk[hb]], w=[t_ps2[b]])
                S.stt(Ssb[b][:, 0:512], ps1[b][:, :], 0.125, Bh[hb][:, ri0:ri0 + 8, :].rearrange("p a b -> p (a b)"), ALU.mult, ALU.add,
                      r=[t_ps1[b], t_Bh[hb]], w=[t_S[b]])
                S.act(Ssb[b][:, 512:768], ps2[b][:, 0:256], AF.Copy, scale=0.125, r=[t_ps2[b]], w=[t_S[b]])
                S.reduce(stt_[b][:, 0:1], Ssb[b][:], ALU.max, r=[t_S[b]], w=[t_st[b]])
                S.ts("dve", stt_[b][:, 1:2], stt_[b][:, 0:1], -1.0, None, ALU.mult, r=[t_st[b]], w=[t_st[b]])
                S.act(Ssb[b][:], Ssb[b][:], AF.Exp, bias=stt_[b][:, 1:2], scale=1.0, accum_out=stt_[b][:, 2:3], r=[t_S[b], t_st[b]], w=[t_S[b], t_st[b]])
                S.recip(stt_[b][:, 3:4], stt_[b][:, 2:3], r=[t_st[b]], w=[t_st[b]])
                S.ts("dve", Pn[b][:], Ssb[b][:], stt_[b][:, 3:4], None, ALU.mult, r=[t_S[b], t_st[b]], w=[t_Pn[b]])
                for blk in range(6):
                    S.tr(psT[bt][:, blk, :], Pn[b][:, blk * 128:(blk + 1) * 128], identb[:], r=[t_Pn[b], t_k], w=[t_psT[bt]])
                S.copy("act", PnT[b][:].rearrange("p a b -> p (a b)"), psT[bt][:].rearrange("p a b -> p (a b)"), r=[t_psT[bt]], w=[t_PnT[b]])
                for hh in range(2):
                    hc = slice((2 * hp + hh) * 64, (2 * hp + hh + 1) * 64)
                    for blk in range(6):
                        if blk < 4:
                            vb = V0[:, 2 + rs // 2 + blk, hc] if rs % 2 == 0 else V1[:, (rs - 1) // 2 + blk, hc]
                        else:
                            vb = V0[:, blk - 4, hc]
                        S.mm(ps2[b][0:64, 256 + hh * 64:256 + (hh + 1) * 64], vb, PnT[b][:, blk, hh * 64:(hh + 1) * 64], blk == 0, blk == 5,
                             r=[t_V, t_PnT[b]], w=[t_ps2[b]])
                S.copy("dve", oTh[hb][:, :, qs], ps2[b][0:64, 256:384].rearrange("p (h q) -> p h q", h=2), r=[t_ps2[b]], w=[t_oT[hb]])
            S.dma("sp", g.oT[hp * 128:(hp + 1) * 128, :].rearrange("(h k) t -> k h t", h=2), oTh[hb][:], r=[t_oT[hb]])
        S.barrier(); S.flush()


def phase_l1_post(g):
    nc, S, I = g.nc, g.S, g.I
    NT = TL
    with ExitStack() as st:
        sb = lambda name, shape, dt=F32: st.enter_context(nc.sbuf_tensor(name, shape, dt))
        h = sb("ph", [128, 8, NT]); t_h = Tok("ph")
        aT = sb("paT", [128, 8, NT], BF16); t_aT = Tok("paT")
        gatesT = sb("pgatesT", [8, NT]); t_gates = Tok("gatesT")
        Sel8 = sb("pSel8", [8, 8, 128]); t_k = Tok("pconst")
        S.copy("dve", Sel8[:], g.ident[0:8, 0:8].unsqueeze(2).to_broadcast([8, 8, 128]), r=[g.t_const], w=[t_k])
        hTd = g.hT.rearrange("(c p) t -> p c t", p=128)
        for c in range(8):
            S.dma("sp" if c % 2 else "act", h[:, c, :], hTd[:, c, TC:T], w=[t_h])
        chunks = tok_chunks(NT)
        with ExitStack() as st2:
            sb2 = lambda name, shape, dt=F32: st2.enter_context(nc.sbuf_tensor(name, shape, dt))
            wout = sb2("pwout", [128, 8, D], BF16); t_wout = Tok("pwout")
            cst = Caster(g, st2, "pcst", D, nbuf=1)
            ob = [sb2("pob%d" % i, [128, 8, 512], BF16) for i in range(1)] * 2; t_ob = toks(1, "pob") * 2
            a32 = sb2("pa32", [128, 8, 512]); t_a32 = Tok("pa32")
            wr = sb2("pwr", [128, 8, 128]); t_wr = Tok("pwr")
            logT = sb2("plogT", [8, NT]); t_log = Tok("plogT")
            L = sb2("pL", [128, 16, 8]); W = sb2("pW", [128, 16, 8]); tmp = sb2("ptmp", [128, 16, 8]); t_L = Tok("pL")
            m1 = sb2("pm1", [128, 16]); m2_ = sb2("pm2", [128, 16]); den = sb2("pden", [128, 16])
            po = [st2.enter_context(nc.psum_tensor("ppo%d" % i, [128, 512], F32)) for i in range(3)]; t_po = toks(3, "ppo", True)
            pl = st2.enter_context(nc.psum_tensor("ppl", [128, 512], F32)); t_pl = Tok("ppl", True)
            npool = norm_pools(nc, st2, "p")
            rb, t_rb = load_cols(g, st2, "prb", [I["moe_router_b"].rearrange("(o p) -> o p", o=1)], ps=pl[:, 0:128], t_p=t_pl)
            wv = I["na_w_out"].rearrange("(k p) f -> p k f", p=128)
            for k in range(8):
                cst.load(wout[:, k, :], wv[:, k, :], t_wout)
            S.memset("pool", wr[:], 0.0, w=[t_wr])
            S.dma("sp", wr[:, :, 0:8], I["moe_router_w"].rearrange("(k p) e -> p k e", p=128), w=[t_wr])
            oTv = g.oT.rearrange("(k p) t -> p k t", p=128)
            npo = 0
            for ci, (c0, n) in enumerate(chunks):
                b = ci % 2
                S.dma("sp", ob[b][:, :, :n], oTv[:, :, c0:c0 + n], w=[t_ob[b]])
                for ft in range(8):
                    p = po[npo % 3]; tp = t_po[npo % 3]; npo += 1
                    for k in range(8):
                        S.mm(p[:, :n], wout[:, k, ft * 128:(ft + 1) * 128], ob[b][:, k, :n], k == 0, k == 7, r=[t_wout, t_ob[b]], w=[tp])
                    S.stt(h[:, ft, c0:c0 + n], p[:, :n], g.modT[:, 1, 0, 16 + ft:17 + ft], h[:, ft, c0:c0 + n], ALU.mult, ALU.add,
                          r=[tp, t_h, g.t_mod], w=[t_h])
                norm_mod(g, h[:, :, c0:c0 + n], n, g.Gf[:, 1, 0, :], g.modT[:, 1, 0, 24:32], npool, t_h, aT[:, :, c0:c0 + n], t_aT,
                         out_f32=a32)
                for k in range(8):
                    S.mm(pl[:, :n], wr[:, k, :], a32[:, k, :n], k == 0, k == 7, r=[t_wr, t_aT], w=[t_pl])
                S.ts("dve", logT[:, c0:c0 + n], pl[0:8, :n], rb[0:8, 0:1], None, ALU.add, r=[t_pl, t_rb], w=[t_log])
            if "h1att_o" in g.dbg:
                dd = nc.dram_tensor("h1attT", [D, NT], F32, kind="ExternalOutput").ap()
                S.dma("sp", dd.rearrange("(c p) t -> p c t", p=128), h[:], r=[t_h])
            for j in range(16):
                S.tr(pl[:, j * 8:(j + 1) * 8], logT[:, j * 128:(j + 1) * 128], g.ident[0:8, 0:8], r=[t_log, g.t_const], w=[t_pl])
            S.copy("dve", L[:].rearrange("p a b -> p (a b)"), pl[:, 0:128], r=[t_pl], w=[t_L])
            if "logits_o" in g.dbg:
                dd = nc.dram_tensor("logitsL", [128, 128], F32, kind="ExternalOutput").ap()
                S.dma("sp", dd[:, :], L[:].rearrange("p a b -> p (a b)"), r=[t_L])
            b3 = lambda a: a[:].unsqueeze(2).to_broadcast([128, 16, 8])
            S.reduce(m1[:], L[:], ALU.max, r=[t_L], w=[t_L])
            S.tt("dve", tmp[:], L[:], b3(m1), ALU.is_equal, r=[t_L], w=[t_L])
            S.stt(tmp[:], tmp[:], NEG, L[:], ALU.mult, ALU.add, r=[t_L], w=[t_L])
            S.reduce(m2_[:], tmp[:], ALU.max, r=[t_L], w=[t_L])
            S.tt("dve", tmp[:], L[:], b3(m2_), ALU.is_ge, r=[t_L], w=[t_L])
            S.tt("dve", W[:], L[:], b3(m1), ALU.subtract, r=[t_L], w=[t_L])
            S.act(W[:], W[:], AF.Exp, r=[t_L], w=[t_L])
            S.tt("dve", W[:], W[:], tmp[:], ALU.mult, r=[t_L], w=[t_L])
            S.reduce(den[:], W[:], ALU.add, r=[t_L], w=[t_L])
            S.recip(den[:], den[:], r=[t_L], w=[t_L])
            S.tt("dve", W[:], W[:], b3(den), ALU.mult, r=[t_L], w=[t_L])
            for j4 in range(4):
                for j in range(4):
                    S.tr(pl[0:8, j * 128:(j + 1) * 128], W[:, j4 * 4 + j, :], g.ident[:], r=[t_L, g.t_const], w=[t_pl])
                S.copy("dve", gatesT[:, j4 * 512:(j4 + 1) * 512], pl[0:8, :], r=[t_pl], w=[t_gates])
            S.barrier(); S.flush()
        gbs = [sb("pgb%d" % i, [128, NT]) for i in range(2)]; t_gb = toks(2, "pgb")
        pg = st.enter_context(nc.psum_tensor("ppg", [128, 512], F32)); t_pg = Tok("ppg", True)
        gate_col = lambda c0: g.modT[:, 1, 0, 40:48]
        pools = None
        NEXP = int(os.environ.get("MOE_NE", str(NE)))
        for e in range(NEXP):
            gb, tgb = gbs[e % 2], t_gb[e % 2]
            for (c0, n) in chunks:
                S.mm(pg[:, :n], Sel8[:, e, :], gatesT[:, c0:c0 + n], True, True, r=[t_k, t_gates], w=[t_pg])
                S.copy("act", gb[:, c0:c0 + n], pg[:, :n], r=[t_pg], w=[tgb])
            pools = swiglu_pass(g, st, aT, t_aT, h, t_h, NT, I["moe_w1"][e], I["moe_w3"][e], I["moe_w2"][e], gate_col, "p",
                                gb=gb, t_gb=tgb, pools=pools)
        if "h1_o" in g.dbg:
            dd = nc.dram_tensor("h1T", [D, NT], F32, kind="ExternalOutput").ap()
            S.dma("sp", dd.rearrange("(c p) t -> p c t", p=128), h[:], r=[t_h])
        with ExitStack() as st3:
            sb3 = lambda name, shape, dt=F32: st3.enter_context(nc.sbuf_tensor(name, shape, dt))
            sq = sb3("psq", [128, 8, 512]); rstd = sb3("prstd", [128, 512]); y = sq
            ot = [gbs[i][:, 0:D] for i in range(2)]
            t_sq, t_rstd = Tok("psq"), Tok("prstd"); t_yy = t_sq; t_ot = t_gb
            pt = [pools["ph"][i] for i in range(4)]; t_pt = [pools["t_ph"][i] for i in range(4)]
            pss = pools["po"][0]; t_pss = pools["t_po"][0]
            no = 0
            for (c0, n) in chunks:
                S.act(sq[:, :, :n], h[:, :, c0:c0 + n], AF.Square, r=[t_h], w=[t_sq])
                for c in range(8):
                    S.mm(pss[:, :n], g.ones[:], sq[:, c, :n], c == 0, c == 7, r=[t_sq, g.t_const], w=[t_pss])
                S.ts("dve", rstd[:, :n], pss[:, :n], 1.0 / D, NORM_EPS, ALU.mult, ALU.add, r=[t_pss], w=[t_rstd])
                S.act(rstd[:, :n], rstd[:, :n], AF.Sqrt, r=[t_rstd], w=[t_rstd])
                S.recip(rstd[:, :n], rstd[:, :n], r=[t_rstd], w=[t_rstd])
                for c in range(8):
                    S.stt(y[:, c, :n], h[:, c, c0:c0 + n], g.nfin[:, c:c + 1], rstd[:, :n], ALU.mult, ALU.mult, r=[t_h, t_rstd, g.t_mod], w=[t_yy])
                for tt_ in range(n // 128):
                    o_ = ot[no % 2]; to = t_ot[no % 2]; no += 1
                    for half in range(2):
                        p = pt[(2 * no + half) % 4]; tp = t_pt[(2 * no + half) % 4]
                        for c in range(4):
                            S.tr(p[:, c * 128:(c + 1) * 128], y[:, half * 4 + c, tt_ * 128:(tt_ + 1) * 128], g.ident[:], r=[t_yy, g.t_const], w=[tp])
                        S.copy("act" if half else "dve", o_[:, half * 512:(half + 1) * 512], p[:, :], r=[tp], w=[to])
                    S.dma("sp", g.out[c0 + tt_ * 128:c0 + (tt_ + 1) * 128, :], o_[:], r=[to])
            S.barrier(); S.flush()
        S.barrier(); S.flush()
```

```python
from contextlib import ExitStack
import os
import numpy as np
import concourse.bass as bass
import concourse.mybir as mybir
from concourse.bass_utils import run_bass_kernel_spmd

F32 = mybir.dt.float32
BF16 = mybir.dt.bfloat16
AF = mybir.ActivationFunctionType
ALU = mybir.AluOpType
AX = mybir.AxisListType

D = 1024
TC, TL = 256, 2048
T = TC + TL
NORM_EPS = 1e-6
MIX_IN = 3312
RW_COLS = 1760
D_FF = 2816
NE = 8

ENGS = ("pe", "act", "dve", "pool", "sp")
N_DSEM = 24


class Tok:
    __slots__ = ("name", "w", "r", "psum")

    def __init__(self, name="", psum=False):
        self.name = name
        self.w = None
        self.r = {}
        self.psum = psum


def toks(n, name="", psum=False):
    return [Tok("%s%d" % (name, i), psum) for i in range(n)]


class Sched:
    def __init__(self, nc, stack, self_sync=True):
        self.nc = nc
        self.self_sync = self_sync
        self.csem = {e: stack.enter_context(nc.semaphore("c_" + e)) for e in ENGS if e != "sp"}
        self.dsem = [stack.enter_context(nc.semaphore("d%d" % i)) for i in range(N_DSEM)]
        self.cnt = {e: 0 for e in ENGS}
        self.dcount = 0
        self.dval = [0] * N_DSEM
        self.waited = {e: {} for e in ENGS}
        self.rec = {e: [] for e in ENGS}
        self.n_ops = 0
        self.fence = stack.enter_context(nc.sbuf_tensor("fence", [128, 80], F32))
        self.t_fconst = Tok("fconst")
        self.t_floc = {"dve": toks(32, "fd"), "act": toks(32, "fa")}
        self.nfence = {"dve": 0, "act": 0}

    def _fence(self, eng):
        j = self.nfence[eng] % 32
        self.nfence[eng] += 1
        f = self.fence
        if eng == "dve":
            self.op("dve", lambda e: e.memset(f[0:1, j:j + 1], 0.0), r=(), w=[self.t_floc["dve"][j]])
        else:
            self.op("act", lambda e: e.activation(out=f[0:1, 32 + j:33 + j], in_=f[0:1, 72:73], func=AF.Copy),
                    r=[self.t_fconst], w=[self.t_floc["act"][j]])
        return self.cnt[eng]

    def _need(self, eng, dep, waits):
        if dep is None:
            return
        if dep[0] == "c":
            _, e2, idx = dep
            if e2 == eng and (eng == "pe" or not self.self_sync):
                return
            key = ("c", e2)
            val = idx
        else:
            _, slot, val = dep
            key = ("d", slot)
        if self.waited[eng].get(key, 0) >= val:
            return
        self.waited[eng][key] = val
        waits.append((key, val))

    def _deps(self, eng, r, w):
        waits = []
        for t in r:
            self._need(eng, t.w, waits)
            if t.psum:
                for k, v in t.r.items():
                    if not isinstance(k, tuple) and k != eng:
                        self._need(eng, ("c", k, v), waits)
        for t in w:
            self._need(eng, t.w, waits)
            for k, v in t.r.items():
                if isinstance(k, tuple):
                    self._need(eng, ("d", k[1], v), waits)
                else:
                    self._need(eng, ("c", k, v), waits)
        return waits

    def op(self, eng, fn, r=(), w=()):
        waits = self._deps(eng, r, w)
        self.cnt[eng] += 1
        idx = self.cnt[eng]
        for t in r:
            t.r[eng] = idx
        for t in w:
            t.w = ("c", eng, idx)
            t.r = {}
        self.rec[eng].append((waits, fn, ("c", eng)))
        self.n_ops += 1
        if eng in ("dve", "act") and any(t.psum for t in r):
            fidx = self._fence(eng)
            for t in r:
                if t.psum:
                    t.r[eng] = fidx

    def dma(self, eng, out, in_, r=(), w=(), **kw):
        slot = self.dcount % N_DSEM
        self.dcount += 1
        waits = self._deps(eng, r, w)
        if self.dval[slot] > 0:
            self._need(eng, ("d", slot, self.dval[slot]), waits)
        self.dval[slot] += 16
        val = self.dval[slot]
        for t in r:
            t.r[("d", slot)] = val
        for t in w:
            t.w = ("d", slot, val)
            t.r = {}
        fn = (lambda e, out=out, in_=in_, kw=kw: e.dma_start(out=out, in_=in_, **kw))
        self.rec[eng].append((waits, fn, ("d", slot)))
        self.n_ops += 1

    def barrier(self):
        for eng in ENGS:
            waits = []
            for e2 in ENGS:
                if e2 == "sp" or self.cnt[e2] == 0:
                    continue
                self._need(eng, ("c", e2, self.cnt[e2]), waits)
            for slot in range(N_DSEM):
                if self.dval[slot] > 0:
                    self._need(eng, ("d", slot, self.dval[slot]), waits)
            if waits:
                self.rec[eng].append((waits, None, None))

    def _sem(self, key):
        return self.csem[key[1]] if key[0] == "c" else self.dsem[key[1]]

    def flush(self):
        nc = self.nc
        rec = self.rec
        self.rec = {e: [] for e in ENGS}

        def replay(eng_name):
            def body(e):
                for waits, fn, sig in rec[eng_name]:
                    for key, val in waits:
                        e.wait_ge(self._sem(key), val)
                    if fn is None:
                        continue
                    ins = fn(e)
                    if sig[0] == "c":
                        ins.then_inc(self.csem[sig[1]], 1)
                    else:
                        ins.then_inc(self.dsem[sig[1]], 16)
            return body

        with nc.Block() as block:
            if rec["pe"]:
                block.tensor(replay("pe"))
            if rec["act"]:
                block.scalar(replay("act"))
            if rec["dve"]:
                block.vector(replay("dve"))
            if rec["pool"]:
                block.gpsimd(replay("pool"))
            if rec["sp"]:
                block.sync(replay("sp"))

    def mm(self, out, lhsT, rhs, start, stop, r=(), w=(), f32r=False):
        if (f32r or getattr(self, "default_f32r", False)) and os.environ.get("NO_F32R") is None and lhsT.dtype == F32 and rhs.dtype == F32:
            lhsT = lhsT.bitcast(mybir.dt.float32r)
            rhs = rhs.bitcast(mybir.dt.float32r)
        self.op("pe", lambda e: e.matmul(out, lhsT=lhsT, rhs=rhs, start=start, stop=stop), r, w)

    def tr(self, out, in_, ident, r=(), w=()):
        self.op("pe", lambda e: e.transpose(out, in_, ident), r, w)

    def act(self, out, in_, func, bias=None, scale=None, accum_out=None, r=(), w=()):
        kw = {}
        if bias is not None:
            kw["bias"] = bias
        if scale is not None:
            kw["scale"] = scale
        if accum_out is not None:
            kw["accum_out"] = accum_out
        self.op("act", lambda e: e.activation(out=out, in_=in_, func=func, **kw), r, w)

    def ts(self, eng, out, in0, s1, s2, op0, op1=None, accum_out=None, r=(), w=()):
        if eng == "act_":
            return self.act(out, in0, AF.Copy, scale=s1, r=r, w=w)
        kw = {}
        if op1 is not None:
            kw["op1"] = op1
        if accum_out is not None:
            kw["accum_out"] = accum_out
        self.op(eng, lambda e: e.tensor_scalar(out=out, in0=in0, scalar1=s1, scalar2=s2, op0=op0, **kw), r, w)

    def tt(self, eng, out, in0, in1, op, r=(), w=()):
        self.op(eng, lambda e: e.tensor_tensor(out=out, in0=in0, in1=in1, op=op), r, w)

    def stt(self, out, in0, scalar, in1, op0, op1, r=(), w=()):
        self.op("dve", lambda e: e.scalar_tensor_tensor(out=out, in0=in0, scalar=scalar, in1=in1, op0=op0, op1=op1), r, w)

    def copy(self, eng, out, in_, r=(), w=()):
        if eng == "act":
            self.op("act", lambda e: e.copy(out=out, in_=in_), r, w)
        else:
            self.op(eng, lambda e: e.tensor_copy(out=out, in_=in_), r, w)

    def recip(self, out, in_, r=(), w=()):
        self.op("dve", lambda e: e.reciprocal(out=out, in_=in_), r, w)

    def reduce(self, out, in_, op, axis=None, r=(), w=()):
        self.op("dve", lambda e: e.tensor_reduce(out=out, in_=in_, axis=axis or AX.X, op=op), r, w)

    def memset(self, eng, ap, val, r=(), w=()):
        self.op(eng, lambda e: e.memset(ap, val), r, w)


class Ctx:
    pass


def build_program(dbg=(), upto=99):
    nc = bass.Bass("TRN2", target_bir_lowering=False)
    g = Ctx()
    g.nc = nc
    g.dbg = set(dbg)

    def din(name, shape):
        return nc.dram_tensor(name, list(shape), F32, kind="ExternalInput").ap()

    def dscr(name, shape, dtype=F32):
        if name in g.dbg:
            return nc.dram_tensor(name, list(shape), dtype, kind="ExternalOutput").ap()
        return nc.dram_tensor(name, list(shape), dtype).ap()

    specs = dict(
        x=(TL, D), c=(D,), ctx=(TC, D), c_ctx=(D,), w_ada=(2, D, 6 * D), b_ada=(2, 6 * D),
        norm_mix=(2, D), norm_ffn=(2, D), norm_final=(D,), mix_w_in=(D, MIX_IN), mix_w_out=(D, D),
        rw_mu=(2, RW_COLS), rw_w0=(2, 512), rw_w_up=(2, 32, 512), rw_a0=(2, 512), rw_a_up=(2, 32, 512),
        rw_g_up=(96, 512), rw_k_k=(512,), rw_k_a=(512,), rw_r_k=(2, 512), rw_gn_w=(512,), rw_gn_b=(512,),
        mb_conv_w=(5, 1024), mb_conv_b=(1024,), mb_dt_bias=(16,), mb_a_log=(16,), mb_d=(8,), mb_norm_w=(512,),
        ffn_w1=(D, D_FF), ffn_w3=(D, D_FF), ffn_w2=(D_FF, D),
        na_w_qkv=(D, 3 * D), na_w_out=(D, D), na_rpb=(16, 15, 31),
        moe_router_w=(D, NE), moe_router_b=(NE,), moe_w1=(NE, D, D_FF), moe_w3=(NE, D, D_FF), moe_w2=(NE, D_FF, D),
    )

    class LazyIn(dict):
        def __missing__(self, k):
            v = din(k, specs[k])
            self[k] = v
            return v
    I = LazyIn()
    g.I = I
    out = nc.dram_tensor("out", [TL, D], F32, kind="ExternalOutput").ap()
    g.out = out

    g.hT = dscr("hT", (D, T))
    g.zT = dscr("zT", (MIX_IN, T))

    with ExitStack() as st:
        S = Sched(nc, st)
        g.S = S
        g.ident = st.enter_context(nc.sbuf_tensor("ident", [128, 128], F32))
        g.ones = st.enter_context(nc.sbuf_tensor("ones", [128, 128], F32))
        g.modT = st.enter_context(nc.sbuf_tensor("modT", [128, 2, 2, 48], F32))
        g.Gm = st.enter_context(nc.sbuf_tensor("Gm", [128, 2, 2, 8], F32))
        g.Gf = st.enter_context(nc.sbuf_tensor("Gf", [128, 2, 2, 8], F32))
        g.nfin = st.enter_context(nc.sbuf_tensor("nfin", [128, 8], F32))
        g.t_const = Tok("const")
        g.t_mod = Tok("mod")

        S.memset("pool", g.ident[:], 0.0, w=[g.t_const])
        S.op("pool", lambda e: e.affine_select(out=g.ident[:], in_=g.ident[:], compare_op=ALU.not_equal, fill=1.0,
                                                base=0, pattern=[[-1, 128]], channel_multiplier=1),
             r=[g.t_const], w=[g.t_const])
        S.memset("pool", g.ones[:], 1.0, w=[g.t_const])
        S.memset("pool", S.fence[:], 0.0, w=[S.t_fconst])

        phase_adaln(g)
        S.barrier(); S.flush()
        if upto >= 1:
            phase_l0_pre(g)
        if upto >= 2:
            alloc_rwkv_scratch(g)
            phase_rwkv_lora(g)
        if upto >= 3:
            phase_rwkv_prep(g)
        if upto >= 4 and not os.environ.get("SKIP_RWSCAN"):
            phase_rwkv_scan(g)
        if upto >= 5:
            alloc_ssd_scratch(g)
            phase_ssd_prep(g)
        if upto >= 6:
            phase_ssd_scan(g)
        if upto >= 7 and not os.environ.get("SKIP_L0"):
            phase_l0_post(g)
        if upto >= 8:
            alloc_l1_scratch(g)
            phase_l1_qkv(g)
        if upto >= 9:
            phase_l1_attn(g)
        if upto >= 10:
            phase_l1_post(g)
        S.barrier(); S.flush()
    g.used_inputs = list(I.keys())
    nc.used_inputs = g.used_inputs
    return nc


def col1(ap1d, p=128):
    return ap1d.rearrange("(k p o) -> p k o", p=p, o=1)


def load_cols(g, st, name, srcs, ps=None, t_p=None):
    nc, S = g.nc, g.S
    R = sum(a.shape[0] for a in srcs)
    assert R <= 128
    stage = st.enter_context(nc.sbuf_tensor(name + "_stg", [128, 128], F32))
    dst = st.enter_context(nc.sbuf_tensor(name, [128, R], F32))
    if ps is None:
        ps = st.enter_context(nc.psum_tensor(name + "_ps", [128, 128], F32))
        t_p = Tok(name + "p", True)
    t_s, t_d = Tok(name + "s"), Tok(name)
    S.memset("pool", stage[:], 0.0, w=[t_s])
    r0 = 0
    for a in srcs:
        r, w = a.shape
        q0 = 0
        while q0 < r:
            rem = r - q0
            n = (rem // 16) * 16 if rem >= 16 else max(x for x in (8, 4, 2, 1) if x <= rem)
            S.dma("sp", stage[r0 + q0:r0 + q0 + n, :w], a[q0:q0 + n, :], w=[t_s])
            q0 += n
        r0 += r
    S.tr(ps[:, :R], stage[:R, :], g.ident[:R, :R], r=[t_s, g.t_const], w=[t_p])
    S.copy("dve", dst[:], ps[:, :R], r=[t_p], w=[t_d])
    return dst, t_d


def phase_adaln(g):
    nc, S, I = g.nc, g.S, g.I
    with ExitStack() as st:
        cT = st.enter_context(nc.sbuf_tensor("cT", [128, 8, 2], F32))
        wb = [st.enter_context(nc.sbuf_tensor("wada%d" % i, [128, 8, 1024], F32)) for i in range(2)]
        ps = st.enter_context(nc.psum_tensor("ps_ada", [128, 2, 48, 2], F32))
        t_c, t_ps = Tok("c"), Tok("ps", True)
        t_w = toks(2, "wada")
        v2 = lambda a: a.rearrange("(j p) -> j p", p=128)
        colA, t_b = load_cols(g, st, "colA", [v2(I["c"]), v2(I["c_ctx"]), I["norm_mix"].rearrange("i (j p) -> (i j) p", p=128),
                                              I["norm_ffn"].rearrange("i (j p) -> (i j) p", p=128), v2(I["norm_final"])])
        bada, t_b2 = load_cols(g, st, "colB", [I["b_ada"].rearrange("i (j p) -> (i j) p", p=128)])
        nm = colA[:, 16:32].rearrange("p (i j) -> p i j", i=2)
        nf = colA[:, 32:48].rearrange("p (i j) -> p i j", i=2)
        badav = bada[:].rearrange("p (i j) -> p i j", i=2)
        S.act(cT[:, :, 0], colA[:, 0:8], AF.Silu, r=[t_b], w=[t_c])
        S.act(cT[:, :, 1], colA[:, 8:16], AF.Silu, r=[t_b], w=[t_c])
        n = 0
        for i in range(2):
            wv = I["w_ada"][i].rearrange("(k p) n -> p k n", p=128)
            for blk in range(6):
                buf = wb[n % 2]; tw = t_w[n % 2]; n += 1
                S.dma("sp" if n % 2 else "act", buf[:], wv[:, :, blk * 1024:(blk + 1) * 1024], w=[tw])
                for jj in range(8):
                    j = blk * 8 + jj
                    for k in range(8):
                        S.mm(ps[:, i, j, :], buf[:, k, jj * 128:(jj + 1) * 128], cT[:, k, :], k == 0, k == 7,
                             r=[tw, t_c], w=[t_ps])
        for i in range(2):
            for w_ in range(2):
                S.tt("dve", g.modT[:, i, w_, :], ps[:, i, :, w_], badav[:, i, :], ALU.add, r=[t_ps, t_b2], w=[g.t_mod])
        for i in range(2):
            for w_ in range(2):
                S.stt(g.Gm[:, i, w_, :], g.modT[:, i, w_, 8:16], 1.0, nm[:, i, :], ALU.add, ALU.mult, r=[g.t_mod, t_b], w=[g.t_mod])
                S.stt(g.Gf[:, i, w_, :], g.modT[:, i, w_, 32:40], 1.0, nf[:, i, :], ALU.add, ALU.mult, r=[g.t_mod, t_b], w=[g.t_mod])
        S.copy("dve", g.nfin[:], colA[:, 48:56], r=[t_b], w=[g.t_mod])
        if "modT" in g.dbg:
            dd = nc.dram_tensor("modT_o", [128, 2 * 2 * 48], F32, kind="ExternalOutput").ap()
            S.dma("sp", dd[:, :], g.modT[:].rearrange("p a b c -> p (a b c)"), r=[g.t_mod])
        S.barrier(); S.flush()


def norm_mod(g, hT, n, Gsc, shift, pools, r_h, out_bf, t_out, out_f32=None):
    S = g.S
    sq, rstd, ps, tmp = pools["sq"], pools["rstd"], pools["ps"], pools["tmp"]
    t_sq, t_rstd, t_ps, t_tmp = pools["t_sq"], pools["t_rstd"], pools["t_ps"], pools["t_tmp"]
    S.act(sq[:, :, :n], hT[:, :, :n], AF.Square, r=[r_h], w=[t_sq])
    for c in range(8):
        S.mm(ps[:, :n], g.ones[:], sq[:, c, :n], c == 0, c == 7, r=[t_sq, g.t_const], w=[t_ps])
    S.ts("dve", rstd[:, :n], ps[:, :n], 1.0 / D, NORM_EPS, ALU.mult, ALU.add, r=[t_ps], w=[t_rstd])
    S.act(rstd[:, :n], rstd[:, :n], AF.Sqrt, r=[t_rstd], w=[t_rstd])
    S.op("dve", lambda e: e.reciprocal(out=rstd[:, :n], in_=rstd[:, :n]), r=[t_rstd], w=[t_rstd])
    for c in range(8):
        S.stt(tmp[:, c, :n], hT[:, c, :n], Gsc[:, c:c + 1], rstd[:, :n], ALU.mult, ALU.mult, r=[r_h, t_rstd, g.t_mod], w=[t_tmp])
    for c in range(8):
        S.act(out_bf[:, c, :n], tmp[:, c, :n], AF.Identity, bias=shift[:, c:c + 1], scale=1.0, r=[t_tmp, g.t_mod], w=[t_out])
        if out_f32 is not None:
            S.ts("pool", out_f32[:, c, :n], tmp[:, c, :n], shift[:, c:c + 1], None, ALU.add, r=[t_tmp, g.t_mod], w=[t_out])


def norm_pools(nc, st, tag):
    p = {}
    p["sq"] = st.enter_context(nc.sbuf_tensor("nsq" + tag, [128, 8, 512], F32))
    p["tmp"] = st.enter_context(nc.sbuf_tensor("ntmp" + tag, [128, 8, 512], F32))
    p["rstd"] = st.enter_context(nc.sbuf_tensor("nrstd" + tag, [128, 512], F32))
    p["ps"] = st.enter_context(nc.psum_tensor("nps" + tag, [128, 512], F32))
    for k in ("sq", "tmp", "rstd", "ps"):
        p["t_" + k] = Tok("n" + k, k == "ps")
    return p


class Caster:
    def __init__(self, g, st, name, cols, nbuf=2):
        self.S = g.S
        self.stage = [st.enter_context(g.nc.sbuf_tensor("%s_stg%d" % (name, i), [128, cols], F32)) for i in range(nbuf)]
        self.tok = toks(nbuf, name + "_stg")
        self.n = 0
        self.nbuf = nbuf
        self.engs = ("pool", "dve", "act")

    def load(self, dst, src, w_tok, rows=128, eng=None, dma_eng=None):
        i = self.n % self.nbuf
        n = dst.shape[1]
        e = eng or self.engs[self.n % 2]
        q = dma_eng or ("sp" if self.n % 2 == 0 else "act")
        self.n += 1
        self.S.dma(q, self.stage[i][:rows, :n], src, w=[self.tok[i]])
        self.S.copy(e, dst, self.stage[i][:rows, :n], r=[self.tok[i]], w=[w_tok])


CHUNKS = [(0, 256)] + [(256 + 512 * i, 512) for i in range(4)]


def phase_l0_pre(g):
    nc, S, I = g.nc, g.S, g.I
    with ExitStack() as st:
        win = st.enter_context(nc.sbuf_tensor("win", [128, 8, MIX_IN], BF16))
        xt = [st.enter_context(nc.sbuf_tensor("xt%d" % i, [128, 4, D], F32)) for i in range(2)]
        hT = [st.enter_context(nc.sbuf_tensor("hTs%d" % i, [128, 8, 512], F32)) for i in range(2)]
        aT = [st.enter_context(nc.sbuf_tensor("aT%d" % i, [128, 8, 512], BF16)) for i in range(2)]
        zt = [st.enter_context(nc.sbuf_tensor("zt%d" % i, [128, 512], F32)) for i in range(3)]
        pT = [st.enter_context(nc.psum_tensor("pT%d" % i, [128, 512], F32)) for i in range(2)]
        pz = [st.enter_context(nc.psum_tensor("pz%d" % i, [128, 512], F32)) for i in range(3)]
        npool = norm_pools(nc, st, "a")
        t_win = toks(8, "win"); t_xt = toks(2, "xt"); t_hT = toks(2, "hT"); t_aT = toks(2, "aT")
        t_zt = toks(3, "zt"); t_pT = toks(2, "pT", True); t_pz = toks(3, "pz", True)
        wv = I["mix_w_in"].rearrange("(k p) f -> p k f", p=128)
        cst = Caster(g, st, "winc", MIX_IN)
        for k in range(8):
            cst.load(win[:, k, :], wv[:, k, :], t_win[k])
        hTd = g.hT.rearrange("(c p) t -> p c t", p=128)
        nz = 0; npt = 0
        for ci, (c0, n) in enumerate(CHUNKS):
            nt = n // 128
            b = ci % 2
            src = I["ctx"] if ci == 0 else I["x"][(ci - 1) * 512:ci * 512, :]
            S.dma("sp", xt[b][:, :nt, :], src.rearrange("(t p) d -> p t d", p=128), w=[t_xt[b]])
            for c in range(8):
                pb = npt % 2; npt += 1
                for t in range(nt):
                    S.tr(pT[pb][:, t * 128:(t + 1) * 128], xt[b][:, t, c * 128:(c + 1) * 128], g.ident[:],
                         r=[t_xt[b], g.t_const], w=[t_pT[pb]])
                S.copy("act" if c % 2 else "dve", hT[b][:, c, :n], pT[pb][:, :n], r=[t_pT[pb]], w=[t_hT[b]])
            S.dma("sp", hTd[:, :, c0:c0 + n], hT[b][:, :, :n], r=[t_hT[b]])
            w_ = 1 if ci == 0 else 0
            norm_mod(g, hT[b], n, g.Gm[:, 0, w_, :], g.modT[:, 0, w_, 0:8], npool, t_hT[b], aT[b], t_aT[b])
            for ft in range(26):
                f0 = ft * 128; fs = min(128, MIX_IN - f0)
                zb = nz % 3; nz += 1
                for k in range(8):
                    S.mm(pz[zb][:fs, :n], win[:, k, f0:f0 + fs], aT[b][:, k, :n], k == 0, k == 7,
                         r=[t_win[k], t_aT[b]], w=[t_pz[zb]])
                S.copy("act" if ft % 2 else "dve", zt[zb][:fs, :n], pz[zb][:fs, :n], r=[t_pz[zb]], w=[t_zt[zb]])
                S.dma("sp", g.zT[f0:f0 + fs, c0:c0 + n], zt[zb][:fs, :n], r=[t_zt[zb]])
        S.barrier(); S.flush()


_SQUEEZE0 = ("mix_w_in", "mix_w_out", "rw_mu", "rw_w0", "rw_w_up", "rw_a0", "rw_a_up", "rw_g_up", "rw_k_k", "rw_k_a",
             "rw_gn_w", "rw_gn_b", "mb_conv_w", "mb_conv_b", "mb_d", "mb_norm_w", "ffn_w1", "ffn_w3", "ffn_w2",
             "na_w_qkv", "na_w_out", "na_rpb", "moe_router_w", "moe_router_b", "moe_w1", "moe_w3", "moe_w2")


def make_in_maps(inputs):
    shared = {}
    for k, v in inputs.items():
        if k in ("x", "c", "ctx"):
            continue
        a = np.ascontiguousarray(np.asarray(v, dtype=np.float32))
        if k in _SQUEEZE0:
            a = a[0]
        elif k == "rw_r_k":
            a = a[0].reshape(2, 512)
        elif k in ("mb_dt_bias", "mb_a_log"):
            a = a[0].reshape(16)
        shared[k] = np.ascontiguousarray(a)
    maps = []
    for b in range(8):
        m = dict(shared)
        m["x"] = np.ascontiguousarray(np.asarray(inputs["x"][b], dtype=np.float32))
        m["c"] = np.ascontiguousarray(np.asarray(inputs["c"][b], dtype=np.float32))
        m["ctx"] = np.ascontiguousarray(np.asarray(inputs["ctx"][b], dtype=np.float32))
        maps.append(m)
    return maps


def kernel(**inputs):
    nc = build_program()
    maps = make_in_maps(inputs)
    res = run_bass_kernel_spmd(nc, maps, core_ids=list(range(8)))
    return np.stack([np.asarray(r["out"], dtype=np.float32) for r in res.results], axis=0)


KAPPA = 0.6065306597126334
NCH = T // 64
SEGS = [(0, TC), (TC, T)]


def shift_tile(S, dst, src, c0, mp, mn, rows, r, w):
    S.act(dst[:rows, :], src[:rows, :], AF.Copy, scale=c0, r=r, w=w)
    for a, b in SEGS:
        S.stt(dst[:rows, a + 1:b], src[:rows, a:b - 1], mp, dst[:rows, a + 1:b], ALU.mult, ALU.add, r=r + w, w=w)
        S.stt(dst[:rows, a:b - 1], src[:rows, a + 1:b], mn, dst[:rows, a:b - 1], ALU.mult, ALU.add, r=r + w, w=w)


def alloc_rwkv_scratch(g):
    nc = g.nc
    def dscr(name, shape, dt=F32):
        kind = {"kind": "ExternalOutput"} if name in g.dbg else {}
        return nc.dram_tensor(name, list(shape), dt, **kind).ap()
    g.PR = [[dscr("PR%d_%d" % (d, q), (512, T), BF16) for q in range(6)] for d in range(2)]
    g.vS = dscr("vS", (512, T), BF16)
    g.bonusT = dscr("bonusT", (512, T))
    g.gateT = dscr("gateT", (512, T))
    g.WCdd = [dscr("WCd%d" % d, (512, NCH)) for d in range(2)]
    g.sS = [dscr("sS%d" % d, (512, T)) for d in range(2)]
    g.iclrS = [dscr("iclrS%d" % d, (512, T)) for d in range(2)]
    g.catT = dscr("catT", (D, T))


def phase_rwkv_lora(g):
    nc, S, I = g.nc, g.S, g.I
    with ExitStack() as st:
        v2 = lambda a: a.rearrange("(j p) -> j p", p=128)
        mu0, mu1 = I["rw_mu"][0], I["rw_mu"][1]
        cols, t_cols = load_cols(g, st, "lcols", [
            mu0[1536:1664].rearrange("(j p) -> j p", p=128), mu1[1536:1664].rearrange("(j p) -> j p", p=128),
            mu0[1664:1760].rearrange("(j p) -> j p", p=96), mu1[1664:1760].rearrange("(j p) -> j p", p=96),
            I["rw_w0"].rearrange("d (j p) -> (d j) p", p=128), I["rw_a0"].rearrange("d (j p) -> (d j) p", p=128)])
        c0 = st.enter_context(nc.sbuf_tensor("lc0", [128, 2], F32))
        t_c0 = Tok("c0")
        for i in range(2):
            S.tt("dve", c0[:, i:i + 1], cols[:, 2 * i:2 * i + 1], cols[:, 2 * i + 1:2 * i + 2], ALU.add, r=[t_cols], w=[t_c0])
        S.ts("dve", c0[:], c0[:], -1.0, 1.0, ALU.mult, ALU.add, r=[t_c0], w=[t_c0])
        zraw = st.enter_context(nc.sbuf_tensor("lzraw", [128, T], F32))
        z12 = st.enter_context(nc.sbuf_tensor("lz12", [128, T], F32))
        z12t = st.enter_context(nc.sbuf_tensor("lz12t", [128, T], F32))
        zlg = st.enter_context(nc.sbuf_tensor("lzlg", [128, T], F32))
        wupE = st.enter_context(nc.sbuf_tensor("wupE", [128, 2, 512], F32))
        aupE = st.enter_context(nc.sbuf_tensor("aupE", [128, 2, 512], F32))
        gup = st.enter_context(nc.sbuf_tensor("gup", [96, 512], F32))
        ob = [st.enter_context(nc.sbuf_tensor("lob%d" % i, [128, T], F32)) for i in range(2)]
        pl = [st.enter_context(nc.psum_tensor("lps%d" % i, [128, 512], F32)) for i in range(3)]
        t_zr, t_12, t_12t, t_lg, t_wE = Tok("zr"), Tok("z12"), Tok("z12t"), Tok("zlg"), Tok("wE")
        t_ob = toks(2, "lob"); t_pl = toks(3, "lps", True)
        S.memset("pool", wupE[:], 0.0, w=[t_wE])
        S.memset("pool", aupE[:], 0.0, w=[t_wE])
        for d in range(2):
            S.dma("sp", wupE[d * 32:(d + 1) * 32, d, :], I["rw_w_up"][d], w=[t_wE])
            S.dma("sp", aupE[64 + d * 32:64 + (d + 1) * 32, d, :], I["rw_a_up"][d], w=[t_wE])
        S.dma("sp", gup[:], I["rw_g_up"], w=[t_wE])
        S.dma("sp", zraw[:], g.zT[1536:1664, :], w=[t_zr])
        shift_tile(S, z12, zraw, c0[:, 0:1], cols[:, 0:1], cols[:, 1:2], 128, [t_zr, t_cols, t_c0], [t_12])
        S.act(z12t[:], z12[:], AF.Tanh, r=[t_12], w=[t_12t])
        S.dma("sp", zraw[:96, :], g.zT[1664:1760, :], w=[t_zr])
        shift_tile(S, zlg, zraw, c0[:96, 1:2], cols[:96, 2:3], cols[:96, 3:4], 96, [t_zr, t_cols, t_c0], [t_lg])
        S.act(zlg[:96, :], zlg[:96, :], AF.Sigmoid, r=[t_lg], w=[t_lg])
        npl = 0; nob = 0
        jobs = []
        for tl in range(4):
            for d in range(2):
                jobs.append((wupE[:, d, tl * 128:(tl + 1) * 128], z12t, 128, t_12t, cols[:, 4 + d * 4 + tl:5 + d * 4 + tl], g.sS[d], tl))
                jobs.append((aupE[:, d, tl * 128:(tl + 1) * 128], z12, 128, t_12, cols[:, 12 + d * 4 + tl:13 + d * 4 + tl], g.iclrS[d], tl))
            jobs.append((gup[:, tl * 128:(tl + 1) * 128], zlg, 96, t_lg, None, g.gateT, tl))
        for (lhsT, rhs, K, t_rhs, bias, dst, tl) in jobs:
            o = ob[nob % 2]; to = t_ob[nob % 2]; nob += 1
            for (c0_, n) in CHUNKS:
                p = pl[npl % 3]; tp = t_pl[npl % 3]; npl += 1
                S.mm(p[:, :n], lhsT[:K, :] if K < 128 else lhsT, rhs[:K, c0_:c0_ + n], True, True, r=[t_wE, t_rhs], w=[tp])
                if bias is not None:
                    S.act(o[:, c0_:c0_ + n], p[:, :n], AF.Sigmoid, bias=bias, scale=1.0, r=[tp, t_cols], w=[to])
                else:
                    S.copy("dve", o[:, c0_:c0_ + n], p[:, :n], r=[tp], w=[to])
            S.dma("sp", dst[tl * 128:(tl + 1) * 128, :], o[:], r=[to])
        S.barrier(); S.flush()


def phase_rwkv_prep(g):
    nc, S, I = g.nc, g.S, g.I
    with ExitStack() as st:
        mu0, mu1 = I["rw_mu"][0], I["rw_mu"][1]
        r128 = lambda a: a.rearrange("(j p) -> j p", p=128)
        cols, t_cols = load_cols(g, st, "pcols", [
            r128(mu0[0:1536]), r128(mu1[0:1536]), r128(I["rw_k_k"]), r128(I["rw_k_a"]),
            I["rw_r_k"].rearrange("d (j p) -> (d j) p", p=128)])
        c0 = st.enter_context(nc.sbuf_tensor("pc0", [128, 12], F32))
        omka = st.enter_context(nc.sbuf_tensor("pomka", [128, 4], F32))
        t_c0 = Tok("pc0")
        S.tt("dve", c0[:], cols[:, 0:12], cols[:, 12:24], ALU.add, r=[t_cols], w=[t_c0])
        S.ts("dve", c0[:], c0[:], -1.0, 1.0, ALU.mult, ALU.add, r=[t_c0], w=[t_c0])
        S.ts("dve", omka[:], cols[:, 28:32], -1.0, 1.0, ALU.mult, ALU.add, r=[t_cols], w=[t_c0])
        bones = st.enter_context(nc.sbuf_tensor("bones", [128, 128], F32))
        maskC = st.enter_context(nc.sbuf_tensor("maskC", [128, T], F32))
        t_k = Tok("pconst")
        S.memset("pool", bones[:], 0.0, w=[t_k])
        S.memset("pool", bones[0:64, 0:64], 1.0, w=[t_k])
        S.memset("pool", bones[64:128, 64:128], 1.0, w=[t_k])
        S.memset("pool", maskC[:], 1.0, w=[t_k])
        S.memset("pool", maskC[:].rearrange("p (c j) -> p c j", j=64)[:, :, 0:1], 0.0, w=[t_k])
        names = ["zraw", "r", "k", "v", "kk", "bon", "s", "icl", "kdir", "b", "P", "E", "Q", "Qi", "ex0", "ex1", "o0", "o1"]
        tl_ = {n: st.enter_context(nc.sbuf_tensor("p_" + n, [128, T], BF16 if n in ("o0", "o1") else F32)) for n in names}
        tk = {n: Tok("p_" + n) for n in names}
        tot = st.enter_context(nc.sbuf_tensor("ptot", [128, NCH], F32))
        wc = st.enter_context(nc.sbuf_tensor("pwc", [128, NCH], F32))
        t_tot = Tok("tot")
        pp = [st.enter_context(nc.psum_tensor("pps%d" % i, [128, 512], F32)) for i in range(2)]
        t_pp = toks(2, "pps", True)
        npp = 0
        nout = 0

        def out_tile(dst, compute):
            nonlocal nout
            o = tl_["o%d" % (nout % 2)]; to = tk["o%d" % (nout % 2)]; nout += 1
            compute(o, to)
            S.dma("sp", dst, o[:], r=[to])

        for tl in range(4):
            rows = slice(tl * 128, (tl + 1) * 128)
            for qi, q in enumerate(("r", "k", "v")):
                S.dma("sp", tl_["zraw"][:], g.zT[qi * 512 + tl * 128:qi * 512 + (tl + 1) * 128, :], w=[tk["zraw"]])
                ci = qi * 4 + tl
                shift_tile(S, tl_[q], tl_["zraw"], c0[:, ci:ci + 1], cols[:, ci:ci + 1], cols[:, 12 + ci:13 + ci], 128,
                           [tk["zraw"], t_cols, t_c0], [tk[q]])
            out_tile(g.vS[rows, :], lambda o, to: S.copy("pool", o[:], tl_["v"][:], r=[tk["v"]], w=[to]))
            kk = tl_["kk"]; ex0 = tl_["ex0"]; ex1 = tl_["ex1"]
            S.ts("dve", kk[:], tl_["k"][:], cols[:, 24 + tl:25 + tl], None, ALU.mult, r=[tk["k"], t_cols], w=[tk["kk"]])
            S.act(ex0[:], kk[:], AF.Square, r=[tk["kk"]], w=[tk["ex0"]])
            for (c0_, n) in CHUNKS:
                p = pp[npp % 2]; tp = t_pp[npp % 2]; npp += 1
                S.mm(p[:, :n], bones[:], ex0[:, c0_:c0_ + n], True, True, r=[t_k, tk["ex0"]], w=[tp])
                S.act(ex1[:, c0_:c0_ + n], p[:, :n], AF.Sqrt, r=[tp], w=[tk["ex1"]])
            S.ts("dve", ex1[:], ex1[:], 1e-12, None, ALU.max, r=[tk["ex1"]], w=[tk["ex1"]])
            S.op("dve", lambda e, ex1=ex1: e.reciprocal(out=ex1[:], in_=ex1[:]), r=[tk["ex1"]], w=[tk["ex1"]])
            S.tt("dve", kk[:], kk[:], ex1[:], ALU.mult, r=[tk["kk"], tk["ex1"]], w=[tk["kk"]])
            S.memset("pool", tl_["bon"][:], 0.0, w=[tk["bon"]])
            for d in range(2):
                s, icl, kdir, b = tl_["s"], tl_["icl"], tl_["kdir"], tl_["b"]
                P, E, Q, Qi = tl_["P"], tl_["E"], tl_["Q"], tl_["Qi"]
                S.dma("sp", s[:], g.sS[d][rows, :], w=[tk["s"]])
                S.dma("sp", icl[:], g.iclrS[d][rows, :], w=[tk["icl"]])
                S.ts("dve", kdir[:], icl[:], cols[:, 28 + tl:29 + tl], omka[:, tl:tl + 1], ALU.mult, ALU.add,
                     r=[tk["icl"], t_cols, t_c0], w=[tk["kdir"]])
                S.tt("dve", kdir[:], kdir[:], tl_["k"][:], ALU.mult, r=[tk["kdir"], tk["k"]], w=[tk["kdir"]])
                S.tt("pool", b[:], kk[:], icl[:], ALU.mult, r=[tk["kk"], tk["icl"]], w=[tk["b"]])
                S.stt(ex0[:], tl_["r"][:], cols[:, 32 + d * 4 + tl:33 + d * 4 + tl], kdir[:], ALU.mult, ALU.mult,
                      r=[tk["r"], tk["kdir"], t_cols], w=[tk["ex0"]])
                for (c0_, n) in CHUNKS:
                    p = pp[npp % 2]; tp = t_pp[npp % 2]; npp += 1
                    S.mm(p[:, :n], bones[:], ex0[:, c0_:c0_ + n], True, True, r=[t_k, tk["ex0"]], w=[tp])
                    S.tt("dve", tl_["bon"][:, c0_:c0_ + n], tl_["bon"][:, c0_:c0_ + n], p[:, :n], ALU.add, r=[tp, tk["bon"]], w=[tk["bon"]])
                S.op("dve", lambda e, P=P, s=s: e.tensor_tensor_scan(out=P[:], data0=maskC[:], data1=s[:], initial=0.0,
                                                                   op0=ALU.mult, op1=ALU.add), r=[t_k, tk["s"]], w=[tk["P"]])
                S.copy("dve", tot[:], P[:].rearrange("p (c j) -> p c j", j=64)[:, :, 63], r=[tk["P"]], w=[t_tot])
                S.tt("pool", E[:], P[:], s[:], ALU.subtract, r=[tk["P"], tk["s"]], w=[tk["E"]])
                v3 = lambda a: a[:].rearrange("p (c j) -> p c j", j=64)
                S.tt("dve", v3(Q), tot[:].unsqueeze(2).to_broadcast([128, NCH, 64]), v3(P), ALU.subtract, r=[tk["P"], t_tot], w=[tk["Q"]])
                S.tt("pool", Qi[:], Q[:], s[:], ALU.add, r=[tk["Q"], tk["s"]], w=[tk["Qi"]])
                S.act(wc[:], tot[:], AF.Exp, scale=-KAPPA, r=[t_tot], w=[t_tot])
                S.dma("sp", g.WCdd[d][rows, :], wc[:], r=[t_tot])
                lr, la, ln, lc = (P, E, P, Q) if d == 0 else (Qi, Q, Qi, E)
                tn = {id(P): "P", id(E): "E", id(Q): "Q", id(Qi): "Qi"}
                S.act(ex0[:], lr[:], AF.Exp, scale=-KAPPA, r=[tk[tn[id(lr)]]], w=[tk["ex0"]])
                out_tile(g.PR[d][0][rows, :], lambda o, to: S.tt("dve", o[:], tl_["r"][:], ex0[:], ALU.mult, r=[tk["r"], tk["ex0"]], w=[to]))
                S.act(ex1[:], la[:], AF.Exp, scale=-KAPPA, r=[tk[tn[id(la)]]], w=[tk["ex1"]])
                out_tile(g.PR[d][1][rows, :], lambda o, to: S.stt(o[:], kk[:], -1.0, ex1[:], ALU.mult, ALU.mult, r=[tk["kk"], tk["ex1"]], w=[to]))
                S.act(ex0[:], ln[:], AF.Exp, scale=KAPPA, r=[tk[tn[id(ln)]]], w=[tk["ex0"]])
                out_tile(g.PR[d][2][rows, :], lambda o, to: S.tt("dve", o[:], b[:], ex0[:], ALU.mult, r=[tk["b"], tk["ex0"]], w=[to]))
                out_tile(g.PR[d][3][rows, :], lambda o, to: S.tt("pool", o[:], kdir[:], ex0[:], ALU.mult, r=[tk["kdir"], tk["ex0"]], w=[to]))
                S.act(ex1[:], lc[:], AF.Exp, scale=-KAPPA, r=[tk[tn[id(lc)]]], w=[tk["ex1"]])
                out_tile(g.PR[d][4][rows, :], lambda o, to: S.tt("dve", o[:], b[:], ex1[:], ALU.mult, r=[tk["b"], tk["ex1"]], w=[to]))
                out_tile(g.PR[d][5][rows, :], lambda o, to: S.tt("pool", o[:], kdir[:], ex1[:], ALU.mult, r=[tk["kdir"], tk["ex1"]], w=[to]))
            S.tt("dve", tl_["ex0"][:], tl_["bon"][:], tl_["v"][:], ALU.mult, r=[tk["bon"], tk["v"]], w=[tk["ex0"]])
            S.dma("sp", g.bonusT[rows, :], tl_["ex0"][:], r=[tk["ex0"]])
        S.barrier(); S.flush()


def rwkv_chunk_order(d):
    if d == 0:
        return list(range(NCH))
    return [3, 2, 1, 0] + list(range(NCH - 1, 3, -1))


def phase_rwkv_scan(g):
    nc, S, I = g.nc, g.S, g.I
    G = int(os.environ.get("RW_G", "3"))
    with ExitStack() as st:
        sb = lambda name, shape, dt=F32: st.enter_context(nc.sbuf_tensor(name, shape, dt))
        id64 = g.ident[0:64, 0:64]
        identb = sb("ridb", [128, 128], BF16)
        id64b = identb[0:64, 0:64]
        mask = [sb("rmask%d" % d, [64, 320]) for d in range(2)]
        t_k = Tok("rconst")
        S.copy("dve", identb[:], g.ident[:], r=[g.t_const], w=[t_k])
        for d in range(2):
            S.memset("pool", mask[d][:], 1.0, w=[t_k])
            for blk, strict in ((0, True), (1, False), (2, True), (3, False)):
                sgn = 1 if d == 0 else -1
                S.op("pool", lambda e, d=d, blk=blk, strict=strict, sgn=sgn: e.affine_select(
                    out=mask[d][:, blk * 64:(blk + 1) * 64], in_=mask[d][:, blk * 64:(blk + 1) * 64],
                    compare_op=ALU.is_ge, fill=0.0, base=-(1 if strict else 0), pattern=[[sgn, 64]], channel_multiplier=-sgn),
                    r=[t_k], w=[t_k])
            sgn = -1 if d == 0 else 1
            S.op("pool", lambda e, d=d, sgn=sgn: e.affine_select(
                out=mask[d][:, 256:320], in_=mask[d][:, 256:320], compare_op=ALU.is_ge, fill=0.0, base=-1,
                pattern=[[sgn, 64]], channel_multiplier=-sgn), r=[t_k], w=[t_k])
        fmraw = [sb("rfm%d" % q, [128, 2 * T], BF16) for q in range(7)]
        fm = [b[0:64, :].rearrange("p (h t) -> p h t", h=2) for b in fmraw]
        t_fm = toks(7, "rfm")
        wc2 = sb("rwc2", [64, 2, NCH]); t_wc = Tok("wc2"); t_bg = Tok("bg"); t_ofm = Tok("ofm")
        yacc = sb("ryacc", [64, NCH, 128]); t_y = toks(NCH, "yacc")
        bon = sb("rbon", [128, T])[:]; gat = sb("rgat", [128, T])[:]; ofm = sb("rofm", [128, T])[:]
        slots = []
        for s_ in range(G):
            d_ = {}
            d_["A"] = sb("rA%d" % s_, [64, 2, 320], BF16)
            for nm in ("tm", "T0", "T1", "P0", "P1", "Q0", "Q1", "AbT", "X0", "U0", "Rb", "Y0", "GT", "ZT"):
                d_[nm] = sb("r%s%d" % (nm, s_), [64, 2, 64], F32 if nm in ("Y0", "ZT") else BF16)
            d_["tk"] = {nm: Tok(nm + str(s_), nm.startswith("ps")) for nm in ("A", "tm", "T0", "T1", "P0", "P1", "Q0", "Q1", "AbT", "X0", "U0", "Rb", "Y0", "GT", "ZT", "ps0", "ps1")}
            d_["ps"] = [st.enter_context(nc.psum_tensor("rps%d_%d" % (s_, i), [64, 512], F32)) for i in range(2)]
            slots.append(d_)
        ptr = st.enter_context(nc.psum_tensor("rptr", [128, 1024], BF16)); t_ptr = Tok("ptr", True)
        prc = st.enter_context(nc.psum_tensor("rprc", [128, 512], F32)); t_prc = Tok("prc", True)
        gcols, t_gc = load_cols(g, st, "gncols", [I["rw_gn_w"].rearrange("(j p) -> j p", p=128), I["rw_gn_b"].rearrange("(j p) -> j p", p=128)],
                                ps=prc[:, 0:128], t_p=t_prc)
        ST = [sb("rST%d" % i, [64, 2, 64], BF16) for i in range(2)]; t_ST = toks(2, "ST")

        def chunk_gen(sl, d, c, state):
            tk = sl["tk"]; ps = sl["ps"]
            cs = slice(c * 64, (c + 1) * 64)
            Rt, At, Bt, Kt, Bh, Kh, V = fm
            tR, tA, tB, tK, tBh, tKh, tV = t_fm
            A = sl["A"]
            tmT = {}
            for nm, src, tsrc in (("AtT", At, tA), ("BhT", Bh, tBh), ("KhT", Kh, tKh), ("vT", V, tV)):
                pass
            bufs = {"AtT": sl["tm"], "BhT": sl["P1"], "KhT": sl["Q1"], "vT": sl["T1"]}
            btok = {"AtT": tk["tm"], "BhT": tk["P1"], "KhT": tk["Q1"], "vT": tk["T1"]}
            for i, (nm, src, tsrc) in enumerate((("AtT", At, tA), ("BhT", Bh, tBh), ("KhT", Kh, tKh), ("vT", V, tV))):
                if os.environ.get("RW_SKIP_TR"):
                    break
                for hh in range(2):
                    S.tr(ptr[0:64, i * 128 + hh * 64:i * 128 + (hh + 1) * 64], src[:, hh, cs], id64b, r=[tsrc, t_k], w=[t_ptr])
            for i, nm in enumerate(("AtT", "BhT", "KhT", "vT")):
                if os.environ.get("RW_SKIP_TR"):
                    break
                S.copy("act" if i % 2 else "dve", bufs[nm][:].rearrange("p a b -> p (a b)"), ptr[0:64, i * 128:(i + 1) * 128], r=[t_ptr], w=[btok[nm]])
            AtT, BhT, KhT, vT = bufs["AtT"], bufs["BhT"], bufs["KhT"], bufs["vT"]
            tAtT, tBhT, tKhT, tvT = btok["AtT"], btok["BhT"], btok["KhT"], btok["vT"]
            for hh in range(2):
                if os.environ.get("RW_SKIP_A"):
                    break
                ph = slice(hh * 64, (hh + 1) * 64)
                S.mm(ps[hh][:, 0:64], Bt[:, hh, cs], At[:, hh, cs], True, True, r=[tB, tA], w=[tk["ps%d" % hh]])
                S.mm(ps[hh][:, 64:128], Bt[:, hh, cs], Rt[:, hh, cs], True, True, r=[tB, tR], w=[tk["ps%d" % hh]])
                S.mm(ps[hh][:, 128:192], Kt[:, hh, cs], At[:, hh, cs], True, True, r=[tK, tA], w=[tk["ps%d" % hh]])
                S.mm(ps[hh][:, 192:256], Kt[:, hh, cs], Rt[:, hh, cs], True, True, r=[tK, tR], w=[tk["ps%d" % hh]])
                S.mm(ps[hh][:, 256:320], At[:, hh, cs], Bt[:, hh, cs], True, True, r=[tB, tA], w=[tk["ps%d" % hh]])
            for hh in range(2):
                S.tt("dve", A[:, hh, :], ps[hh][:, 0:320], mask[d][:], ALU.mult, r=[tk["ps%d" % hh], t_k], w=[tk["A"]])
                if os.environ.get("RW_FENCE"):
                    S.memset("dve", sl["GT"][0:1, 0, 0:1], 0.0, r=[tk["ps%d" % hh]], w=[tk["GT"]])
            yield
            Tc, tTc = sl["T0"], tk["T0"]
            for hh in range(2):
                S.tt("pool", Tc[:, hh, :], A[:, hh, 0:64], id64, ALU.add, r=[tk["A"], g.t_const], w=[tTc])
            Pc = (A, 0); PTc = (A, 256)
            tPc = tk["A"]; tPTc = tk["A"]
            pbuf = [(sl["P0"], tk["P0"]), (sl["Q0"], tk["Q0"])]
            pairs = [((sl["P0"], tk["P0"]), (sl["Q0"], tk["Q0"])), ((sl["AbT"], tk["AbT"]), (sl["X0"], tk["X0"]))]
            Tbufs = [(sl["T0"], tk["T0"]), (sl["U0"], tk["U0"])]
            ti = 0
            for j in range(1, 6):
                (Pn, tPn), (PTn, tPTn) = pairs[j % 2]
                def ap(x, hh):
                    t_, off = x
                    return t_[:, hh, off:off + 64]
                for hh in range(2):
                    if j < 5:
                        S.mm(ps[0][:, hh * 64:(hh + 1) * 64], ap(PTc, hh), ap(Pc, hh), True, True, r=[tPc, tPTc], w=[tk["ps0"]])
                    S.mm(ps[1][:, hh * 64:(hh + 1) * 64], ap(Pc, hh), ap(PTc, hh), True, True, r=[tPc, tPTc], w=[tk["ps1"]])
                if j < 5:
                    S.copy("dve", Pn[:].rearrange("p a b -> p (a b)"), ps[0][:, 0:128], r=[tk["ps0"]], w=[tPn])
                S.copy("act", PTn[:].rearrange("p a b -> p (a b)"), ps[1][:, 0:128], r=[tk["ps1"]], w=[tPTn])
                Pc, PTc, tPc, tPTc = (Pn, 0), (PTn, 0), tPn, tPTn
                yield
                Tcur, tTcur = Tbufs[ti]; Tnxt, tTnxt = Tbufs[1 - ti]
                for hh in range(2):
                    S.mm(ps[0][:, 128 + hh * 64:128 + (hh + 1) * 64], PTn[:, hh, :], Tcur[:, hh, :], True, True, r=[tPTn, tTcur], w=[tk["ps0"]])
                S.tt("dve", Tnxt[:].rearrange("p a b -> p (a b)"), ps[0][:, 128:256], Tcur[:].rearrange("p a b -> p (a b)"), ALU.add,
                     r=[tk["ps0"], tTcur], w=[tTnxt])
                ti = 1 - ti
                yield
            Tinv, tTinv = Tbufs[ti]
            AbT, tAbT = sl["P0"], tk["P0"]
            X0, tX0 = sl["Q0"], tk["Q0"]
            for hh in range(2):
                hc = slice(hh * 64, (hh + 1) * 64)
                S.mm(ps[0][:, 256 + hh * 64:256 + (hh + 1) * 64], Tinv[:, hh, :], AtT[:, hh, :], True, True, r=[tTinv, tAtT], w=[tk["ps0"]])
                S.mm(ps[1][:, 256 + hh * 64:256 + (hh + 1) * 64], A[:, hh, 128:192], vT[:, hh, :], True, True, r=[tk["A"], tvT], w=[tk["ps1"]])
            S.copy("dve", AbT[:].rearrange("p a b -> p (a b)"), ps[0][:, 256:384], r=[tk["ps0"]], w=[tAbT])
            S.copy("act", X0[:].rearrange("p a b -> p (a b)"), ps[1][:, 256:384], r=[tk["ps1"]], w=[tX0])
            for hh in range(2):
                S.mm(ps[0][:, hh * 64:(hh + 1) * 64], A[:, hh, 192:256], vT[:, hh, :], True, True, r=[tk["A"], tvT], w=[tk["ps0"]])
                S.mm(ps[1][:, hh * 64:(hh + 1) * 64], KhT[:, hh, :], vT[:, hh, :], True, True, r=[tKhT, tvT], w=[tk["ps1"]])
            S.copy("dve", sl["Y0"][:].rearrange("p a b -> p (a b)"), ps[0][:, 0:128], r=[tk["ps0"]], w=[tk["Y0"]])
            S.copy("act", sl["ZT"][:].rearrange("p a b -> p (a b)"), ps[1][:, 0:128], r=[tk["ps1"]], w=[tk["ZT"]])
            yield
            U0, tU0 = sl["AbT"], tk["AbT"]
            for hh in range(2):
                ph = slice(hh * 64, (hh + 1) * 64)
                S.mm(ps[0][:, 384 + hh * 64:384 + (hh + 1) * 64], Tinv[:, hh, :], X0[:, hh, :], True, True, r=[tTinv, tX0], w=[tk["ps0"]])
                S.mm(ps[1][:, 384 + hh * 64:384 + (hh + 1) * 64], AbT[:, hh, :], A[:, hh, 64:128], True, True, r=[tAbT, tk["A"]], w=[tk["ps1"]])
            S.copy("dve", U0[:].rearrange("p a b -> p (a b)"), ps[0][:, 384:512], r=[tk["ps0"]], w=[tU0])
            S.tt("dve", sl["Rb"][:], ps[1][:, 384:512].rearrange("p (a b) -> p a b", a=2), Rt[:, :, cs], ALU.add, r=[tk["ps1"], tR], w=[tk["Rb"]])
            yield
            for hh in range(2):
                o0 = hh * 64
                S.mm(ps[0][:, o0:o0 + 64], A[:, hh, 64:128], U0[:, hh, :], True, True, r=[tk["A"], tU0], w=[tk["ps0"]])
                S.mm(ps[1][:, o0:o0 + 64], AbT[:, hh, :], BhT[:, hh, :], True, True, r=[tAbT, tBhT], w=[tk["ps1"]])
                S.mm(ps[1][:, 128 + o0:128 + o0 + 64], BhT[:, hh, :], U0[:, hh, :], True, True, r=[tBhT, tU0], w=[tk["ps1"]])
            S.tt("dve", sl["Y0"][:].rearrange("p a b -> p (a b)"), ps[0][:, 0:128], sl["Y0"][:].rearrange("p a b -> p (a b)"), ALU.add,
                 r=[tk["ps0"], tk["Y0"]], w=[tk["Y0"]])
            for hh in range(2):
                S.stt(sl["GT"][:, hh, :], id64, wc2[:, hh, c:c + 1], ps[1][:, hh * 64:(hh + 1) * 64], ALU.mult, ALU.add,
                      r=[g.t_const, t_wc, tk["ps1"]], w=[tk["GT"]])
            S.tt("dve", sl["ZT"][:].rearrange("p a b -> p (a b)"), ps[1][:, 128:256], sl["ZT"][:].rearrange("p a b -> p (a b)"), ALU.add,
                 r=[tk["ps1"], tk["ZT"]], w=[tk["ZT"]])
            yield
            si = state["i"]
            Sc, tSc = ST[si], t_ST[si]; Sn, tSn = ST[1 - si], t_ST[1 - si]
            for hh in range(2):
                o0 = hh * 64
                S.mm(prc[0:64, o0:o0 + 64], sl["Rb"][:, hh, :], Sc[:, hh, :], True, True, r=[tk["Rb"], tSc], w=[t_prc])
                S.mm(prc[0:64, 128 + o0:128 + o0 + 64], sl["GT"][:, hh, :], Sc[:, hh, :], True, True, r=[tk["GT"], tSc], w=[t_prc])
            if d == 0:
                S.tt("dve", yacc[:, c, :], prc[0:64, 0:128], sl["Y0"][:].rearrange("p a b -> p (a b)"), ALU.add, r=[t_prc, tk["Y0"]], w=[t_y[c]])
            else:
                S.tt("dve", sl["Y0"][:].rearrange("p a b -> p (a b)"), prc[0:64, 0:128], sl["Y0"][:].rearrange("p a b -> p (a b)"), ALU.add,
                     r=[t_prc, tk["Y0"]], w=[tk["Y0"]])
                S.tt("pool", yacc[:, c, :], yacc[:, c, :], sl["Y0"][:].rearrange("p a b -> p (a b)"), ALU.add, r=[tk["Y0"], t_y[c]], w=[t_y[c]])
            S.tt("dve", Sn[:].rearrange("p a b -> p (a b)"), prc[0:64, 128:256], sl["ZT"][:].rearrange("p a b -> p (a b)"), ALU.add,
                 r=[t_prc, tk["ZT"]], w=[tSn])
            state["i"] = 1 - si
            yield

        DBG_NTL = int(os.environ.get("RW_TL", "4")); DBG_ND = int(os.environ.get("RW_D", "2"))
        DBG_NCH = int(os.environ.get("RW_NCH", "99")); DBG_STG = int(os.environ.get("RW_STG", "99"))
        for tl in range(DBG_NTL):
            rows = slice(tl * 128, (tl + 1) * 128)
            for d in range(DBG_ND):
                for q in range(6):
                    S.dma("sp" if q % 2 else "act", fm[q], g.PR[d][q][rows, :].rearrange("(h k) t -> k h t", h=2), w=[t_fm[q]])
                S.dma("sp", fm[6], g.vS[rows, :].rearrange("(h k) t -> k h t", h=2), w=[t_fm[6]])
                S.dma("sp", wc2[:], g.WCdd[d][rows, :].rearrange("(hh k) c -> k hh c", hh=2), w=[t_wc])
                state = {"i": 0}
                S.memset("pool", ST[0][:], 0.0, w=[t_ST[0]])
                order = rwkv_chunk_order(d)[:DBG_NCH]
                if os.environ.get('RW_SAME'):
                    order = [int(x) for x in os.environ['RW_SAME'].split(',')]
                gens = []
                nxt = 0
                slot_free = list(range(G))
                active = []
                while nxt < len(order) or active:
                    if nxt < len(order) and slot_free:
                        s_ = slot_free.pop(0)
                        import itertools
                        active.append((itertools.islice(chunk_gen(slots[s_], d, order[nxt], state), DBG_STG), s_))
                        nxt += 1
                    still = []
                    for gen, s_ in active:
                        try:
                            next(gen)
                            still.append((gen, s_))
                        except StopIteration:
                            slot_free.append(s_)
                    active = still
            S.dma("sp", bon, g.bonusT[rows, :], w=[t_bg])
            S.dma("act", gat, g.gateT[rows, :], w=[t_bg])
            stat = sb("rstat%d" % tl, [64, NCH * 2, 4]); t_st = Tok("stat")
            sqb = sb("rsq%d" % tl, [64, 128]); t_sq = Tok("sq")
            y4 = yacc[:].rearrange("p c (h v) -> p (c h) v", h=2)
            for c in range(NCH):
                S.reduce(stat[:, 2 * c:2 * c + 2, 0], y4[:, 2 * c:2 * c + 2, :], ALU.add, r=[t_y[c]], w=[t_st])
                S.act(sqb[:], yacc[:, c, :], AF.Square, r=[t_y[c]], w=[t_sq])
                S.reduce(stat[:, 2 * c:2 * c + 2, 1], sqb[:].rearrange("p (h v) -> p h v", h=2), ALU.add, r=[t_sq], w=[t_st])
            S.ts("dve", stat[:, :, 0], stat[:, :, 0], 1.0 / 64, None, ALU.mult, r=[t_st], w=[t_st])
            S.tt("dve", stat[:, :, 2], stat[:, :, 0], stat[:, :, 0], ALU.mult, r=[t_st], w=[t_st])
            S.stt(stat[:, :, 3], stat[:, :, 1], 1.0 / 64, stat[:, :, 2], ALU.mult, ALU.subtract, r=[t_st], w=[t_st])
            S.ts("dve", stat[:, :, 3], stat[:, :, 3], 64e-5, None, ALU.add, r=[t_st], w=[t_st])
            S.act(stat[:, :, 3], stat[:, :, 3], AF.Sqrt, r=[t_st], w=[t_st])
            S.recip(stat[:, :, 3], stat[:, :, 3], r=[t_st], w=[t_st])
            for c in range(NCH):
                for hh in range(2):
                    j = 2 * c + hh
                    S.ts("dve" if hh else "pool", yacc[:, c, hh * 64:(hh + 1) * 64], yacc[:, c, hh * 64:(hh + 1) * 64],
                         stat[:, j, 0:1], stat[:, j, 3:4], ALU.subtract, ALU.mult, r=[t_y[c], t_st], w=[t_y[c]])
            for c8 in range(0, NCH, 8):
                nn = min(8, NCH - c8)
                for c in range(c8, c8 + nn):
                    S.tr(prc[:, (c - c8) * 64:(c - c8 + 1) * 64], yacc[:, c, :], id64, r=[t_y[c], g.t_const], w=[t_prc])
                cs8 = slice(c8 * 64, (c8 + nn) * 64)
                S.ts("dve", ofm[:, cs8], prc[:, 0:nn * 64], gcols[:, tl:tl + 1], gcols[:, 4 + tl:5 + tl], ALU.mult, ALU.add,
                     r=[t_prc, t_gc], w=[t_ofm])
            S.tt("pool", ofm, ofm, bon, ALU.add, r=[t_ofm, t_bg], w=[t_ofm])
            S.tt("dve", ofm, ofm, gat, ALU.mult, r=[t_ofm, t_bg], w=[t_ofm])
            S.dma("sp", g.catT[rows, :], ofm, r=[t_ofm])
            if "yrw_o" in g.dbg:
                pass
        S.barrier(); S.flush()


MB0 = RW_COLS
NC2 = T // 128
NEG = -30000.0


def ssd_chunk_order(d):
    if d == 0:
        return list(range(NC2))
    return [1, 0] + list(range(NC2 - 1, 1, -1))


def alloc_ssd_scratch(g):
    nc = g.nc
    def dscr(name, shape):
        kind = {"kind": "ExternalOutput"} if name in g.dbg else {}
        return nc.dram_tensor(name, list(shape), F32, **kind).ap()
    g.xbcS = dscr("xbcS", (1024, T))
    g.ssd16 = dscr("ssd16", (4, 16, T))


def phase_ssd_prep(g):
    nc, S, I = g.nc, g.S, g.I
    with ExitStack() as st:
        sb = lambda name, shape, dt=F32: st.enter_context(nc.sbuf_tensor(name, shape, dt))
        cols, t_cols = load_cols(g, st, "ccols", [I["mb_conv_w"].rearrange("j (t p) -> (j t) p", p=128),
                                                  I["mb_conv_b"].rearrange("(t p) -> t p", p=128)])
        c16, t_c16 = load_cols(g, st, "c16", [I["mb_dt_bias"].rearrange("(o p) -> o p", o=1), I["mb_a_log"].rearrange("(o p) -> o p", o=1)])
        zin = [sb("szin%d" % i, [128, T]) for i in range(2)]; t_zin = toks(2, "szin")
        acc = [sb("sacc%d" % i, [128, T]) for i in range(2)]; t_acc = toks(2, "sacc")
        for tl in range(8):
            b = tl % 2
            S.dma("sp" if b else "act", zin[b][:], g.zT[MB0 + 512 + tl * 128:MB0 + 512 + (tl + 1) * 128, :], w=[t_zin[b]])
            S.act(acc[b][:], zin[b][:], AF.Identity, bias=cols[:, 40 + tl:41 + tl], scale=cols[:, 16 + tl:17 + tl],
                  r=[t_zin[b], t_cols], w=[t_acc[b]])
            for j in (0, 1, 3, 4):
                sh = j - 2
                for a, e_ in SEGS:
                    if sh < 0:
                        o_sl, i_sl = slice(a - sh, e_), slice(a, e_ + sh)
                    else:
                        o_sl, i_sl = slice(a, e_ - sh), slice(a + sh, e_)
                    S.stt(acc[b][:, o_sl], zin[b][:, i_sl], cols[:, j * 8 + tl:j * 8 + tl + 1], acc[b][:, o_sl], ALU.mult, ALU.add,
                          r=[t_zin[b], t_cols, t_acc[b]], w=[t_acc[b]])
            S.act(acc[b][:], acc[b][:], AF.Silu, r=[t_acc[b]], w=[t_acc[b]])
            S.dma("sp", g.xbcS[tl * 128:(tl + 1) * 128, :], acc[b][:], r=[t_acc[b]])
        x16 = sb("sx16", [16, T]); ax = sb("sax", [16, T]); dt16 = sb("sdt16", [16, T]); A16 = sb("sA16", [16, T])
        P16 = sb("sP16", [16, T]); Q16 = sb("sQ16", [16, T]); m128 = sb("sm128", [16, T]); tot = sb("stot", [16, NC2])
        aneg = sb("saneg", [16, 1])
        t16 = Tok("s16")
        S.dma("sp", x16[:], g.zT[MB0 + 1536:MB0 + 1552, :], w=[t16])
        S.act(aneg[:], c16[0:16, 1:2], AF.Exp, r=[t_c16], w=[t16])
        S.ts("dve", aneg[:], aneg[:], -1.0, None, ALU.mult, r=[t16], w=[t16])
        S.ts("dve", x16[:], x16[:], c16[0:16, 0:1], None, ALU.add, r=[t16, t_c16], w=[t16])
        S.ts("dve", ax[:], x16[:], -1.0, None, ALU.mult, r=[t16], w=[t16])
        S.tt("dve", ax[:], ax[:], x16[:], ALU.min, r=[t16], w=[t16])
        S.act(ax[:], ax[:], AF.Exp, r=[t16], w=[t16])
        S.act(ax[:], ax[:], AF.Ln, bias=1.0, scale=1.0, r=[t16], w=[t16])
        S.ts("dve", x16[:], x16[:], 0.0, None, ALU.max, r=[t16], w=[t16])
        S.tt("dve", dt16[:], x16[:], ax[:], ALU.add, r=[t16], w=[t16])
        S.ts("dve", A16[:], dt16[:], aneg[:, 0:1], None, ALU.mult, r=[t16], w=[t16])
        S.memset("pool", m128[:], 1.0, w=[t16])
        S.memset("pool", m128[:].rearrange("p (c j) -> p c j", j=128)[:, :, 0:1], 0.0, w=[t16])
        S.op("dve", lambda e: e.tensor_tensor_scan(out=P16[:], data0=m128[:], data1=A16[:], initial=0.0, op0=ALU.mult, op1=ALU.add),
             r=[t16], w=[t16])
        S.copy("dve", tot[:], P16[:].rearrange("p (c j) -> p c j", j=128)[:, :, 127], r=[t16], w=[t16])
        v3 = lambda a: a[:].rearrange("p (c j) -> p c j", j=128)
        S.tt("dve", v3(Q16), tot[:].unsqueeze(2).to_broadcast([16, NC2, 128]), v3(P16), ALU.subtract, r=[t16], w=[t16])
        S.tt("dve", Q16[:], Q16[:], A16[:], ALU.add, r=[t16], w=[t16])
        S.dma("sp", g.ssd16[0], dt16[:], r=[t16])
        S.dma("sp", g.ssd16[1], P16[:], r=[t16])
        S.dma("sp", g.ssd16[2], Q16[:], r=[t16])
        S.barrier(); S.flush()


def phase_ssd_scan(g):
    nc, S, I = g.nc, g.S, g.I
    with ExitStack() as st:
        sb = lambda name, shape, dt=F32: st.enter_context(nc.sbuf_tensor(name, shape, dt))
        pst = lambda name, shape: st.enter_context(nc.psum_tensor(name, shape, F32))
        x_tok = sb("mxtok", [128, NC2, 512]); t_xt = Tok("xtok")
        yacc = sb("myacc", [128, NC2, 512]); t_y = toks(NC2, "my")
        Bfm = sb("mBfm", [128, 2, T]); Cfm = sb("mCfm", [128, 2, T]); t_bc = Tok("bcfm")
        cum16 = sb("mcum16", [16, T]); t_cum = Tok("cum16")
        colT = sb("mcolT", [128, 3, NC2, 16]); t_colT = Tok("colT")
        Sel = sb("mSel", [16, 16, 128]); Mneg = sb("mMneg", [128, 2, 128]); Dbc = sb("mDbc", [128, 8]); t_k = Tok("mconst")
        psT = pst("mpsT", [128, 512]); t_psT = Tok("mpsT", True)
        psP = [pst("mpsP%d" % i, [128, 512]) for i in range(2)]; t_psP = toks(2, "mpsP", True)
        psG = pst("mpsG", [128, 512]); t_psG = Tok("mpsG", True)
        psY = pst("mpsY", [128, 512]); t_psY = Tok("mpsY", True)
        psS = pst("mpsS", [128, 512]); t_psS = Tok("mpsS", True)
        ncols, t_nc = load_cols(g, st, "mncols", [I["mb_norm_w"].rearrange("(j p) -> j p", p=128)], ps=psT[:, 0:128], t_p=t_psT)
        S.copy("dve", Sel[:], g.ident[0:16, 0:16].unsqueeze(2).to_broadcast([16, 16, 128]), r=[g.t_const], w=[t_k])
        S.memset("pool", Mneg[:], 0.0, w=[t_k])
        S.op("pool", lambda e: e.affine_select(out=Mneg[:, 0, :], in_=Mneg[:, 0, :], compare_op=ALU.is_ge, fill=NEG, base=0,
                                                pattern=[[1, 128]], channel_multiplier=-1), r=[t_k], w=[t_k])
        S.op("pool", lambda e: e.affine_select(out=Mneg[:, 1, :], in_=Mneg[:, 1, :], compare_op=ALU.is_ge, fill=NEG, base=0,
                                                pattern=[[-1, 128]], channel_multiplier=1), r=[t_k], w=[t_k])
        drow = sb("mdrow", [1, 8])
        S.dma("sp", drow[:], I["mb_d"].rearrange("(o h) -> o h", o=1), w=[t_k])
        S.mm(psT[:, 0:8], g.ones[0:1, :], drow[:], True, True, r=[t_k, g.t_const], w=[t_psT])
        S.copy("dve", Dbc[:], psT[:, 0:8], r=[t_psT], w=[t_k])
        S.dma("sp", Bfm[:], g.xbcS[512:768, :].rearrange("(g p) t -> p g t", p=128), w=[t_bc])
        S.dma("act", Cfm[:], g.xbcS[768:1024, :].rearrange("(g p) t -> p g t", p=128), w=[t_bc])
        S.dma("sp", cum16[0:8, :], g.ssd16[1][0:8, :], w=[t_cum])
        S.dma("sp", cum16[8:16, :], g.ssd16[2][8:16, :], w=[t_cum])
        with ExitStack() as st2:
            sb2 = lambda name, shape: st2.enter_context(nc.sbuf_tensor(name, shape, F32))
            xfm = sb2("mxfm", [128, T]); t_xfm = Tok("xfm")
            dt16 = sb2("mdt16", [16, T]); e16 = sb2("me16", [16, T]); t_16 = Tok("m16")
            tot = sb2("mtot", [16, NC2]); dm = sb2("mdm", [16, 2])
            for tl in range(4):
                S.dma("sp", xfm[:], g.xbcS[tl * 128:(tl + 1) * 128, :], w=[t_xfm])
                for c4 in range(0, NC2, 4):
                    nn = min(4, NC2 - c4)
                    for c in range(c4, c4 + nn):
                        S.tr(psT[:, (c - c4) * 128:(c - c4 + 1) * 128], xfm[:, c * 128:(c + 1) * 128], g.ident[:], r=[t_xfm, g.t_const], w=[t_psT])
                    S.copy("act" if (c4 // 4) % 2 else "dve", x_tok[:, c4:c4 + nn, tl * 128:(tl + 1) * 128],
                           psT[:, 0:nn * 128].rearrange("p (c k) -> p c k", k=128), r=[t_psT], w=[t_xt])
            S.dma("sp", dt16[:], g.ssd16[0], w=[t_16])
            S.memset("pool", dm[:], 1.0, w=[t_16])
            S.op("pool", lambda e: e.affine_select(out=dm[:, 0:1], in_=dm[:, 0:1], compare_op=ALU.is_ge, fill=0.0, base=7,
                                                    pattern=[[0, 1]], channel_multiplier=-1), r=[t_16], w=[t_16])
            S.op("pool", lambda e: e.affine_select(out=dm[:, 1:2], in_=dm[:, 1:2], compare_op=ALU.is_ge, fill=0.0, base=-8,
                                                    pattern=[[0, 1]], channel_multiplier=1), r=[t_16], w=[t_16])
            c3 = cum16[:].rearrange("p (c j) -> p c j", j=128)
            S.ts("dve", tot[:], c3[:, :, 127], dm[:, 0:1], None, ALU.mult, r=[t_cum, t_16], w=[t_16])
            S.stt(tot[:], c3[:, :, 0], dm[:, 1:2], tot[:], ALU.mult, ALU.add, r=[t_cum, t_16], w=[t_16])
            S.tt("dve", e16[:].rearrange("p (c j) -> p c j", j=128), tot[:].unsqueeze(2).to_broadcast([16, NC2, 128]), c3, ALU.subtract,
                 r=[t_cum, t_16], w=[t_16])
            S.act(e16[:], e16[:], AF.Exp, r=[t_16], w=[t_16])
            for qi, (src, tsrc) in enumerate(((dt16, t_16), (cum16, t_cum), (e16, t_16))):
                for c in range(NC2):
                    S.tr(psT[:, c * 16:(c + 1) * 16], src[:, c * 128:(c + 1) * 128], g.ident[0:16, 0:16], r=[tsrc, g.t_const], w=[t_psT])
                S.copy("dve", colT[:, qi, :, :], psT[:, 0:NC2 * 16].rearrange("p (c j) -> p c j", j=16), r=[t_psT], w=[t_colT])
            S.barrier(); S.flush()
        E = sb("mE", [128, 8, 128]); eP = sb("meP", [128, 8, 128]); M = sb("mM", [128, 8, 128]); Ct = sb("mCt", [128, 8, 128])
        t_E, t_eP, t_M, t_Ct = Tok("E"), Tok("eP"), Tok("M"), Tok("Ct")
        Xd = [sb("mXd%d" % i, [128, 8, 64]) for i in range(2)]; Xdd = [sb("mXdd%d" % i, [128, 8, 64]) for i in range(2)]
        Gsb = [sb("mG%d" % i, [128, 2, 128]) for i in range(2)]; Btok = [sb("mBt%d" % i, [128, 2, 128]) for i in range(2)]
        t_Xd, t_Xdd, t_G, t_Bt = toks(2, "Xd"), toks(2, "Xdd"), toks(2, "G"), toks(2, "Bt")
        ST = [sb("mST%d" % i, [128, 8, 64]) for i in range(2)]; t_ST = toks(2, "mST")
        tmpS = sb("mtmpS", [128, 8, 64]); t_tmpS = Tok("tmpS")
        n = 0
        for d in range(2):
            si = 0
            S.memset("pool", ST[0][:], 0.0, w=[t_ST[0]])
            hs = slice(d * 8, (d + 1) * 8)
            for c in ssd_chunk_order(d):
                b = n % 2; n += 1
                cs = slice(c * 128, (c + 1) * 128)
                for h in range(8):
                    S.mm(psP[h // 4][:, (h % 4) * 128:(h % 4 + 1) * 128], Sel[:, d * 8 + h, :], cum16[:, cs], True, True,
                         r=[t_k, t_cum], w=[t_psP[h // 4]])
                for g_ in range(2):
                    S.mm(psG[:, g_ * 128:(g_ + 1) * 128], Bfm[:, g_, cs], Cfm[:, g_, cs], True, True, r=[t_bc], w=[t_psG])
                    S.tr(psG[:, 256 + g_ * 128:256 + (g_ + 1) * 128], Bfm[:, g_, cs], g.ident[:], r=[t_bc, g.t_const], w=[t_psG])
                S.copy("act", Gsb[b][:].rearrange("p a b -> p (a b)"), psG[:, 0:256], r=[t_psG], w=[t_G[b]])
                S.copy("act", Btok[b][:].rearrange("p a b -> p (a b)"), psG[:, 256:512], r=[t_psG], w=[t_Bt[b]])
                for hb in range(2):
                    h4 = slice(hb * 4, hb * 4 + 4)
                    pv = psP[hb][:].rearrange("p (h t) -> p h t", t=128)
                    S.tt("dve", E[:, h4, :], pv, Mneg[:, d, :].unsqueeze(1).to_broadcast([128, 4, 128]), ALU.add, r=[t_psP[hb], t_k], w=[t_E])
                    S.act(eP[:, h4, :], pv, AF.Exp, r=[t_psP[hb]], w=[t_eP])
                S.tt("pool", E[:], E[:], colT[:, 1, c, hs].unsqueeze(2).to_broadcast([128, 8, 128]), ALU.subtract, r=[t_E, t_colT], w=[t_E])
                S.act(E[:], E[:], AF.Exp, r=[t_E], w=[t_E])
                for g_ in range(2):
                    h4 = slice(g_ * 4, g_ * 4 + 4)
                    S.tt("dve", M[:, h4, :], E[:, h4, :], Gsb[b][:, g_, :].unsqueeze(1).to_broadcast([128, 4, 128]), ALU.mult, r=[t_E, t_G[b]], w=[t_M])
                    S.tt("pool", Ct[:, h4, :], eP[:, h4, :], Cfm[:, g_, cs].unsqueeze(1).to_broadcast([128, 4, 128]), ALU.mult, r=[t_eP, t_bc], w=[t_Ct])
                xv = x_tok[:, c, :].rearrange("p (h k) -> p h k", k=64)
                S.tt("dve", Xd[b][:], xv, colT[:, 0, c, hs].unsqueeze(2).to_broadcast([128, 8, 64]), ALU.mult, r=[t_xt, t_colT], w=[t_Xd[b]])
                S.tt("pool", Xdd[b][:], Xd[b][:], colT[:, 2, c, hs].unsqueeze(2).to_broadcast([128, 8, 64]), ALU.mult, r=[t_Xd[b], t_colT], w=[t_Xdd[b]])
                Sc, tSc = ST[si], t_ST[si]; Sn, tSn = ST[1 - si], t_ST[1 - si]
                for h in range(8):
                    g_ = h // 4
                    S.mm(psY[:, h * 64:(h + 1) * 64], M[:, h, :], Xd[b][:, h, :], True, False, r=[t_M, t_Xd[b]], w=[t_psY])
                    S.mm(psY[:, h * 64:(h + 1) * 64], Ct[:, h, :], Sc[:, h, :], False, True, r=[t_Ct, tSc], w=[t_psY])
                    S.mm(psS[:, h * 64:(h + 1) * 64], Btok[b][:, g_, :], Xdd[b][:, h, :], True, True, r=[t_Bt[b], t_Xdd[b]], w=[t_psS])
                if d == 0:
                    S.copy("act", yacc[:, c, :], psY[:], r=[t_psY], w=[t_y[c]])
                else:
                    S.tt("dve", yacc[:, c, :], yacc[:, c, :], psY[:], ALU.add, r=[t_psY, t_y[c]], w=[t_y[c]])
                ecol = 127 if d == 0 else 0
                S.tt("pool", tmpS[:], Sc[:], eP[:, :, ecol:ecol + 1].to_broadcast([128, 8, 64]), ALU.mult, r=[tSc, t_eP], w=[t_tmpS])
                S.tt("dve", Sn[:].rearrange("p a b -> p (a b)"), psS[:], tmpS[:].rearrange("p a b -> p (a b)"), ALU.add, r=[t_psS, t_tmpS], w=[tSn])
                si = 1 - si
        zg = [sb("mzg%d" % i, [128, 4, 128]) for i in range(2)]; t_zg = toks(2, "zg")
        zs = sb("mzs", [128, 512]); sq = sb("msq", [128, 512]); ofm = [sb("mofm%d" % i, [128, 4, 128]) for i in range(2)]
        ms = sb("mms", [128, 2]); t_zs, t_sq, t_ms = Tok("zs"), Tok("sq"), Tok("ms"); t_ofm = toks(2, "mofm")
        for c in range(NC2):
            b = c % 2
            cs = slice(c * 128, (c + 1) * 128)
            S.dma("sp", zg[b][:], g.zT[MB0:MB0 + 512, cs].rearrange("(tl p) t -> p tl t", p=128), w=[t_zg[b]])
            for tl in range(4):
                S.tr(psT[:, tl * 128:(tl + 1) * 128], zg[b][:, tl, :], g.ident[:], r=[t_zg[b], g.t_const], w=[t_psT])
            S.act(zs[:], psT[:], AF.Silu, r=[t_psT], w=[t_zs])
            yv = yacc[:, c, :]
            S.tt("pool", sq[:].rearrange("p (h k) -> p h k", k=64), x_tok[:, c, :].rearrange("p (h k) -> p h k", k=64),
                 Dbc[:].unsqueeze(2).to_broadcast([128, 8, 64]), ALU.mult, r=[t_xt, t_k], w=[t_sq])
            S.tt("dve", yv, yv, sq[:], ALU.add, r=[t_y[c], t_sq], w=[t_y[c]])
            S.tt("dve", yv, yv, zs[:], ALU.mult, r=[t_y[c], t_zs], w=[t_y[c]])
            S.act(sq[:], yv, AF.Square, r=[t_y[c]], w=[t_sq])
            S.reduce(ms[:], sq[:].rearrange("p (g k) -> p g k", g=2), ALU.add, r=[t_sq], w=[t_ms])
            S.ts("dve", ms[:], ms[:], 1.0 / 256, NORM_EPS, ALU.mult, ALU.add, r=[t_ms], w=[t_ms])
            S.act(ms[:], ms[:], AF.Sqrt, r=[t_ms], w=[t_ms])
            S.recip(ms[:], ms[:], r=[t_ms], w=[t_ms])
            S.tt("dve", yv.rearrange("p (g k) -> p g k", g=2), yv.rearrange("p (g k) -> p g k", g=2),
                 ms[:].unsqueeze(2).to_broadcast([128, 2, 256]), ALU.mult, r=[t_y[c], t_ms], w=[t_y[c]])
            for tl in range(4):
                S.tr(psG[:, tl * 128:(tl + 1) * 128], yacc[:, c, tl * 128:(tl + 1) * 128], g.ident[:], r=[t_y[c], g.t_const], w=[t_psG])
            for tl in range(4):
                S.ts("dve" if tl % 2 else "act_", ofm[b][:, tl, :], psG[:, tl * 128:(tl + 1) * 128], ncols[:, tl:tl + 1], None, ALU.mult,
                     r=[t_psG, t_nc], w=[t_ofm[b]])
            S.dma("sp", g.catT[512:1024, cs].rearrange("(tl p) t -> p tl t", p=128), ofm[b][:], r=[t_ofm[b]])
        S.barrier(); S.flush()


FGROUPS = [(i * 3, min(3, 22 - i * 3)) for i in range(8)]


def tok_chunks(n_tok):
    out = []
    c0 = 0
    while c0 < n_tok:
        n = min(512, n_tok - c0)
        out.append((c0, n))
        c0 += n
    return out


def swiglu_pass(g, st, aT, t_aT, h, t_h, NT, w1d, w3d, w2d, gate_col, tag, gb=None, t_gb=None, pools=None, chunks=None):
    nc, S = g.nc, g.S
    if pools is None:
        pools = {}
        sb = lambda name, shape, dt=F32: st.enter_context(nc.sbuf_tensor(name + tag, shape, dt))
        pools["w1"] = [sb("fw1_%d" % i, [128, 8, 384], BF16) for i in range(2)]
        pools["w3"] = [sb("fw3_%d" % i, [128, 8, 384], BF16) for i in range(2)]
        pools["w2"] = [sb("fw2_%d" % i, [128, 3, 1024], BF16) for i in range(2)]
        pools["t_w"] = toks(2, "fw")
        pools["gT"] = [sb("fgT%d" % i, [128, 3, 512], BF16) for i in range(2)]
        pools["t_gT"] = toks(2, "fgT")
        pools["sl"] = [sb("fsl%d" % i, [128, 512]) for i in range(2)]
        pools["t_sl"] = toks(2, "fsl")
        pools["cst"] = Caster(g, st, "fcst" + tag, 1024, nbuf=3)
        pools["ph"] = [st.enter_context(nc.psum_tensor("fph%d%s" % (i, tag), [128, 512], F32)) for i in range(4)]
        pools["t_ph"] = toks(4, "fph", True)
        pools["po"] = [st.enter_context(nc.psum_tensor("fpo%d%s" % (i, tag), [128, 512], F32)) for i in range(3)]
        pools["t_po"] = toks(3, "fpo", True)
        pools["n"] = {"w": 0, "g": 0, "ph": 0, "po": 0, "sl": 0}
    P = pools
    cst = P["cst"]
    w1v = w1d.rearrange("(k p) f -> p k f", p=128)
    w3v = w3d.rearrange("(k p) f -> p k f", p=128)
    w2v = w2d.rearrange("(k p) f -> p k f", p=128)
    chunks = chunks or tok_chunks(NT)
    for (f0, nf) in FGROUPS:
        wb = P["n"]["w"] % 2; P["n"]["w"] += 1
        tw = P["t_w"][wb]
        for k in range(8):
            cst.load(P["w1"][wb][:, k, :nf * 128], w1v[:, k, f0 * 128:(f0 + nf) * 128], tw)
            cst.load(P["w3"][wb][:, k, :nf * 128], w3v[:, k, f0 * 128:(f0 + nf) * 128], tw)
        for j in range(nf):
            cst.load(P["w2"][wb][:, j, :], w2v[:, f0 + j, :], tw)
        for (c0, n) in chunks:
            gi = P["n"]["g"] % 2; P["n"]["g"] += 1
            gT, tgT = P["gT"][gi], P["t_gT"][gi]
            for j in range(nf):
                i1 = P["n"]["ph"] % 4; i3 = (P["n"]["ph"] + 1) % 4; P["n"]["ph"] += 2
                p1, p3, t1, t3 = P["ph"][i1], P["ph"][i3], P["t_ph"][i1], P["t_ph"][i3]
                for k in range(8):
                    S.mm(p1[:, :n], P["w1"][wb][:, k, j * 128:(j + 1) * 128], aT[:, k, c0:c0 + n], k == 0, k == 7, r=[tw, t_aT], w=[t1])
                for k in range(8):
                    S.mm(p3[:, :n], P["w3"][wb][:, k, j * 128:(j + 1) * 128], aT[:, k, c0:c0 + n], k == 0, k == 7, r=[tw, t_aT], w=[t3])
                si = P["n"]["sl"] % 2; P["n"]["sl"] += 1
                sl, tsl = P["sl"][si], P["t_sl"][si]
                S.act(sl[:, :n], p1[:, :n], AF.Silu, r=[t1], w=[tsl])
                if gb is not None:
                    S.tt("pool", sl[:, :n], sl[:, :n], gb[:, c0:c0 + n], ALU.mult, r=[tsl, t_gb], w=[tsl])
                S.tt("dve", gT[:, j, :n], p3[:, :n], sl[:, :n], ALU.mult, r=[t3, tsl], w=[tgT])
            for dt_ in range(8):
                oi = P["n"]["po"] % 3; P["n"]["po"] += 1
                po, tpo = P["po"][oi], P["t_po"][oi]
                for j in range(nf):
                    S.mm(po[:, :n], P["w2"][wb][:, j, dt_ * 128:(dt_ + 1) * 128], gT[:, j, :n], j == 0, j == nf - 1, r=[tw, tgT], w=[tpo])
                S.stt(h[:, dt_, c0:c0 + n], po[:, :n], gate_col(c0)[:, dt_:dt_ + 1], h[:, dt_, c0:c0 + n], ALU.mult, ALU.add,
                      r=[tpo, t_h, g.t_mod], w=[t_h])
    return pools


def phase_l0_post(g):
    nc, S, I = g.nc, g.S, g.I
    with ExitStack() as st:
        sb = lambda name, shape, dt=F32: st.enter_context(nc.sbuf_tensor(name, shape, dt))
        h = sb("qh", [128, 8, T]); t_h = Tok("qh")
        aT = sb("qaT", [128, 8, T], BF16); t_aT = Tok("qaT")
        hTd = g.hT.rearrange("(c p) t -> p c t", p=128)
        for c in range(8):
            S.dma("sp" if c % 2 else "act", h[:, c, :], hTd[:, c, :], w=[t_h])
        with ExitStack() as st2:
            sb2 = lambda name, shape, dt=F32: st2.enter_context(nc.sbuf_tensor(name, shape, dt))
            wout = sb2("qwout", [128, 8, D], BF16); t_wout = Tok("qwout")
            cst = Caster(g, st2, "qcst", D, nbuf=2)
            catb = [sb2("qcatb%d" % i, [128, 8, 512], BF16) for i in range(2)]; t_catb = toks(2, "qcatb")
            cst2 = Caster(g, st2, "qcst2", 512, nbuf=3)
            po = [st2.enter_context(nc.psum_tensor("qpo%d" % i, [128, 512], F32)) for i in range(3)]; t_po = toks(3, "qpo", True)
            npool = norm_pools(nc, st2, "q")
            wv = I["mix_w_out"].rearrange("(k p) f -> p k f", p=128)
            for k in range(8):
                cst.load(wout[:, k, :], wv[:, k, :], t_wout)
            catv = g.catT.rearrange("(k p) t -> p k t", p=128)
            npo = 0
            for ci, (c0, n) in enumerate(CHUNKS):
                b = ci % 2
                w_ = 1 if ci == 0 else 0
                for k in range(8):
                    cst2.load(catb[b][:, k, :n], catv[:, k, c0:c0 + n], t_catb[b])
                for ft in range(8):
                    p = po[npo % 3]; tp = t_po[npo % 3]; npo += 1
                    for k in range(8):
                        S.mm(p[:, :n], wout[:, k, ft * 128:(ft + 1) * 128], catb[b][:, k, :n], k == 0, k == 7, r=[t_wout, t_catb[b]], w=[tp])
                    S.stt(h[:, ft, c0:c0 + n], p[:, :n], g.modT[:, 0, w_, 16 + ft:17 + ft], h[:, ft, c0:c0 + n], ALU.mult, ALU.add,
                          r=[tp, t_h, g.t_mod], w=[t_h])
                norm_mod(g, h[:, :, c0:c0 + n], n, g.Gf[:, 0, w_, :], g.modT[:, 0, w_, 24:32], npool, t_h, aT[:, :, c0:c0 + n], t_aT)
            if "h0mix_o" in g.dbg:
                dd = nc.dram_tensor("h0mixT", [D, T], F32, kind="ExternalOutput").ap()
                S.dma("sp", dd.rearrange("(c p) t -> p c t", p=128), h[:], r=[t_h])
            S.barrier(); S.flush()
        gate_col = lambda c0: g.modT[:, 0, 1 if c0 < TC else 0, 40:48]
        swiglu_pass(g, st, aT, t_aT, h, t_h, T, I["ffn_w1"], I["ffn_w3"], I["ffn_w2"], gate_col, "q", chunks=CHUNKS)
        for c in range(8):
            S.dma("sp" if c % 2 else "act", hTd[:, c, :], h[:, c, :], r=[t_h])
        S.barrier(); S.flush()


def alloc_l1_scratch(g):
    nc = g.nc
    def dscr(name, shape, dt):
        kind = {"kind": "ExternalOutput"} if name in g.dbg else {}
        return nc.dram_tensor(name, list(shape), dt, **kind).ap()
    g.qT = dscr("qT", (D, TL), BF16)
    g.kT = dscr("kT", (D, T), BF16)
    g.vtok = dscr("vtok", (T, D), BF16)
    g.oT = dscr("oT", (D, TL), BF16)
    g.rpbpad = dscr("rpbpad", (16, 15, 127), F32)


def phase_l1_qkv(g):
    nc, S, I = g.nc, g.S, g.I
    with ExitStack() as st:
        sb = lambda name, shape, dt=F32: st.enter_context(nc.sbuf_tensor(name, shape, dt))
        wq = sb("awq", [128, 8, 3 * D], BF16); t_wq = toks(8, "awq")
        cst = Caster(g, st, "acst", 3 * D, nbuf=2)
        hT = [sb("ahT%d" % i, [128, 8, 512]) for i in range(2)]; t_hT = toks(2, "ahT")
        aT = [sb("aaT%d" % i, [128, 8, 512], BF16) for i in range(2)]; t_aT = toks(2, "aaT")
        ob = [sb("aob%d" % i, [128, 512], BF16) for i in range(4)]; t_ob = toks(4, "aob")
        pz = [st.enter_context(nc.psum_tensor("apz%d" % i, [128, 512], F32)) for i in range(4)]; t_pz = toks(4, "apz", True)
        npool = norm_pools(nc, st, "b")
        wv = I["na_w_qkv"].rearrange("(k p) f -> p k f", p=128)
        for k in range(8):
            cst.load(wq[:, k, :], wv[:, k, :], t_wq[k])
        rp = sb("arp", [16, 15, 127]); t_rp = Tok("arp")
        S.memset("pool", rp[:], 0.0, w=[t_rp])
        S.dma("sp", rp[:, :, 48:79], I["na_rpb"], w=[t_rp])
        S.dma("sp", g.rpbpad, rp[:], r=[t_rp])
        hTd = g.hT.rearrange("(c p) t -> p c t", p=128)
        nz = 0
        for ci, (c0, n) in enumerate(CHUNKS):
            b = ci % 2
            w_ = 1 if ci == 0 else 0
            S.dma("sp", hT[b][:, :, :n], hTd[:, :, c0:c0 + n], w=[t_hT[b]])
            norm_mod(g, hT[b], n, g.Gm[:, 1, w_, :], g.modT[:, 1, w_, 0:8], npool, t_hT[b], aT[b], t_aT[b])
            for ft in range(16):
                if ft < 8 and ci == 0:
                    continue
                zb = nz % 4; nz += 1
                for k in range(8):
                    S.mm(pz[zb][:, :n], wq[:, k, ft * 128:(ft + 1) * 128], aT[b][:, k, :n], k == 0, k == 7, r=[t_wq[k], t_aT[b]], w=[t_pz[zb]])
                S.copy("act" if ft % 2 else "dve", ob[zb][:, :n], pz[zb][:, :n], r=[t_pz[zb]], w=[t_ob[zb]])
                if ft < 8:
                    S.dma("sp", g.qT[ft * 128:(ft + 1) * 128, c0 - TC:c0 - TC + n], ob[zb][:, :n], r=[t_ob[zb]])
                else:
                    S.dma("sp", g.kT[(ft - 8) * 128:(ft - 7) * 128, c0:c0 + n], ob[zb][:, :n], r=[t_ob[zb]])
            for tt_ in range(n // 128):
                for half in range(2):
                    zb = nz % 4; nz += 1
                    for k in range(8):
                        S.mm(pz[zb][:, :], aT[b][:, k, tt_ * 128:(tt_ + 1) * 128], wq[:, k, 2048 + half * 512:2048 + (half + 1) * 512],
                             k == 0, k == 7, r=[t_wq[k], t_aT[b]], w=[t_pz[zb]])
                    S.copy("act" if half else "dve", ob[zb][:, :], pz[zb][:, :], r=[t_pz[zb]], w=[t_ob[zb]])
                    S.dma("act", g.vtok[c0 + tt_ * 128:c0 + (tt_ + 1) * 128, half * 512:(half + 1) * 512], ob[zb][:, :], r=[t_ob[zb]])
        S.barrier(); S.flush()


def phase_l1_attn(g):
    nc, S, I = g.nc, g.S, g.I
    GW = 64
    NB = 3
    with ExitStack() as st:
        sb = lambda name, shape, dt=F32: st.enter_context(nc.sbuf_tensor(name, shape, dt))
        V0 = sb("nV0", [128, 18, D], BF16)
        V1 = sb("nV1", [128, 16, D], BF16)
        t_V = Tok("nV")
        S.dma("sp", V0[:], g.vtok.rearrange("(j p) f -> p j f", p=128), w=[t_V])
        S.dma("act", V1[:, 0:15, :], g.vtok[TC + 64:TC + 64 + 15 * 128, :].rearrange("(j p) f -> p j f", p=128), w=[t_V])
        S.dma("sp", V1[0:64, 15, :], g.vtok[TC + 64 + 15 * 128:T, :], w=[t_V])
        identb = sb("nidb", [128, 128], BF16); t_k = Tok("nconst")
        S.copy("dve", identb[:], g.ident[:], r=[g.t_const], w=[t_k])
        kcr = sb("nkcr", [128, 64]); qc = sb("nqc", [128, 2]); Mcol = sb("nMcol", [128, 64]); m2 = sb("nm2", [128, 64])
        S.op("pool", lambda e: e.iota(kcr[:], [[1, 64]], base=0, channel_multiplier=0, allow_small_or_imprecise_dtypes=True), w=[t_k])
        S.op("pool", lambda e: e.iota(qc[0:64, 0:1], [[0, 1]], base=-8, channel_multiplier=1, allow_small_or_imprecise_dtypes=True), w=[t_k])
        S.op("pool", lambda e: e.iota(qc[64:128, 0:1], [[0, 1]], base=-8, channel_multiplier=1, allow_small_or_imprecise_dtypes=True), w=[t_k])
        S.ts("dve", qc[:, 0:1], qc[:, 0:1], 0.0, 48.0, ALU.max, ALU.min, r=[t_k], w=[t_k])
        S.ts("dve", qc[:, 1:2], qc[:, 0:1], 16.0, None, ALU.add, r=[t_k], w=[t_k])
        S.ts("dve", Mcol[:], kcr[:], qc[:, 0:1], None, ALU.is_ge, r=[t_k], w=[t_k])
        S.ts("dve", m2[:], kcr[:], qc[:, 1:2], None, ALU.is_lt, r=[t_k], w=[t_k])
        S.tt("dve", Mcol[:], Mcol[:], m2[:], ALU.mult, r=[t_k], w=[t_k])
        S.ts("dve", Mcol[:], Mcol[:], -NEG, NEG, ALU.mult, ALU.add, r=[t_k], w=[t_k])
        J = sb("nJ", [128, 128]); Bhr = sb("nBhr", [128, 15, 64]); t_Bhr = Tok("nBhr")
        S.memset("pool", J[:], 0.0, w=[t_k])
        for hh in range(2):
            S.op("pool", lambda e, hh=hh: e.affine_select(out=J[hh * 64:(hh + 1) * 64, hh * 64:(hh + 1) * 64], in_=J[hh * 64:(hh + 1) * 64, hh * 64:(hh + 1) * 64],
                                                        compare_op=ALU.not_equal, fill=1.0, base=-63, pattern=[[1, 64]], channel_multiplier=1),
                 r=[t_k], w=[t_k])
        qh = [sb("nqh%d" % i, [64, 2, TL], BF16) for i in range(2)]
        kh = [sb("nkh%d" % i, [64, 2, T], BF16) for i in range(2)]
        Bh = [sb("nBh%d" % i, [128, 15, 64]) for i in range(2)]
        oTh = [sb("noT%d" % i, [64, 2, TL], BF16) for i in range(2)]
        t_qk = toks(2, "nqk"); t_Bh = toks(2, "nBh"); t_oT = toks(2, "noT")
        Ssb = [sb("nS%d" % i, [128, 768]) for i in range(NB)]; t_S = toks(NB, "nS")
        Pn = [sb("nPn%d" % i, [128, 768], BF16) for i in range(NB)]; t_Pn = toks(NB, "nPn")
        PnT = [sb("nPnT%d" % i, [128, 6, 128], BF16) for i in range(NB)]; t_PnT = toks(NB, "nPnT")
        stt_ = [sb("nst%d" % i, [128, 4]) for i in range(NB)]; t_st = toks(NB, "nst")
        ps1 = [st.enter_context(nc.psum_tensor("nps1_%d" % i, [128, 512], F32)) for i in range(NB)]; t_ps1 = toks(NB, "nps1", True)
        ps2 = [st.enter_context(nc.psum_tensor("nps2_%d" % i, [128, 512], F32)) for i in range(NB)]; t_ps2 = toks(NB, "nps2", True)
        psT = [st.enter_context(nc.psum_tensor("npsT%d" % i, [128, 6, 128], BF16)) for i in range(2)]; t_psT = toks(2, "npsT", True)
        n = 0
        for hp in range(8):
            hb = hp % 2
            S.dma("sp", qh[hb][:], g.qT[hp * 128:(hp + 1) * 128, :].rearrange("(h k) t -> k h t", h=2), w=[t_qk[hb]])
            S.dma("act", kh[hb][:], g.kT[hp * 128:(hp + 1) * 128, :].rearrange("(h k) t -> k h t", h=2), w=[t_qk[hb]])
            for hh in range(2):
                src = bass.AP(tensor=g.rpbpad.tensor, offset=(2 * hp + hh) * 15 * 127, ap=[[1, 64], [127, 15], [1, 64]])
                S.dma("sp", Bhr[hh * 64:(hh + 1) * 64, :, :], src, w=[t_Bhr])
            Bflat = Bhr[:].rearrange("p a b -> p (a b)")
            S.mm(ps1[0][:, :], J[:], Bflat[:, 0:512], True, True, r=[t_k, t_Bhr], w=[t_ps1[0]])
            S.mm(ps2[0][:, 0:448], J[:], Bflat[:, 512:960], True, True, r=[t_k, t_Bhr], w=[t_ps2[0]])
            Mc = Mcol[:].unsqueeze(1)
            S.tt("dve", Bh[hb][:, 0:8, :], ps1[0][:, :].rearrange("p (a b) -> p a b", b=64), Mc.to_broadcast([128, 8, 64]), ALU.add,
                 r=[t_ps1[0], t_k], w=[t_Bh[hb]])
            S.tt("dve", Bh[hb][:, 8:15, :], ps2[0][:, 0:448].rearrange("p (a b) -> p a b", b=64), Mc.to_broadcast([128, 7, 64]), ALU.add,
                 r=[t_ps2[0], t_k], w=[t_Bh[hb]])
            for r in range(32):
                b = n % NB; bt = n % 2; n += 1
                rs = min(max(r - 4, 0), 24)
                ri0 = rs - r + 7
                qs = slice(r * GW, (r + 1) * GW)
                k0 = TC + rs * GW
                for hh in range(2):
                    po = slice(hh * 64, (hh + 1) * 64)
                    S.mm(ps1[b][po, :], qh[hb][:, hh, qs], kh[hb][:, hh, k0:k0 + 512], True, True, r=[t_qk[hb]], w=[t_ps1[b]])
                    S.mm(ps2[b][po, 0:256], qh[hb][:, hh, qs], kh[hb][:, hh, 0:TC], True, True, r=[t_qk[hb]], w=[t_ps2[b]])
                S.stt(Ssb[b][:, 0:512], ps1[b][:, :], 0.125, Bh[hb][:, ri0:ri0 + 8, :].rearrange("p a b -> p (a b)"), ALU.mult, ALU.add,
                      r=[t_ps1[b], t_Bh[hb]], w=[t_S[b]])
                S.act(Ssb[b][:, 512:768], ps2[b][:, 0:256], AF.Copy, scale=0.125, r=[t_ps2[b]], w=[t_S[b]])
                S.reduce(stt_[b][:, 0:1], Ssb[b][:], ALU.max, r=[t_S[b]], w=[t_st[b]])
                S.ts("dve", stt_[b][:, 1:2], stt_[b][:, 0:1], -1.0, None, ALU.mult, r=[t_st[b]], w=[t_st[b]])
                S.act(Ssb[b][:], Ssb[b][:], AF.Exp, bias=stt_[b][:, 1:2], scale=1.0, accum_out=stt_[b][:, 2:3], r=[t_S[b], t_st[b]], w=[t_S[b], t_st[b]])
                S.recip(stt_[b][:, 3:4], stt_[b][:, 2:3], r=[t_st[b]], w=[t_st[b]])
                S.ts("dve", Pn[b][:], Ssb[b][:], stt_[b][:, 3:4], None, ALU.mult, r=[t_S[b], t_st[b]], w=[t_Pn[b]])
                for blk in range(6):
                    S.tr(psT[bt][:, blk, :], Pn[b][:, blk * 128:(blk + 1) * 128], identb[:], r=[t_Pn[b], t_k], w=[t_psT[bt]])
                S.copy("act", PnT[b][:].rearrange("p a b -> p (a b)"), psT[bt][:].rearrange("p a b -> p (a b)"), r=[t_psT[bt]], w=[t_PnT[b]])
                for hh in range(2):
                    hc = slice((2 * hp + hh) * 64, (2 * hp + hh + 1) * 64)
                    for blk in range(6):
                        if blk < 4:
                            vb = V0[:, 2 + rs // 2 + blk, hc] if rs % 2 == 0 else V1[:, (rs - 1) // 2 + blk, hc]
                        else:
                            vb = V0[:, blk - 4, hc]
                        S.mm(ps2[b][0:64, 256 + hh * 64:256 + (hh + 1) * 64], vb, PnT[b][:, blk, hh * 64:(hh + 1) * 64], blk == 0, blk == 5,
                             r=[t_V, t_PnT[b]], w=[t_ps2[b]])
                S.copy("dve", oTh[hb][:, :, qs], ps2[b][0:64, 256:384].rearrange("p (h q) -> p h q", h=2), r=[t_ps2[b]], w=[t_oT[hb]])
            S.dma("sp", g.oT[hp * 128:(hp + 1) * 128, :].rearrange("(h k) t -> k h t", h=2), oTh[hb][:], r=[t_oT[hb]])
        S.barrier(); S.flush()


def phase_l1_post(g):
    nc, S, I = g.nc, g.S, g.I
    NT = TL
    with ExitStack() as st:
        sb = lambda name, shape, dt=F32: st.enter_context(nc.sbuf_tensor(name, shape, dt))
        h = sb("ph", [128, 8, NT]); t_h = Tok("ph")
        aT = sb("paT", [128, 8, NT], BF16); t_aT = Tok("paT")
        gatesT = sb("pgatesT", [8, NT]); t_gates = Tok("gatesT")
        Sel8 = sb("pSel8", [8, 8, 128]); t_k = Tok("pconst")
        S.copy("dve", Sel8[:], g.ident[0:8, 0:8].unsqueeze(2).to_broadcast([8, 8, 128]), r=[g.t_const], w=[t_k])
        hTd = g.hT.rearrange("(c p) t -> p c t", p=128)
        for c in range(8):
            S.dma("sp" if c % 2 else "act", h[:, c, :], hTd[:, c, TC:T], w=[t_h])
        chunks = tok_chunks(NT)
        with ExitStack() as st2:
            sb2 = lambda name, shape, dt=F32: st2.enter_context(nc.sbuf_tensor(name, shape, dt))
            wout = sb2("pwout", [128, 8, D], BF16); t_wout = Tok("pwout")
            cst = Caster(g, st2, "pcst", D, nbuf=1)
            ob = [sb2("pob%d" % i, [128, 8, 512], BF16) for i in range(1)] * 2; t_ob = toks(1, "pob") * 2
            a32 = sb2("pa32", [128, 8, 512]); t_a32 = Tok("pa32")
            wr = sb2("pwr", [128, 8, 128]); t_wr = Tok("pwr")
            logT = sb2("plogT", [8, NT]); t_log = Tok("plogT")
            L = sb2("pL", [128, 16, 8]); W = sb2("pW", [128, 16, 8]); tmp = sb2("ptmp", [128, 16, 8]); t_L = Tok("pL")
            m1 = sb2("pm1", [128, 16]); m2_ = sb2("pm2", [128, 16]); den = sb2("pden", [128, 16])
            po = [st2.enter_context(nc.psum_tensor("ppo%d" % i, [128, 512], F32)) for i in range(3)]; t_po = toks(3, "ppo", True)
            pl = st2.enter_context(nc.psum_tensor("ppl", [128, 512], F32)); t_pl = Tok("ppl", True)
            npool = norm_pools(nc, st2, "p")
            rb, t_rb = load_cols(g, st2, "prb", [I["moe_router_b"].rearrange("(o p) -> o p", o=1)], ps=pl[:, 0:128], t_p=t_pl)
            wv = I["na_w_out"].rearrange("(k p) f -> p k f", p=128)
            for k in range(8):
                cst.load(wout[:, k, :], wv[:, k, :], t_wout)
            S.memset("pool", wr[:], 0.0, w=[t_wr])
            S.dma("sp", wr[:, :, 0:8], I["moe_router_w"].rearrange("(k p) e -> p k e", p=128), w=[t_wr])
            oTv = g.oT.rearrange("(k p) t -> p k t", p=128)
            npo = 0
            for ci, (c0, n) in enumerate(chunks):
                b = ci % 2
                S.dma("sp", ob[b][:, :, :n], oTv[:, :, c0:c0 + n], w=[t_ob[b]])
                for ft in range(8):
                    p = po[npo % 3]; tp = t_po[npo % 3]; npo += 1
                    for k in range(8):
                        S.mm(p[:, :n], wout[:, k, ft * 128:(ft + 1) * 128], ob[b][:, k, :n], k == 0, k == 7, r=[t_wout, t_ob[b]], w=[tp])
                    S.stt(h[:, ft, c0:c0 + n], p[:, :n], g.modT[:, 1, 0, 16 + ft:17 + ft], h[:, ft, c0:c0 + n], ALU.mult, ALU.add,
                          r=[tp, t_h, g.t_mod], w=[t_h])
                norm_mod(g, h[:, :, c0:c0 + n], n, g.Gf[:, 1, 0, :], g.modT[:, 1, 0, 24:32], npool, t_h, aT[:, :, c0:c0 + n], t_aT,
                         out_f32=a32)
                for k in range(8):
                    S.mm(pl[:, :n], wr[:, k, :], a32[:, k, :n], k == 0, k == 7, r=[t_wr, t_aT], w=[t_pl])
                S.ts("dve", logT[:, c0:c0 + n], pl[0:8, :n], rb[0:8, 0:1], None, ALU.add, r=[t_pl, t_rb], w=[t_log])
            if "h1att_o" in g.dbg:
                dd = nc.dram_tensor("h1attT", [D, NT], F32, kind="ExternalOutput").ap()
                S.dma("sp", dd.rearrange("(c p) t -> p c t", p=128), h[:], r=[t_h])
            for j in range(16):
                S.tr(pl[:, j * 8:(j + 1) * 8], logT[:, j * 128:(j + 1) * 128], g.ident[0:8, 0:8], r=[t_log, g.t_const], w=[t_pl])
            S.copy("dve", L[:].rearrange("p a b -> p (a b)"), pl[:, 0:128], r=[t_pl], w=[t_L])
            if "logits_o" in g.dbg:
                dd = nc.dram_tensor("logitsL", [128, 128], F32, kind="ExternalOutput").ap()
                S.dma("sp", dd[:, :], L[:].rearrange("p a b -> p (a b)"), r=[t_L])
            b3 = lambda a: a[:].unsqueeze(2).to_broadcast([128, 16, 8])
            S.reduce(m1[:], L[:], ALU.max, r=[t_L], w=[t_L])
            S.tt("dve", tmp[:], L[:], b3(m1), ALU.is_equal, r=[t_L], w=[t_L])
            S.stt(tmp[:], tmp[:], NEG, L[:], ALU.mult, ALU.add, r=[t_L], w=[t_L])
            S.reduce(m2_[:], tmp[:], ALU.max, r=[t_L], w=[t_L])
            S.tt("dve", tmp[:], L[:], b3(m2_), ALU.is_ge, r=[t_L], w=[t_L])
            S.tt("dve", W[:], L[:], b3(m1), ALU.subtract, r=[t_L], w=[t_L])
            S.act(W[:], W[:], AF.Exp, r=[t_L], w=[t_L])
            S.tt("dve", W[:], W[:], tmp[:], ALU.mult, r=[t_L], w=[t_L])
            S.reduce(den[:], W[:], ALU.add, r=[t_L], w=[t_L])
            S.recip(den[:], den[:], r=[t_L], w=[t_L])
            S.tt("dve", W[:], W[:], b3(den), ALU.mult, r=[t_L], w=[t_L])
            for j4 in range(4):
                for j in range(4):
                    S.tr(pl[0:8, j * 128:(j + 1) * 128], W[:, j4 * 4 + j, :], g.ident[:], r=[t_L, g.t_const], w=[t_pl])
                S.copy("dve", gatesT[:, j4 * 512:(j4 + 1) * 512], pl[0:8, :], r=[t_pl], w=[t_gates])
            S.barrier(); S.flush()
        gbs = [sb("pgb%d" % i, [128, NT]) for i in range(2)]; t_gb = toks(2, "pgb")
        pg = st.enter_context(nc.psum_tensor("ppg", [128, 512], F32)); t_pg = Tok("ppg", True)
        gate_col = lambda c0: g.modT[:, 1, 0, 40:48]
        pools = None
        NEXP = int(os.environ.get("MOE_NE", str(NE)))
        for e in range(NEXP):
            gb, tgb = gbs[e % 2], t_gb[e % 2]
            for (c0, n) in chunks:
                S.mm(pg[:, :n], Sel8[:, e, :], gatesT[:, c0:c0 + n], True, True, r=[t_k, t_gates], w=[t_pg])
                S.copy("act", gb[:, c0:c0 + n], pg[:, :n], r=[t_pg], w=[tgb])
            pools = swiglu_pass(g, st, aT, t_aT, h, t_h, NT, I["moe_w1"][e], I["moe_w3"][e], I["moe_w2"][e], gate_col, "p",
                                gb=gb, t_gb=tgb, pools=pools)
        if "h1_o" in g.dbg:
            dd = nc.dram_tensor("h1T", [D, NT], F32, kind="ExternalOutput").ap()
            S.dma("sp", dd.rearrange("(c p) t -> p c t", p=128), h[:], r=[t_h])
        with ExitStack() as st3:
            sb3 = lambda name, shape, dt=F32: st3.enter_context(nc.sbuf_tensor(name, shape, dt))
            sq = sb3("psq", [128, 8, 512]); rstd = sb3("prstd", [128, 512]); y = sq
            ot = [gbs[i][:, 0:D] for i in range(2)]
            t_sq, t_rstd = Tok("psq"), Tok("prstd"); t_yy = t_sq; t_ot = t_gb
            pt = [pools["ph"][i] for i in range(4)]; t_pt = [pools["t_ph"][i] for i in range(4)]
            pss = pools["po"][0]; t_pss = pools["t_po"][0]
            no = 0
            for (c0, n) in chunks:
                S.act(sq[:, :, :n], h[:, :, c0:c0 + n], AF.Square, r=[t_h], w=[t_sq])
                for c in range(8):
                    S.mm(pss[:, :n], g.ones[:], sq[:, c, :n], c == 0, c == 7, r=[t_sq, g.t_const], w=[t_pss])
                S.ts("dve", rstd[:, :n], pss[:, :n], 1.0 / D, NORM_EPS, ALU.mult, ALU.add, r=[t_pss], w=[t_rstd])
                S.act(rstd[:, :n], rstd[:, :n], AF.Sqrt, r=[t_rstd], w=[t_rstd])
                S.recip(rstd[:, :n], rstd[:, :n], r=[t_rstd], w=[t_rstd])
                for c in range(8):
                    S.stt(y[:, c, :n], h[:, c, c0:c0 + n], g.nfin[:, c:c + 1], rstd[:, :n], ALU.mult, ALU.mult, r=[t_h, t_rstd, g.t_mod], w=[t_yy])
                for tt_ in range(n // 128):
                    o_ = ot[no % 2]; to = t_ot[no % 2]; no += 1
                    for half in range(2):
                        p = pt[(2 * no + half) % 4]; tp = t_pt[(2 * no + half) % 4]
                        for c in range(4):
                            S.tr(p[:, c * 128:(c + 1) * 128], y[:, half * 4 + c, tt_ * 128:(tt_ + 1) * 128], g.ident[:], r=[t_yy, g.t_const], w=[tp])
                        S.copy("act" if half else "dve", o_[:, half * 512:(half + 1) * 512], p[:, :], r=[tp], w=[to])
                    S.dma("sp", g.out[c0 + tt_ * 128:c0 + (tt_ + 1) * 128, :], o_[:], r=[to])
            S.barrier(); S.flush()
        S.barrier(); S.flush()
```
